# Optimizing a Trainium2 kernel written in Bass

```python
import jax, jax.numpy as jnp
from jax import lax
import numpy as np

D_MODEL = 1024
BATCH = 32
SEQ = 2048
DEPTH = 2

CHUNK = 64
P_DIM = 256
SB_HEAD_DIM = 64
SB_WIDTH = 3 * D_MODEL // 8
SB_HEADS = SB_WIDTH // SB_HEAD_DIM
SB_QBLOCK = 128
ML_HEADS = 4
ML_WIDTH = 3 * D_MODEL // 8
ML_HEAD_DIM = ML_WIDTH // ML_HEADS
CONV_K = 4
SGU_WIDTH = D_MODEL - SB_WIDTH - ML_WIDTH
SGU_GROUPS = 4
SGU_GROUP_DIM = SGU_WIDTH // SGU_GROUPS
SGU_BLOCK = 128
IN_SPLITS = (SB_WIDTH, SB_WIDTH, SB_WIDTH, ML_WIDTH, ML_WIDTH, ML_WIDTH, ML_WIDTH, ML_HEADS, ML_HEADS, SGU_WIDTH, SGU_WIDTH)
N_IN = 3 * SB_WIDTH + 4 * ML_WIDTH + 2 * ML_HEADS + 2 * SGU_WIDTH
N_GROUPS = 4
EXPERTS_PER_GROUP = 8
N_EXPERTS = N_GROUPS * EXPERTS_PER_GROUP
TOP_K = 2
D_EXPERT = D_MODEL // 4
MOE_BLOCK = 512
EPS = 1e-6

kernel_name = "hymba_style_sb_mlstm_gmlp_hmoe_ple"

F32 = jnp.float32


def rms_norm(x, g):
    x32 = x.astype(F32)
    return x32 * lax.rsqrt(jnp.mean(x32 * x32, axis=-1, keepdims=True) + EPS) * g.astype(F32)


def split_cols(z, sizes):
    idx = np.cumsum(sizes)[:-1].tolist()
    return jnp.split(z, idx, axis=-1)


def causal_depthwise_conv(x, w, b):
    y = lax.conv_general_dilated(x.astype(F32), w.astype(F32)[:, None, :], window_strides=(1,),
                                 padding=[(CONV_K - 1, 0)], dimension_numbers=('NWC', 'WIO', 'NWC'),
                                 feature_group_count=x.shape[-1])
    return y + b.astype(F32)


def stick_breaking_attention(q, k, v):
    S = q.shape[1]
    scale = q.shape[-1] ** -0.5
    outs = []
    for n in range(S // SB_QBLOCK):
        t0 = n * SB_QBLOCK
        kend = t0 + SB_QBLOCK
        z = jnp.einsum('bthd,bshd->bhts', q[:, t0:kend], k[:, :kend]) * scale
        tpos = t0 + jnp.arange(SB_QBLOCK)
        spos = jnp.arange(kend)
        mask = spos[None, :] < tpos[:, None]
        log_1m = jnp.where(mask, -jax.nn.softplus(z), 0.0)
        after = lax.cumsum(log_1m, axis=3, reverse=True) - log_1m
        a = jnp.where(mask, jnp.exp(jax.nn.log_sigmoid(z) + after), 0.0)
        outs.append(jnp.einsum('bhts,bshd->bthd', a, v[:, :kend]))
    return jnp.concatenate(outs, axis=1)


def mlstm_chunkwise(q, k, v, i_pre, f_pre):
    Bn, S, H, d = q.shape
    N, L = S // CHUNK, CHUNK
    q = q.reshape(Bn, N, L, H, d)
    k = k.reshape(Bn, N, L, H, d) * (d ** -0.5)
    v = v.reshape(Bn, N, L, H, d)
    ig = i_pre.reshape(Bn, N, L, H)
    bcum = jnp.cumsum(jax.nn.log_sigmoid(f_pre.reshape(Bn, N, L, H)), axis=2)
    btot = bcum[:, :, -1]
    a = btot[:, :, None] - bcum + ig

    def step(carry, xs):
        c, nvec, m = carry
        k_c, v_c, a_c, bt_c = xs
        m_new = jnp.maximum(bt_c + m, a_c.max(axis=1))
        decay = jnp.exp(bt_c + m - m_new)
        w = jnp.exp(a_c - m_new[:, None])
        c_new = decay[..., None, None] * c + jnp.einsum('blh,blhd,blhe->bhde', w, k_c, v_c)
        n_new = decay[..., None] * nvec + jnp.einsum('blh,blhd->bhd', w, k_c)
        return (c_new, n_new, m_new), (c, nvec, m)

    init = (jnp.zeros((Bn, H, d, d), F32), jnp.zeros((Bn, H, d), F32), jnp.zeros((Bn, H), F32))
    xs = (jnp.moveaxis(k, 1, 0), jnp.moveaxis(v, 1, 0), jnp.moveaxis(a, 1, 0), jnp.moveaxis(btot, 1, 0))
    _, (c_in, n_in, m_in) = lax.scan(step, init, xs)
    c_in = jnp.moveaxis(c_in, 0, 1)
    n_in = jnp.moveaxis(n_in, 0, 1)
    m_in = jnp.moveaxis(m_in, 0, 1)

    bT = jnp.moveaxis(bcum, 2, -1)
    igT = jnp.moveaxis(ig, 2, -1)
    causal = jnp.tril(jnp.ones((L, L), dtype=bool))
    dmat = jnp.where(causal, bT[..., :, None] - bT[..., None, :] + igT[..., None, :], -jnp.inf)
    m_inter = bT + m_in[..., None]
    m_t = jnp.maximum(m_inter, dmat.max(axis=-1))
    wd = jnp.where(causal, jnp.exp(dmat - m_t[..., None]), 0.0)
    s = jnp.einsum('bnthd,bnshd->bnhts', q, k) * wd
    inter = jnp.exp(m_inter - m_t)
    num = jnp.einsum('bnhts,bnshd->bnthd', s, v) + jnp.einsum('bnhl,bnlhd,bnhde->bnlhe', inter, q, c_in)
    den = s.sum(axis=-1) + inter * jnp.einsum('bnlhd,bnhd->bnhl', q, n_in)
    den = jnp.maximum(jnp.abs(den), jnp.exp(-m_t))
    h = num / jnp.moveaxis(den, 2, -1)[..., None]
    return h.reshape(Bn, S, H, d)


def spatial_gating(u, v, ln_g, ln_b, w_s, b_s):
    Bn, S, _ = u.shape
    v = v.reshape(Bn, S, SGU_GROUPS, SGU_GROUP_DIM)
    mu = v.mean(axis=-1, keepdims=True)
    var = jnp.mean(jnp.square(v - mu), axis=-1, keepdims=True)
    v = (v - mu) * lax.rsqrt(var + EPS) * ln_g.reshape(SGU_GROUPS, SGU_GROUP_DIM) + ln_b.reshape(SGU_GROUPS, SGU_GROUP_DIM)
    v = v.reshape(Bn, S // SGU_BLOCK, SGU_BLOCK, SGU_GROUPS, SGU_GROUP_DIM)
    cpos = jnp.arange(SGU_BLOCK) // CHUNK
    mask = cpos[None, :] <= cpos[:, None]
    w = jnp.where(mask[None], w_s.astype(F32), 0.0)
    mixed = jnp.einsum('gts,bnsgc->bntgc', w, v) + b_s.astype(F32).T[:, :, None]
    return u * mixed.reshape(Bn, S, SGU_WIDTH)


def routed_experts(xt, e_idx, e_w, w_gate, w_up, w_down):
    T, D = xt.shape
    M = T * TOP_K
    flat_e = e_idx.reshape(M)
    order = jnp.argsort(flat_e)
    sorted_e = flat_e[order]
    sorted_tok = order // TOP_K
    sorted_w = e_w.reshape(M)[order]
    sizes = jnp.bincount(flat_e, length=N_EXPERTS)
    padded = (sizes + MOE_BLOCK - 1) // MOE_BLOCK * MOE_BLOCK
    start = jnp.cumsum(sizes) - sizes
    pend = jnp.cumsum(padded)
    pstart = pend - padded
    dest = pstart[sorted_e] + jnp.arange(M) - start[sorted_e]
    n_blocks = -(-(M + N_EXPERTS * (MOE_BLOCK - 1)) // MOE_BLOCK)
    rows = n_blocks * MOE_BLOCK
    row_tok = jnp.zeros((rows,), jnp.int32).at[dest].set(sorted_tok.astype(jnp.int32))
    row_w = jnp.zeros((rows,), F32).at[dest].set(sorted_w)
    block_e = jnp.minimum(jnp.searchsorted(pend, jnp.arange(n_blocks) * MOE_BLOCK, side='right'), N_EXPERTS - 1)

    def block_ffn(args):
        tok, wt, e = args
        xb = xt[tok]
        hb = jax.nn.silu(xb @ w_gate[e]) * (xb @ w_up[e])
        return (hb @ w_down[e]).astype(F32) * wt[:, None]

    yb = lax.map(block_ffn, (row_tok.reshape(n_blocks, MOE_BLOCK), row_w.reshape(n_blocks, MOE_BLOCK), block_e))
    return jnp.zeros((T, D), F32).at[row_tok].add(yb.reshape(rows, D))


def hierarchical_moe(h, rg_w, rg_b, re_w, re_b, w_gate, w_up, w_down):
    Bn, S, D = h.shape
    xt = h.reshape(Bn * S, D)
    g_logits = (xt @ rg_w + rg_b).astype(F32)
    g_prob = jax.nn.softmax(g_logits, axis=-1)
    g_top = jnp.argmax(g_logits, axis=-1)
    g_w = jnp.take_along_axis(g_prob, g_top[:, None], axis=-1)[:, 0]
    e_logits = (xt @ re_w + re_b).astype(F32).reshape(-1, N_GROUPS, EXPERTS_PER_GROUP)
    e_in = jnp.take_along_axis(e_logits, g_top[:, None, None], axis=1)[:, 0]
    e_val, e_loc = lax.top_k(e_in, TOP_K)
    e_w = jax.nn.softmax(e_val, axis=-1) * g_w[:, None]
    e_idx = g_top[:, None].astype(jnp.int32) * EXPERTS_PER_GROUP + e_loc.astype(jnp.int32)
    y = routed_experts(xt, e_idx, e_w, w_gate, w_up, w_down)
    return y.reshape(Bn, S, D)


def setup_inputs(seed: int = 0) -> dict:
    key = jax.random.key(seed)
    ks = jax.random.split(key, 32)

    def nrm(k, shape, scale):
        return jax.random.normal(k, shape, F32) * scale

    def gain(k, shape):
        return 1.0 + 0.05 * jax.random.normal(k, shape, F32)

    D = D_MODEL
    return {
        "x": nrm(ks[0], (BATCH, SEQ, D), 1.0),
        "p": nrm(ks[1], (DEPTH, BATCH, SEQ, P_DIM), 1.0),
        "norm1_g": gain(ks[2], (DEPTH, D)),
        "w_in": nrm(ks[3], (DEPTH, D, N_IN), D ** -0.5),
        "conv_w": nrm(ks[4], (DEPTH, CONV_K, 2 * ML_WIDTH), CONV_K ** -0.5),
        "conv_b": nrm(ks[5], (DEPTH, 2 * ML_WIDTH), 0.01),
        "igate_b": nrm(ks[6], (DEPTH, ML_HEADS), 0.1),
        "fgate_b": jnp.linspace(3.0, 6.0, ML_HEADS, dtype=F32)[None, :] + nrm(ks[7], (DEPTH, ML_HEADS), 0.1),
        "mnorm_g": gain(ks[8], (DEPTH, ML_WIDTH)),
        "sb_out_g": gain(ks[9], (DEPTH, SB_WIDTH)),
        "sgu_ln_g": gain(ks[10], (DEPTH, SGU_WIDTH)),
        "sgu_ln_b": nrm(ks[11], (DEPTH, SGU_WIDTH), 0.01),
        "sgu_w": nrm(ks[12], (DEPTH, SGU_GROUPS, SGU_BLOCK, SGU_BLOCK), SGU_BLOCK ** -0.5),
        "sgu_b": gain(ks[13], (DEPTH, SGU_GROUPS, SGU_BLOCK)),
        "sgu_out_g": gain(ks[14], (DEPTH, SGU_WIDTH)),
        "w_out": nrm(ks[15], (DEPTH, D, D), D ** -0.5),
        "norm2_g": gain(ks[16], (DEPTH, D)),
        "router_gw": nrm(ks[17], (DEPTH, D, N_GROUPS), D ** -0.5),
        "router_gb": nrm(ks[18], (DEPTH, N_GROUPS), 0.01),
        "router_ew": nrm(ks[19], (DEPTH, D, N_EXPERTS), D ** -0.5),
        "router_eb": nrm(ks[20], (DEPTH, N_EXPERTS), 0.01),
        "w_gate": nrm(ks[21], (DEPTH, N_EXPERTS, D, D_EXPERT), D ** -0.5),
        "w_up": nrm(ks[22], (DEPTH, N_EXPERTS, D, D_EXPERT), D ** -0.5),
        "w_down": nrm(ks[23], (DEPTH, N_EXPERTS, D_EXPERT, D), D_EXPERT ** -0.5),
        "ple_norm_g": gain(ks[24], (DEPTH, D)),
        "ple_gate_w": nrm(ks[25], (DEPTH, D, D), D ** -0.5),
        "ple_proj_w": nrm(ks[26], (DEPTH, P_DIM, D), P_DIM ** -0.5),
        "final_g": gain(ks[27], (D,)),
    }


def reference(x, p, norm1_g, w_in, conv_w, conv_b, igate_b, fgate_b, mnorm_g, sb_out_g,
              sgu_ln_g, sgu_ln_b, sgu_w, sgu_b, sgu_out_g, w_out, norm2_g,
              router_gw, router_gb, router_ew, router_eb, w_gate, w_up, w_down,
              ple_norm_g, ple_gate_w, ple_proj_w, final_g):
    Bn, S, D = x.shape
    h = x.astype(F32)
    for i in range(DEPTH):
        hn = rms_norm(h, norm1_g[i])
        z = hn @ w_in[i]
        qa, ka, va, qb, kb, vb, ob, ib, fb, uc, vc = split_cols(z, IN_SPLITS)
        ya = stick_breaking_attention(qa.reshape(Bn, S, SB_HEADS, SB_HEAD_DIM),
                                      ka.reshape(Bn, S, SB_HEADS, SB_HEAD_DIM),
                                      va.reshape(Bn, S, SB_HEADS, SB_HEAD_DIM)).reshape(Bn, S, SB_WIDTH)
        ya = rms_norm(ya, sb_out_g[i])
        qk = jax.nn.silu(causal_depthwise_conv(jnp.concatenate([qb, kb], axis=-1), conv_w[i], conv_b[i]))
        qb, kb = jnp.split(qk, 2, axis=-1)
        hb = mlstm_chunkwise(qb.reshape(Bn, S, ML_HEADS, ML_HEAD_DIM),
                             kb.reshape(Bn, S, ML_HEADS, ML_HEAD_DIM),
                             vb.astype(F32).reshape(Bn, S, ML_HEADS, ML_HEAD_DIM),
                             ib + igate_b[i], fb + fgate_b[i])
        hb = rms_norm(hb, mnorm_g[i].reshape(ML_HEADS, ML_HEAD_DIM)).reshape(Bn, S, ML_WIDTH)
        yb = hb * jax.nn.sigmoid(ob)
        yc = spatial_gating(jax.nn.gelu(uc), jax.nn.gelu(vc), sgu_ln_g[i], sgu_ln_b[i], sgu_w[i], sgu_b[i])
        yc = rms_norm(yc, sgu_out_g[i])
        h = h + jnp.concatenate([ya, yb, yc], axis=-1) @ w_out[i]
        h = h + hierarchical_moe(rms_norm(h, norm2_g[i]), router_gw[i], router_gb[i], router_ew[i], router_eb[i],
                                 w_gate[i], w_up[i], w_down[i])
        gate = jax.nn.sigmoid(rms_norm(h, ple_norm_g[i]) @ ple_gate_w[i])
        h = h + gate * (p[i] @ ple_proj_w[i]).astype(F32)
    return rms_norm(h, final_g).astype(x.dtype)
```

```python
import contextlib
import numpy as np
import concourse.bass as bass
import concourse.mybir as mybir

F32 = mybir.dt.float32
BF16 = mybir.dt.bfloat16
I32 = mybir.dt.int32
U32 = mybir.dt.uint32
AF = mybir.ActivationFunctionType
ALU = mybir.AluOpType
AX = mybir.AxisListType


def _dsize(dt):
    return mybir.dt.size(dt)


class Rec:
    ENG = ['pe', 'act', 'dve', 'pool', 'sp']

    def __init__(self, nc, same_engine_sync=True):
        self.nc = nc
        self.ops = {e: [] for e in self.ENG}
        self.cnt = {e: 0 for e in self.ENG}
        self.waited = {e: {} for e in self.ENG}
        self.recs = {}
        self.nslot = {'sp': 8, 'pool': 8, 'act': 4}
        self.slot_cnt = {e: [0] * n for e, n in self.nslot.items()}
        self.slot_rr = {e: 0 for e in self.nslot}
        self.ses = same_engine_sync
        self.nops = 0

    def rng(self, ap):
        if isinstance(ap, tuple):
            return ap
        t = ap.tensor
        name = t.name
        sz = _dsize(ap.dtype)
        dims = list(ap.ap)
        if isinstance(t, bass.DRamTensorHandle):
            lo = ap.offset
            hi = lo + sum((c - 1) * abs(s) for s, c in dims) + 1
            return (name, 0, 1, lo * sz, hi * sz)
        pstride, pcount = dims[0]
        if pstride == 0:
            pstride = 1 << 40
        plo = ap.offset // pstride if pstride < (1 << 40) else 0
        flo = ap.offset - plo * pstride if pstride < (1 << 40) else ap.offset
        fhi = flo + sum((c - 1) * abs(s) for s, c in dims[1:]) + 1
        return (name, plo, plo + pcount, flo * sz, fhi * sz)

    def whole(self, t):
        return (t.name, 0, 1 << 20, 0, 1 << 60)

    def add(self, eng, fn, reads=(), writes=(), dma=False):
        deps = set()
        rr = [self.rng(a) for a in reads]
        ww = [self.rng(a) for a in writes]
        for (name, plo, phi, flo, fhi) in rr:
            for r in self.recs.get(name, ()):
                if r[4] and r[0] < phi and plo < r[1] and r[2] < fhi and flo < r[3]:
                    deps.add(r[5])
        for (name, plo, phi, flo, fhi) in ww:
            for r in self.recs.get(name, ()):
                if r[0] < phi and plo < r[1] and r[2] < fhi and flo < r[3]:
                    deps.add(r[5])
        waits = {}
        for (semkey, val) in deps:
            if semkey[0] == 'c' and semkey[1] == eng:
                if eng == 'pe' or not self.ses:
                    continue
            if self.waited[eng].get(semkey, 0) >= val:
                continue
            if waits.get(semkey, 0) < val:
                waits[semkey] = val
        if dma:
            s = self.slot_rr[eng]
            self.slot_rr[eng] = (s + 1) % self.nslot[eng]
            semkey = ('d', eng, s)
            prev = self.slot_cnt[eng][s]
            if prev > 0 and self.waited[eng].get(semkey, 0) < 16 * prev:
                if waits.get(semkey, 0) < 16 * prev:
                    waits[semkey] = 16 * prev
            self.slot_cnt[eng][s] = prev + 1
            token = (semkey, 16 * (prev + 1))
        else:
            self.cnt[eng] += 1
            token = (('c', eng), self.cnt[eng])
        for k, v in waits.items():
            self.waited[eng][k] = v
        self.ops[eng].append((waits, fn, token))
        self.nops += 1
        for (name, plo, phi, flo, fhi) in ww:
            lst = self.recs.setdefault(name, [])
            lst[:] = [r for r in lst if not (plo <= r[0] and r[1] <= phi and flo <= r[2] and r[3] <= fhi)]
            lst.append((plo, phi, flo, fhi, True, token))
        for (name, plo, phi, flo, fhi) in rr:
            lst = self.recs.setdefault(name, [])
            sk = token[0]
            lst[:] = [r for r in lst if not ((not r[4]) and r[5][0] == sk and r[0] == plo and r[1] == phi
                                             and r[2] == flo and r[3] == fhi)]
            lst.append((plo, phi, flo, fhi, False, token))
        return token

    def wait_all(self, eng, tokens):
        waits = {}
        for (semkey, val) in tokens:
            if waits.get(semkey, 0) < val:
                waits[semkey] = val
        for k, v in waits.items():
            self.waited[eng][k] = max(self.waited[eng].get(k, 0), v)
        self.ops[eng].append((waits, None, None))

    def emit(self):
        nc = self.nc
        needed = {e: set() for e in self.ENG}
        for e in self.ENG:
            for (waits, fn, token) in self.ops[e]:
                for (semkey, val) in waits.items():
                    if semkey[0] == 'c':
                        needed[semkey[1]].add(val)
        rank = {}
        for e in self.ENG:
            srt = sorted(needed[e])
            rank[e] = {v: i + 1 for i, v in enumerate(srt)}
        with contextlib.ExitStack() as st:
            sems = {}
            for e in self.ENG:
                sems[('c', e)] = st.enter_context(nc.semaphore(f"c_{e}"))
            for e, n in self.nslot.items():
                for s in range(n):
                    sems[('d', e, s)] = st.enter_context(nc.semaphore(f"d_{e}_{s}"))
            block = st.enter_context(nc.Block())
            rec = self

            def replay(e, eng):
                for (waits, fn, token) in rec.ops[e]:
                    for (semkey, val) in waits.items():
                        if semkey[0] == 'c':
                            val = rank[semkey[1]][val]
                        eng.wait_ge(sems[semkey], val)
                    if fn is None:
                        continue
                    inst = fn(eng)
                    semkey, val = token
                    if semkey[0] == 'd':
                        inst.then_inc(sems[semkey], 16)
                    elif val in rank[e]:
                        inst.then_inc(sems[semkey], 1)

            if self.ops['pe']:
                @block.tensor
                def _(eng):
                    replay('pe', eng)
            if self.ops['act']:
                @block.scalar
                def _(eng):
                    replay('act', eng)
            if self.ops['dve']:
                @block.vector
                def _(eng):
                    replay('dve', eng)
            if self.ops['pool']:
                @block.gpsimd
                def _(eng):
                    replay('pool', eng)
            if self.ops['sp']:
                @block.sync
                def _(eng):
                    replay('sp', eng)

    def dma(self, eng, out, in_, extra_reads=(), extra_writes=(), **kw):
        return self.add(eng, lambda e: e.dma_start(out=out, in_=in_, **kw),
                        reads=[in_] + list(extra_reads), writes=[out] + list(extra_writes), dma=True)

    def mm(self, out, lhsT, rhs, start=True, stop=True, **kw):
        return self.add('pe', lambda e: e.matmul(out, lhsT, rhs, start=start, stop=stop, **kw),
                        reads=[lhsT, rhs], writes=[out])

    def tr(self, out, in_, ident):
        return self.add('pe', lambda e: e.transpose(out, in_, ident), reads=[in_, ident], writes=[out])

    def act(self, out, in_, func, bias=None, scale=None, accum_out=None, eng='act'):
        reads = [in_]
        kw = {}
        if bias is not None:
            kw['bias'] = bias
            if not isinstance(bias, (int, float)):
                reads.append(bias)
        if scale is not None:
            kw['scale'] = scale
            if not isinstance(scale, (int, float)):
                reads.append(scale)
        writes = [out]
        if accum_out is not None:
            kw['accum_out'] = accum_out
            writes.append(accum_out)
        return self.add(eng, lambda e: e.activation(out, in_, func, **kw), reads=reads, writes=writes)

    def tt(self, out, in0, in1, op, eng='dve'):
        return self.add(eng, lambda e: e.tensor_tensor(out, in0, in1, op), reads=[in0, in1], writes=[out])

    def ts(self, out, in0, s1, s2, op0, op1=None, eng='dve', accum_out=None):
        reads = [in0]
        for s in (s1, s2):
            if s is not None and not isinstance(s, (int, float)):
                reads.append(s)
        writes = [out]
        kw = {}
        if accum_out is not None:
            kw['accum_out'] = accum_out
            writes.append(accum_out)
        if op1 is None:
            return self.add(eng, lambda e: e.tensor_scalar(out, in0, s1, None, op0, **kw), reads=reads, writes=writes)
        return self.add(eng, lambda e: e.tensor_scalar(out, in0, s1, s2, op0, op1, **kw), reads=reads, writes=writes)

    def stt(self, out, in0, scalar, in1, op0, op1, eng='dve'):
        reads = [in0, in1]
        if not isinstance(scalar, (int, float)):
            reads.append(scalar)
        return self.add(eng, lambda e: e.scalar_tensor_tensor(out, in0, scalar, in1, op0, op1), reads=reads, writes=[out])

    def copy(self, out, in_, eng='dve'):
        if eng == 'act':
            return self.add('act', lambda e: e.copy(out, in_), reads=[in_], writes=[out])
        return self.add(eng, lambda e: e.tensor_copy(out, in_), reads=[in_], writes=[out])

    def memset(self, ap, val, eng='dve'):
        return self.add(eng, lambda e: e.memset(ap, val), writes=[ap])


from concourse.bass_utils import run_bass_kernel_spmd

S = 2048
DM = 1024
NIN = 3208
NTB = 16
NGR = 4
GW = 512
OFF = dict(qa=0, ka=384, va=768, qb=1152, kb=1536, vb=1920, ob=2304, ib=2688, fb=2692, uc=2696, vc=2952)
NBLK = 64
WROW = 6144
EPS = 1e-6
NV = 96
C_ID = 0
C_ONE = 128
C_TRIS = 256
C_TRIC = 384
C_MSB = 512
C_MML = 512 + 2048
C_END = 512 + 4096
CX_IOTAP = 0
CX_IOTAJ = 1
CX_THR = 65
CX_SEL4 = 81
CX_SELR = 81 + 512
CX_END = 81 + 512 + 96


def make_consts():
    c = np.zeros((128, C_END), np.float32)
    c[:, C_ID:C_ID + 128] = np.eye(128)
    c[:, C_ONE:C_ONE + 128] = 1.0
    j = np.arange(128)[:, None]
    s = np.arange(128)[None, :]
    c[:, C_TRIS:C_TRIS + 128] = (j >= s)
    c[:, C_TRIC:C_TRIC + 128] = (j < s)
    for jj in range(4):
        m_sb = np.zeros((128, 4, 128), np.float32)
        m_ml = np.zeros((128, 4, 128), np.float32)
        for n in range(4):
            if n > jj:
                m_sb[:, n, :] = 1.0
                m_ml[:, n, :] = 1.0
            elif n == jj:
                m_sb[:, n, :] = (j < s)
                m_ml[:, n, :] = (j <= s)
        c[:, C_MSB + jj * 512:C_MSB + (jj + 1) * 512] = m_sb.reshape(128, 512)
        c[:, C_MML + jj * 512:C_MML + (jj + 1) * 512] = m_ml.reshape(128, 512)
    cx = np.zeros((128, CX_END), np.float32)
    cx[:, CX_IOTAP] = np.arange(128)
    cx[:, CX_IOTAJ:CX_IOTAJ + 64] = np.arange(64)[None, :]
    cx[:, CX_THR:CX_THR + 16] = (np.arange(16) * 128)[None, :]
    for h in range(4):
        cx[h, CX_SEL4 + h * 128:CX_SEL4 + (h + 1) * 128] = 1.0
    cx[96, CX_SELR:CX_SELR + 96] = 1.0
    return c, cx


class PSM:
    def __init__(self, banks):
        self.banks = banks
        self.pinned = set()
        self.i = 0

    def _next(self):
        for _ in range(16):
            b = self.i
            self.i = (self.i + 1) % len(self.banks)
            if b not in self.pinned:
                return b
        raise RuntimeError("no psum bank")

    def get(self):
        return self.banks[self._next()]

    def pin(self):
        b = self._next()
        self.pinned.add(b)
        return self.banks[b]

    def unpin(self, bank):
        self.pinned.discard(self.banks.index(bank))


def run_pipeline(groups):
    timeline = {}
    t0 = 0
    for gi, (n_r, fn, ov) in enumerate(groups):
        for r in range(n_r):
            timeline.setdefault(t0 + r, []).append((gi, r))
        t0 = t0 + n_r - ov
    for t in sorted(timeline):
        for (gi, r) in timeline[t]:
            groups[gi][1](r)


def bcast(ap, dims):
    p = list(ap.ap)[0]
    return bass.AP(ap.tensor, ap.offset, [[p[0], p[1]]] + [list(d) for d in dims])


SKIPMIX = False


def build(NSEQ=4, NLAYER=2, dbg=None, stop=None):
    nc = bass.Bass("TRN2", target_bir_lowering=False)
    R = Rec(nc)
    dt = nc.dram_tensor
    x_d = dt("x", [NSEQ, S, DM], F32, kind="ExternalInput")
    p_d = dt("p", [2, NSEQ, S, 256], F32, kind="ExternalInput")
    win_d = dt("w_in", [2, DM, NIN], F32, kind="ExternalInput")
    wout_d = dt("w_out", [2, DM, DM], F32, kind="ExternalInput")
    pgw_d = dt("ple_gate_w", [2, DM, DM], F32, kind="ExternalInput")
    ppw_d = dt("ple_proj_w", [2, 256, DM], F32, kind="ExternalInput")
    wall_d = dt("wall_src", [2 * 32 * 128, WROW], F32, kind="ExternalInput")
    rw_d = dt("rw", [2, DM, 36], F32, kind="ExternalInput")
    rb_d = dt("rb", [2, 36], F32, kind="ExternalInput")
    vec_d = dt("vec", [2, 128, NV], F32, kind="ExternalInput")
    sguw_d = dt("sgu_w", [2, 4, 128, 128], F32, kind="ExternalInput")
    sgub_d = dt("sgu_b", [2, 4, 128], F32, kind="ExternalInput")
    sguln_d = dt("sgu_ln", [2, 2, 256], F32, kind="ExternalInput")
    g2row_d = dt("g2row", [2, DM], F32, kind="ExternalInput")
    gfrow_d = dt("gfrow", [1, DM], F32, kind="ExternalInput")
    c_d = dt("consts", [128, C_END], F32, kind="ExternalInput")
    cx_d = dt("constsx", [128, CX_END], F32, kind="ExternalInput")
    out_d = dt("out", [NSEQ, S, DM], F32, kind="ExternalOutput")
    Xs = dt("Xs", [NBLK * 128, DM], BF16, kind="Internal")
    Yd = dt("Yd", [NBLK * 128, DM], BF16, kind="Internal")
    Wall = dt("Wall", [2 * 32 * 128, WROW], BF16, kind="Internal")
    dbg_out = {}

    st = contextlib.ExitStack()
    with st:
        def sb(name, shape, dtp):
            return st.enter_context(nc.sbuf_tensor(name, shape, dtp))
        hT = sb("hT", [128, 8, S], F32)
        hnT = sb("hnT", [128, 8, S], BF16)
        NSLAB = 4
        slabs = [sb(f"slab{i}", [128, 4096], BF16) for i in range(NSLAB)]
        cb = sb("cb", [128, C_END], BF16)
        cf = sb("cf", [128, 256], F32)
        cx = sb("cx", [128, CX_END], F32)
        vecs = [sb(f"vec{l}", [128, NV], F32) for l in range(2)]
        rstd_bc = sb("rstd_bc", [128, S], F32)
        tmpA = [sb(f"tmpA{i}", [128, 512], F32) for i in range(3)]
        epsc = sb("epsc", [128, 1], F32)
        AR_BYTES = 51 * 1024
        arena = sb("arena", [128, AR_BYTES // 4], F32)
        arena_b = arena.bitcast(BF16)
        arena_i = arena.bitcast(I32)
        banks = [st.enter_context(nc.psum_tensor(f"ps{i}", [128, 512], F32)) for i in range(8)]
        PS = PSM(banks)
        print("sbuf remaining", nc.sbuf_bytes_remaining)

        identf = cf[:, 0:128]
        onesf = cf[:, 128:256]
        identb = cb[:, C_ID:C_ID + 128]
        onesb = cb[:, C_ONE:C_ONE + 128]
        trisb = cb[:, C_TRIS:C_TRIS + 128]
        tricb = cb[:, C_TRIC:C_TRIC + 128]

        class Arena:
            def __init__(self):
                self.off = 0

            def reset(self):
                self.off = 0

            def alloc(self, n, dtp):
                sz = 4 if dtp in (F32, I32) else 2
                nbytes = (n * sz + 31) // 32 * 32
                assert self.off + nbytes <= AR_BYTES, (self.off, nbytes)
                start = self.off // sz
                self.off += nbytes
                h = arena if dtp == F32 else (arena_i if dtp == I32 else arena_b)
                return h[:, start:start + n]
        AR = Arena()

        slab_i = [0]

        def next_slab():
            s = slabs[slab_i[0] % NSLAB]
            slab_i[0] += 1
            return s

        def dump(name, ap, shape, dtp=F32):
            if dbg is None or name not in dbg:
                return
            d = dt("dbg_" + name, list(shape), dtp, kind="ExternalOutput")
            dbg_out[name] = d
            R.dma('sp', d.ap(), ap)

        flip = [0]
        _breg = {}

        def gather_w(e, o, ix):
            reg = e.to_reg(2 * 32 * 128 - 1)
            inst = e.indirect_dma_start(out=o, out_offset=None, in_=Wall.ap(),
                                        in_offset=bass.IndirectOffsetOnAxis(ap=ix, axis=0),
                                        bounds_check=reg, oob_is_err=False)
            e.free_register(reg)
            return inst

        def pe_warm(n=24):
            bank = PS.get()
            for _ in range(n):
                R.mm(bank[:, :], onesb, cb[:, C_MSB:C_MSB + 512], start=True, stop=True)

        def evac(out, in_, scale=None):
            flip[0] ^= 1
            if scale is not None:
                R.act(out, in_, AF.Copy, scale=scale)
            elif flip[0]:
                R.act(out, in_, AF.Copy)
            else:
                R.copy(out, in_)

        R.dma('pool', cb[:], c_d.ap())
        R.dma('sp', cf[:], c_d[:, 0:256])
        R.dma('sp', cx[:], cx_d.ap())
        for l in range(2):
            R.dma('sp', vecs[l][:], vec_d[l])
        R.memset(epsc[:], EPS)

        wall_jobs = list(range(NLAYER * 32)) if (stop is None or stop in ('moe', 'ple', 'all', 'route')) else []

        def wall_tick(n=1):
            for _ in range(n):
                if not wall_jobs:
                    return
                i = wall_jobs.pop(0)
                R.dma('pool', Wall[i * 128:(i + 1) * 128, :], wall_d[i * 128:(i + 1) * 128, :], max_dma_last_dim=4096)
        if stop is None or stop in ('moe', 'ple', 'all', 'route'):
            AR.reset()
            zt = AR.alloc(4096, BF16)
            R.memset(zt, 0.0)
            for i in range(NBLK * 128 // 512):
                R.dma('sp', Xs[i * 512:(i + 1) * 512, :].rearrange("(a p) d -> p a d", p=128),
                      zt.rearrange("p (a d) -> p a d", d=1024), extra_writes=[("XsV", 0, 1, 0, 1 << 30)])

        def norm_fm(gcol, l, src=None):
            for g in range(NGR):
                ps = PS.get()
                for k in range(8):
                    sq = tmpA[k % 2]
                    R.act(sq[:], hT[:, k, g * GW:(g + 1) * GW], AF.Square)
                    R.mm(ps[:, :], onesf, sq[:], start=(k == 0), stop=(k == 7))
                t = tmpA[2]
                R.act(t[:], ps[:, :], AF.Ln, bias=epsc[:, 0:1], scale=1.0 / DM)
                R.act(rstd_bc[:, g * GW:(g + 1) * GW], t[:], AF.Exp, scale=-0.5)
                for k in range(8):
                    R.stt(hnT[:, k, g * GW:(g + 1) * GW], hT[:, k, g * GW:(g + 1) * GW],
                          vecs[l][:, gcol + k:gcol + k + 1], rstd_bc[:, g * GW:(g + 1) * GW], ALU.mult, ALU.mult)

        def load_in_cols(slab, dst_col, l, col0, ncols, width):
            v = slab[:, 0:8 * width].rearrange("p (k n) -> p k n", n=width)
            src = win_d[l].rearrange("(k p) n -> p k n", p=128)[:, :, col0:col0 + ncols]
            R.dma('pool', v[:, :, dst_col:dst_col + ncols], src)
            return v

        def proj_fm(wv, c0, M, out_fn):
            for g in range(NGR):
                ps = PS.get()
                for k in range(8):
                    R.mm(ps[0:M, :], wv[:, k, c0:c0 + M], hnT[:, k, g * GW:(g + 1) * GW], start=(k == 0), stop=(k == 7))
                out_fn(g, ps)

        def proj_tm(wv, c0, N, out_fn):
            per = 4 if N <= 128 else 1
            for tb0 in range(0, NTB, per):
                ps = PS.get()
                for i in range(per):
                    tb = tb0 + i
                    for k in range(8):
                        R.mm(ps[:, i * N:(i + 1) * N], hnT[:, k, tb * 128:(tb + 1) * 128], wv[:, k, c0:c0 + N],
                             start=(k == 0), stop=(k == 7))
                out_fn(tb0, per, ps)

        def outproj_add(wv, kparts, rhs_fn):
            nk = len(kparts)
            for fo in range(8):
                for g in range(NGR):
                    ps = PS.get()
                    for j, K in enumerate(kparts):
                        R.mm(ps[:, :], wv[0:K, j, fo * 128:(fo + 1) * 128], rhs_fn(j, g), start=(j == 0), stop=(j == nk - 1))
                    R.tt(hT[:, fo, g * GW:(g + 1) * GW], hT[:, fo, g * GW:(g + 1) * GW], ps[:, :], ALU.add)

        for seq in range(NSEQ):
            AR.reset()
            xts = [AR.alloc(1024, F32) for _ in range(2)]
            for tb in range(NTB):
                xt = xts[tb % 2]
                R.dma('sp', xt, x_d[seq, tb * 128:(tb + 1) * 128, :])
                for half in range(2):
                    ps = PS.get()
                    for i in range(4):
                        k = half * 4 + i
                        R.tr(ps[:, i * 128:(i + 1) * 128], xt[:, k * 128:(k + 1) * 128], identf)
                    evac(hT[:, half * 4:half * 4 + 4, tb * 128:(tb + 1) * 128],
                         ps[:, :].rearrange("p (a b) -> p a b", b=128))

            for l in range(NLAYER):
                V = vecs[l]
                norm_fm(0, l)
                dump(f"hn{l}", hnT[:, 0, 0:512], (128, 512), BF16)
                if stop == 'norm1':
                    break
                if not SKIPMIX:
                    AR.reset()
                    yaT = AR.alloc(3 * S, BF16).rearrange("p (j t) -> p j t", t=S)
                    qT = AR.alloc(S, BF16)
                    kT = AR.alloc(S, BF16)
                    vtm = AR.alloc(NTB * 128, BF16).rearrange("p (b c) -> p b c", c=128)
                    e_t = [[AR.alloc(512, BF16) for _ in range(2)] for _ in range(2)]
                    sp_t = [[AR.alloc(512, BF16) for _ in range(3)] for _ in range(2)]
                    p_t = [[AR.alloc(512, BF16) for _ in range(2)] for _ in range(2)]
                    a_t = [[AR.alloc(512, BF16) for _ in range(2)] for _ in range(2)]
                    wA = [None, None, None]

                    def loadA(j):
                        sl = next_slab()
                        load_in_cols(sl, 0, l, OFF['qa'] + j * 128, 128, 384)
                        load_in_cols(sl, 128, l, OFF['ka'] + j * 128, 128, 384)
                        wA[j] = load_in_cols(sl, 256, l, OFF['va'] + j * 128, 128, 384)
                    loadA(0)
                    for j in range(3):
                        wv = wA[j]
                        if j + 1 < 3:
                            loadA(j + 1)
                        proj_fm(wv, 0, 128, lambda g, ps: R.act(qT[:, g * GW:(g + 1) * GW], ps[:, :], AF.Copy, scale=0.125))
                        proj_fm(wv, 128, 128, lambda g, ps: R.copy(kT[:, g * GW:(g + 1) * GW], ps[:, :]))
                        proj_tm(wv, 256, 128, lambda tb0, per, ps: evac(
                            vtm[:, tb0:tb0 + per, :], ps[:, :].rearrange("p (a b) -> p a b", b=128)))
                        groupsA = []
                        for G in range(NGR):
                            def mk(G=G, j=j):
                                nb = 4 * G + 4
                                its = list(range(nb - 1, -1, -1))
                                n = len(its)
                                stt_ = dict(accs=None, crs=None, psz={})

                                def c0f(i):
                                    return max(0, its[i] - 4 * G) * 128

                                def sZ(i):
                                    b = its[i]
                                    c0 = c0f(i)
                                    for hp in range(2):
                                        pl, ph = hp * 64, hp * 64 + 64
                                        psz = PS.get()
                                        stt_['psz'][(i, hp)] = psz
                                        R.mm(psz[:, c0:512], kT[pl:ph, b * 128:(b + 1) * 128], qT[pl:ph, G * GW + c0:(G + 1) * GW])

                                def sEL(i):
                                    b = its[i]
                                    c0 = c0f(i)
                                    for hp in range(2):
                                        et = e_t[hp][i % 2][:, c0:512]
                                        R.act(et, stt_['psz'].pop((i, hp))[:, c0:512], AF.Exp)
                                        if b >= 4 * G:
                                            jj = b - 4 * G
                                            R.tt(et, et, cb[:, C_MSB + jj * 512 + c0:C_MSB + (jj + 1) * 512], ALU.mult)
                                    for hp in range(2):
                                        et, spt = e_t[hp][i % 2][:, c0:512], sp_t[hp][i % 3][:, c0:512]
                                        R.act(spt, et, AF.Ln, bias=1.0)

                                def sT(i):
                                    if stt_['accs'] is None:
                                        stt_['accs'] = [PS.pin(), PS.pin()]
                                        stt_['crs'] = [PS.pin(), PS.pin()]
                                    c0 = c0f(i)
                                    for hp in range(2):
                                        R.mm(stt_['crs'][hp][:, c0:512], trisb, sp_t[hp][i % 3][:, c0:512], start=(i == 0), stop=False)

                                def sX(i):
                                    c0 = c0f(i)
                                    for hp in range(2):
                                        R.act(p_t[hp][i % 2][:, c0:512], stt_['crs'][hp][:, c0:512], AF.Exp, scale=-1.0)
                                        R.tt(a_t[hp][i % 2][:, c0:512], e_t[hp][i % 2][:, c0:512], p_t[hp][i % 2][:, c0:512], ALU.mult)

                                def sC(i):
                                    c0 = c0f(i)
                                    if i < n - 1:
                                        for hp in range(2):
                                            R.mm(stt_['crs'][hp][:, c0:512], tricb, sp_t[hp][i % 3][:, c0:512], start=False, stop=False)

                                def sA(i):
                                    b = its[i]
                                    c0 = c0f(i)
                                    for hp in range(2):
                                        pl, ph = hp * 64, hp * 64 + 64
                                        R.mm(stt_['accs'][hp][pl:ph, c0:512], vtm[:, b, pl:ph], a_t[hp][i % 2][:, c0:512],
                                             start=(i == 0), stop=(i == n - 1))

                                def rnd(r):
                                    wall_tick(1)
                                    if 0 <= r - 3 < n:
                                        sC(r - 3)
                                    if 0 <= r - 2 < n:
                                        sT(r - 2)
                                    if r < n:
                                        sZ(r)
                                    if 0 <= r - 3 < n:
                                        sA(r - 3)
                                    if 0 <= r - 1 < n:
                                        sEL(r - 1)
                                    if 0 <= r - 2 < n:
                                        sX(r - 2)
                                    if r == n + 2:
                                        for hp in range(2):
                                            pl, ph = hp * 64, hp * 64 + 64
                                            evac(yaT[pl:ph, j, G * GW:(G + 1) * GW], stt_['accs'][hp][pl:ph, :])
                                        for bk in stt_['accs'] + stt_['crs']:
                                            PS.unpin(bk)
                                return (n + 3, rnd, 2)
                            groupsA.append(mk())
                        pe_warm()
                        run_pipeline(groupsA)
                    dump(f"yaT{l}", yaT[:, :, :], (128, 3, S), BF16)
                    if stop == 'attnA':
                        break
                    slo = next_slab()
                    wo = slo[:, 0:3 * 1024].rearrange("p (j n) -> p j n", n=1024)
                    R.dma('pool', wo, wout_d[l, 0:384, :].rearrange("(j p) n -> p j n", p=128))
                    for g in range(NGR):
                        ps = PS.get()
                        for j in range(3):
                            sq = tmpA[j % 2]
                            R.act(sq[:], yaT[:, j, g * GW:(g + 1) * GW], AF.Square)
                            R.mm(ps[:, :], onesf, sq[:], start=(j == 0), stop=(j == 2))
                        t = tmpA[2]
                        R.act(t[:], ps[:, :], AF.Ln, bias=epsc[:, 0:1], scale=1.0 / 384)
                        R.act(rstd_bc[:, g * GW:(g + 1) * GW], t[:], AF.Exp, scale=-0.5)
                        for j in range(3):
                            R.stt(yaT[:, j, g * GW:(g + 1) * GW], yaT[:, j, g * GW:(g + 1) * GW],
                                  V[:, 32 + j:33 + j], rstd_bc[:, g * GW:(g + 1) * GW], ALU.mult, ALU.mult)
                    dump(f"yanT{l}", yaT[:, :, :], (128, 3, S), BF16)
                    outproj_add(wo, [128, 128, 128], lambda j, g: yaT[:, j, g * GW:(g + 1) * GW])
                    if stop == 'outA':
                        break

                    AR.reset()
                    t1 = AR.alloc(S, F32)
                    AR.off -= S * 4
                    ybT = AR.alloc(4 * S, BF16).rearrange("p (h t) -> p h t", t=S)
                    Bf = rstd_bc
                    utm = AR.alloc(NTB * 4, F32).rearrange("p (b h) -> p b h", h=4)
                    nfb = AR.alloc(8, F32)[0:4, 0:1]
                    qb = AR.alloc(S, BF16)
                    kb = AR.alloc(S, BF16)
                    sgo = AR.alloc(S, BF16)
                    vbt = AR.alloc(NTB * 97, BF16).rearrange("p (b c) -> p b c", c=97)
                    R.memset(vbt[:, :, 96:97], 1.0)
                    numS = AR.alloc(GW, F32)
                    raw = [AR.alloc(GW + 4, BF16) for _ in range(3)]
                    dgw = AR.alloc(8 * 96, BF16).rearrange("p (m c) -> p m c", c=96)
                    d_t = [AR.alloc(512, BF16) for _ in range(3)]
                    w_t = [AR.alloc(512, BF16) for _ in range(2)]
                    slg = next_slab()
                    wg_ = load_in_cols(slg, 0, l, OFF['ib'], 8, 8)
                    R.ts(nfb, V[0:4, 82:83], -1.0, None, ALU.mult)
                    proj_fm(wg_, 4, 4, lambda g, ps: R.act(t1[0:4, g * GW:(g + 1) * GW], ps[0:4, :], AF.Exp, bias=nfb, scale=-1.0))
                    R.act(t1[0:4, :], t1[0:4, :], AF.Ln, bias=1.0)
                    R.add('dve', lambda e: e.tensor_tensor_scan(Bf[0:4, :], bcast(onesf[0:4, 0:1], [[0, S]]), t1[0:4, :], 0.0,
                                                                 ALU.mult, ALU.subtract),
                          reads=[onesf[0:4, 0:1], t1[0:4, :]], writes=[Bf[0:4, :]])
                    LNS = float(np.log(96.0 ** -0.5))

                    def u_out(g, ps):
                        uu = tmpA[0][0:4, :]
                        R.stt(uu, ps[0:4, :], V[0:4, 81:82], Bf[0:4, g * GW:(g + 1) * GW], ALU.add, ALU.subtract)
                        R.ts(uu, uu, LNS, None, ALU.add)
                        ps2 = PS.get()
                        for i in range(4):
                            R.tr(ps2[:, i * 4:(i + 1) * 4], uu[:, i * 128:(i + 1) * 128], identf[0:4, 0:4])
                        R.copy(utm[:, 4 * g:4 * g + 4, :], ps2[:, 0:16].rearrange("p (a b) -> p a b", b=4))
                    proj_fm(wg_, 0, 4, u_out)
                    dump(f"Bf{l}", Bf[0:4, :], (4, S))
                    dump(f"utm{l}", utm[:, :, :], (128, NTB, 4))
                    wB = [None] * 4

                    def loadB(h):
                        sl = next_slab()
                        load_in_cols(sl, 0, l, OFF['qb'] + h * 96, 96, 384)
                        load_in_cols(sl, 96, l, OFF['kb'] + h * 96, 96, 384)
                        load_in_cols(sl, 192, l, OFF['vb'] + h * 96, 96, 384)
                        wB[h] = load_in_cols(sl, 288, l, OFF['ob'] + h * 96, 96, 384)
                    loadB(0)
                    for h in range(4):
                        wv = wB[h]
                        if h + 1 < 4:
                            loadB(h + 1)
                        for qk in range(2):
                            cw = 41 + qk * 16 + h * 4
                            for tap in range(4):
                                R.ts(dgw[0:96, qk * 4 + tap, :], identf[0:96, 0:96], V[0:96, cw + tap:cw + tap + 1], None, ALU.mult)
                        for qk in range(2):
                            cbias = 73 + qk * 4 + h
                            dst = qb if qk == 0 else kb
                            R.memset(raw[0][0:96, 1:4], 0.0)

                            def conv_out(g, ps, qk=qk, cbias=cbias, dst=dst):
                                rw_ = raw[g % 3]
                                R.act(rw_[0:96, 4:4 + GW], ps[0:96, :], AF.Copy)
                                if g + 1 < NGR:
                                    R.copy(raw[(g + 1) % 3][0:96, 1:4], rw_[0:96, GW + 1:GW + 4])
                                pc = PS.get()
                                for tap in range(4):
                                    R.mm(pc[0:96, :], dgw[0:96, qk * 4 + tap, :], rw_[0:96, 1 + tap:1 + tap + GW],
                                         start=(tap == 0), stop=(tap == 3))
                                R.act(dst[0:96, g * GW:(g + 1) * GW], pc[0:96, :], AF.Silu, bias=V[0:96, cbias:cbias + 1])
                            proj_fm(wv, qk * 96, 96, conv_out)
                        if h == 0:
                            dump(f"qb0_{l}", qb[0:96, :], (96, S), BF16)
                        proj_tm(wv, 192, 96, lambda tb0, per, ps: evac(
                            vbt[:, tb0:tb0 + per, 0:96], ps[:, 0:per * 96].rearrange("p (a b) -> p a b", b=96)))
                        proj_fm(wv, 288, 96, lambda g, ps: R.act(sgo[0:96, g * GW:(g + 1) * GW], ps[0:96, :], AF.Sigmoid))
                        groupsB = []
                        pend_epi = []
                        for G in range(NGR):
                            def mkB(G=G, h=h):
                                nb = 4 * G + 4
                                n = nb
                                stb = dict(psB=None, num=None, den=None, pss={})

                                def sS(i):
                                    b = i
                                    if stb['psB'] is None:
                                        stb['psB'] = PS.pin()
                                        R.mm(stb['psB'][:, :], cx[0:4, CX_SEL4 + h * 128:CX_SEL4 + (h + 1) * 128], Bf[0:4, G * GW:(G + 1) * GW])
                                    pss = PS.get()
                                    stb['pss'][i] = pss
                                    c0 = max(0, b - 4 * G) * 128
                                    R.mm(pss[:, c0:512], kb[0:96, b * 128:(b + 1) * 128], qb[0:96, G * GW + c0:(G + 1) * GW])
                                    dt_ = d_t[i % 3][:, c0:512]
                                    R.act(dt_, stb['psB'][:, c0:512], AF.Exp, bias=utm[:, b, h:h + 1])
                                    if b >= 4 * G:
                                        jj = b - 4 * G
                                        R.tt(dt_, dt_, cb[:, C_MML + jj * 512 + c0:C_MML + (jj + 1) * 512], ALU.mult)
                                    if i == n - 1:
                                        PS.unpin(stb['psB'])

                                def sW(i):
                                    c0 = max(0, i - 4 * G) * 128
                                    R.tt(w_t[i % 2][:, c0:512], stb['pss'].pop(i)[:, c0:512], d_t[i % 3][:, c0:512], ALU.mult)

                                def sN(i):
                                    b = i
                                    c0 = max(0, i - 4 * G) * 128
                                    if stb['num'] is None:
                                        stb['num'] = PS.pin()
                                    R.mm(stb['num'][0:97, c0:512], vbt[:, b, :], w_t[i % 2][:, c0:512], start=(i == 0), stop=(i == n - 1))

                                def epi_steps():
                                    num = stb['num']
                                    dn, hh, sq = tmpA[0], tmpA[1], tmpA[2]
                                    box = {}

                                    def s0():
                                        R.act(numS[0:97, :], num[0:97, :], AF.Copy)
                                        PS.unpin(num)

                                    def s1():
                                        box['den'] = PS.get()
                                        R.mm(box['den'][0:96, :], cx[0:97, CX_SELR:CX_SELR + 96], numS[0:97, :])

                                    def s5():
                                        R.tt(hh[0:96, :], numS[0:96, :], dn[0:96, :], ALU.mult)
                                        if h == 0 and G == 0:
                                            dump(f"hb00_{l}", hh[0:96, :], (96, 512))

                                    def s7():
                                        box['ps'] = PS.get()
                                        R.mm(box['ps'][0:96, :], onesf[0:96, 0:96], sq[0:96, :])
                                    return [
                                        s0,
                                        s1,
                                        lambda: R.act(dn[0:96, :], box['den'][0:96, :], AF.Abs),
                                        lambda: R.ts(dn[0:96, :], dn[0:96, :], 1.0, None, ALU.max),
                                        lambda: R.act(dn[0:96, :], dn[0:96, :], AF.Ln),
                                        lambda: R.act(dn[0:96, :], dn[0:96, :], AF.Exp, scale=-1.0),
                                        s5,
                                        lambda: R.act(sq[0:96, :], hh[0:96, :], AF.Square),
                                        s7,
                                        lambda: R.act(sq[0:96, :], box['ps'][0:96, :], AF.Ln, bias=epsc[0:96, 0:1], scale=1.0 / 96),
                                        lambda: R.act(sq[0:96, :], sq[0:96, :], AF.Exp, scale=-0.5),
                                        lambda: R.stt(hh[0:96, :], hh[0:96, :], V[0:96, 35 + h:36 + h], sq[0:96, :], ALU.mult, ALU.mult),
                                        lambda: R.tt(ybT[0:96, h, G * GW:(G + 1) * GW], hh[0:96, :], sgo[0:96, G * GW:(G + 1) * GW], ALU.mult),
                                    ]

                                def rnd(r):
                                    if 0 <= r - 2 < n:
                                        sN(r - 2)
                                    if r < n:
                                        sS(r)
                                    if 0 <= r - 1 < n:
                                        sW(r - 1)
                                    if r >= 2 and pend_epi:
                                        pend_epi.pop(0)()
                                    if r == n + 1:
                                        while pend_epi:
                                            pend_epi.pop(0)()
                                        pend_epi.extend(epi_steps())
                                return (n + 2, rnd, 2)
                            groupsB.append(mkB())
                        pe_warm()
                        run_pipeline(groupsB)
                        while pend_epi:
                            pend_epi.pop(0)()
                    dump(f"ybT{l}", ybT[0:96, :, :], (96, 4, S), BF16)
                    if stop == 'mlstm':
                        break
                    slo = next_slab()
                    wo = slo[:, 0:4 * 1024].rearrange("p (j n) -> p j n", n=1024)
                    R.dma('pool', wo[0:96, :, :], wout_d[l, 384:768, :].rearrange("(j p) n -> p j n", p=96))
                    outproj_add(wo, [96, 96, 96, 96], lambda j, g: ybT[0:96, j, g * GW:(g + 1) * GW])

                    AR.reset()
                    ycT = AR.alloc(2 * S, BF16).rearrange("p (j t) -> p j t", t=S)
                    uT = AR.alloc(S, BF16)
                    vtmC = AR.alloc(NTB * 128, F32).rearrange("p (b c) -> p b c", c=128)
                    vnb = AR.alloc(NTB * 128, BF16).rearrange("p (b c) -> p b c", c=128)
                    wsf = AR.alloc(4 * 128, F32).rearrange("p (g s) -> p g s", s=128)
                    wsT = AR.alloc(4 * 128, BF16).rearrange("p (g s) -> p g s", s=128)
                    bsT = AR.alloc(512, F32)
                    lng = AR.alloc(128, F32)
                    lnb = AR.alloc(128, F32)
                    st8 = AR.alloc(64, F32)
                    st8b = AR.alloc(64, F32)
                    gx = [AR.alloc(512, F32) for _ in range(3)]
                    R.dma('sp', wsf, sguw_d[l].rearrange("g t s -> t g s"))
                    R.memset(wsf[0:64, :, 64:128], 0.0)
                    ps = PS.get()
                    for gq in range(4):
                        R.tr(ps[:, gq * 128:(gq + 1) * 128], wsf[:, gq, :], identf)
                    R.copy(wsT, ps[:, :].rearrange("p (g s) -> p g s", s=128))

                    def gelu_chain(dst, src_ps, np_, n):
                        R.act(dst, src_ps, AF.Gelu_apprx_tanh)

                    for j in range(2):
                        slc = next_slab()
                        load_in_cols(slc, 0, l, OFF['uc'] + j * 128, 128, 256)
                        wv = load_in_cols(slc, 128, l, OFF['vc'] + j * 128, 128, 256)
                        R.dma('sp', lng, bass.AP(sguln_d, (l * 2 + 0) * 256 + j * 128, [[0, 128], [1, 128]]))
                        R.dma('sp', lnb, bass.AP(sguln_d, (l * 2 + 1) * 256 + j * 128, [[0, 128], [1, 128]]))
                        for gi in range(2):
                            for rep in range(4):
                                R.dma('sp', bsT[gi * 64:(gi + 1) * 64, rep * 128:(rep + 1) * 128],
                                      bass.AP(sgub_d, (l * 4 + 2 * j + gi) * 128, [[0, 64], [1, 128]]))
                        proj_fm(wv, 0, 128, lambda g, ps: gelu_chain(uT[:, g * GW:(g + 1) * GW], ps[:, :], 128, 512))

                        def v_out(tb0, per, ps):
                            gelu_chain(vtmC[:, tb0:tb0 + per, :].rearrange("p a b -> p (a b)"), ps[:, :], 128, 512)
                        proj_tm(wv, 128, 128, v_out)
                        for tb0 in range(0, NTB, 4):
                            vv = vtmC[:, tb0:tb0 + 4, :].rearrange("p a (g c) -> p (a g) c", c=64)
                            mu = st8[:, 0:8]
                            R.add('dve', lambda e, o=mu, i=vv: e.tensor_reduce(o, i, AX.X, ALU.add), reads=[vv], writes=[mu])
                            R.ts(mu, mu, 1.0 / 64, None, ALU.mult)
                            cen = gx[0].rearrange("p (a c) -> p a c", c=64)
                            R.tt(cen, vv, bcast(mu, [[1, 8], [0, 64]]), ALU.subtract)
                            sqv = gx[1].rearrange("p (a c) -> p a c", c=64)
                            R.act(sqv, cen, AF.Square)
                            var = st8b[:, 0:8]
                            R.add('dve', lambda e, o=var, i=sqv: e.tensor_reduce(o, i, AX.X, ALU.add), reads=[sqv], writes=[var])
                            R.act(var, var, AF.Ln, bias=epsc[:, 0:1], scale=1.0 / 64)
                            R.act(var, var, AF.Exp, scale=-0.5)
                            R.tt(cen, cen, bcast(var, [[1, 8], [0, 64]]), ALU.mult)
                            cen4 = gx[0].rearrange("p (a c) -> p a c", c=128)
                            R.tt(cen4, cen4, bcast(lng, [[0, 4], [1, 128]]), ALU.mult)
                            R.tt(vnb[:, tb0:tb0 + 4, :], cen4, bcast(lnb, [[0, 4], [1, 128]]), ALU.add)
                        for tb0 in range(0, NTB, 4):
                            ps = PS.get()
                            for i in range(4):
                                tb = tb0 + i
                                for gi in range(2):
                                    R.mm(ps[gi * 64:(gi + 1) * 64, i * 128:(i + 1) * 128], vnb[:, tb, gi * 64:(gi + 1) * 64],
                                         wsT[:, 2 * j + gi, :])
                            t = gx[2]
                            R.tt(t, ps[:, :], bsT, ALU.add)
                            R.tt(ycT[:, j, tb0 * 128:(tb0 + 4) * 128], t, uT[:, tb0 * 128:(tb0 + 4) * 128], ALU.mult)
                    dump(f"ycT{l}", ycT[:, :, :], (128, 2, S), BF16)
                    if stop == 'sgu':
                        break
                    ycn = ycT
                    slo = next_slab()
                    wo = slo[:, 0:2 * 1024].rearrange("p (j n) -> p j n", n=1024)
                    R.dma('pool', wo, wout_d[l, 768:1024, :].rearrange("(j p) n -> p j n", p=128))
                    for g in range(NGR):
                        ps = PS.get()
                        for j in range(2):
                            sq = tmpA[j % 2]
                            R.act(sq[:], ycT[:, j, g * GW:(g + 1) * GW], AF.Square)
                            R.mm(ps[:, :], onesf, sq[:], start=(j == 0), stop=(j == 1))
                        t = tmpA[2]
                        R.act(t[:], ps[:, :], AF.Ln, bias=epsc[:, 0:1], scale=1.0 / 256)
                        R.act(t[:], t[:], AF.Exp, scale=-0.5)
                        for j in range(2):
                            R.stt(ycn[:, j, g * GW:(g + 1) * GW], ycT[:, j, g * GW:(g + 1) * GW], V[:, 39 + j:40 + j], t[:],
                                  ALU.mult, ALU.mult)
                    outproj_add(wo, [128, 128], lambda j, g: ycn[:, j, g * GW:(g + 1) * GW])
                    dump(f"hmix{l}", hT[:, :, 0:256], (128, 8, 256))
                    if stop == 'mix':
                        break

                wall_tick(len(wall_jobs))
                slg0, slg1, slp = next_slab(), next_slab(), next_slab()
                wgA = slg0[:, 0:4096].rearrange("p (k n) -> p k n", n=1024)
                wgB = slg1[:, 0:4096].rearrange("p (k n) -> p k n", n=1024)
                wpp = slp[:, 0:2048].rearrange("p (k n) -> p k n", n=1024)
                R.dma('pool', wgA, pgw_d[l, 0:512, :].rearrange("(k p) n -> p k n", p=128))
                R.dma('pool', wgB, pgw_d[l, 512:1024, :].rearrange("(k p) n -> p k n", p=128))
                R.dma('pool', wpp, ppw_d[l].rearrange("(k p) n -> p k n", p=128))

                AR.reset()
                wts = AR.alloc(NTB * 2, F32).rearrange("p (b k) -> p b k", k=2)
                idx = AR.alloc(NTB * 2, I32).rearrange("p (b k) -> p b k", k=2)
                idxw = AR.alloc(NBLK, I32)
                mark = AR.off
                g2bc = AR.alloc(DM, F32)
                rws = AR.alloc(8 * 36, F32).rearrange("p (k n) -> p k n", n=36)
                rbb = AR.alloc(36, F32)
                sel = AR.alloc(NTB * 64, F32).rearrange("p (b k e) -> p b k e", k=2, e=32)
                Lall = AR.alloc(NTB * 36, F32)
                AR.off -= (NTB * 36 * 4 + 31) // 32 * 32
                posl = AR.alloc(512, F32).rearrange("p (b e) -> p b e", e=32)
                run = AR.alloc(512, F32).rearrange("p (b e) -> p b e", e=32)
                sm = AR.alloc(512, F32)
                s12b = AR.alloc(NTB * 32, BF16)
                xn2 = AR.alloc(NTB * DM, BF16).rearrange("p (b d) -> p b d", d=DM)
                R.dma('sp', g2bc, bass.AP(g2row_d, l * DM, [[0, 128], [1, DM]]))
                R.dma('sp', rbb, bass.AP(rb_d, l * 36, [[0, 128], [1, 36]]))
                R.dma('sp', rws, rw_d[l].rearrange("(k p) n -> p k n", p=128))
                for k in range(8):
                    R.ts(rws[:, k, :], rws[:, k, :], V[:, 8 + k:9 + k], None, ALU.mult)
                cnt_ps = PS.pin()
                pos_ps = PS.pin()
                L3 = Lall.rearrange("p (b n) -> p b n", n=36)
                for tb in range(NTB):
                    ssq = sm[:, 480 + 2 * (tb % 4):482 + 2 * (tb % 4)]
                    pst = [PS.get(), PS.get()]
                    for half in range(2):
                        for i in range(4):
                            k = half * 4 + i
                            R.tr(pst[half][:, i * 128:(i + 1) * 128], hT[:, k, tb * 128:(tb + 1) * 128], identf)
                        R.act(tmpA[half][:], pst[half][:, :], AF.Square, accum_out=ssq[:, half:half + 1])
                    rs = sm[:, 496 + (tb % 4):497 + (tb % 4)]
                    R.tt(rs, ssq[:, 0:1], ssq[:, 1:2], ALU.add)
                    R.act(rs, rs, AF.Ln, bias=epsc[:, 0:1], scale=1.0 / DM)
                    R.act(rs, rs, AF.Exp, scale=-0.5)
                    for half in range(2):
                        R.stt(xn2[:, tb, half * 512:(half + 1) * 512], pst[half][:, :], rs, g2bc[:, half * 512:(half + 1) * 512],
                              ALU.mult, ALU.mult)
                    psl = PS.get()
                    for k in range(8):
                        R.mm(psl[:, 0:36], hT[:, k, tb * 128:(tb + 1) * 128], rws[:, k, :], start=(k == 0), stop=(k == 7))
                    R.stt(L3[:, tb, :], psl[:, 0:36], rs, rbb, ALU.mult, ALU.add)
                dump(f"Lall{l}", Lall, (128, NTB * 36))
                if stop == 'route1':
                    break
                gl = L3[:, :, 0:4]
                el4 = bass.AP(Lall.tensor, Lall.offset + 4, [list(list(Lall.ap)[0]), [36, NTB], [8, 4], [1, 8]])
                gmax, gsum, v1, v2 = sm[:, 0:16], sm[:, 16:32], sm[:, 32:48], sm[:, 48:64]
                d21, ex, dn_ = sm[:, 64:80], sm[:, 80:96], sm[:, 96:112]
                goh = sm[:, 128:192].rearrange("p (b g) -> p b g", g=4)
                gex = sm[:, 192:256].rearrange("p (b g) -> p b g", g=4)
                pen = sm[:, 256:320].rearrange("p (b g) -> p b g", g=4)
                R.add('dve', lambda e: e.tensor_reduce(gmax, gl, AX.X, ALU.max), reads=[gl], writes=[gmax])
                R.tt(goh, gl, bcast(gmax, [[1, NTB], [0, 4]]), ALU.is_equal)
                R.tt(gex, gl, bcast(gmax, [[1, NTB], [0, 4]]), ALU.subtract)
                R.act(gex, gex, AF.Exp)
                R.add('dve', lambda e: e.tensor_reduce(gsum, gex, AX.X, ALU.add), reads=[gex], writes=[gsum])
                R.add('dve', lambda e: e.reciprocal(gsum, gsum), reads=[gsum], writes=[gsum])
                R.ts(pen, goh, 1e30, -1e30, ALU.mult, ALU.add)
                em = tmpA[0][:, :]
                em3 = em.rearrange("p (b e) -> p b e", e=32)
                R.tt(em.rearrange("p (b g e) -> p b g e", g=4, e=8), el4, bcast(sm[:, 256:320], [[4, NTB], [1, 4], [0, 8]]), ALU.add)
                R.add('dve', lambda e: e.tensor_reduce(v1, em3, AX.X, ALU.max), reads=[em], writes=[v1])
                R.tt(sel[:, :, 0, :], em3, bcast(v1, [[1, NTB], [0, 32]]), ALU.is_equal)
                em2 = tmpA[1][:, :]
                em23 = em2.rearrange("p (b e) -> p b e", e=32)
                R.stt(em23, sel[:, :, 0, :], -1e30, em3, ALU.mult, ALU.add)
                R.add('dve', lambda e: e.tensor_reduce(v2, em23, AX.X, ALU.max), reads=[em2], writes=[v2])
                R.tt(sel[:, :, 1, :], em23, bcast(v2, [[1, NTB], [0, 32]]), ALU.is_equal)
                R.tt(d21, v2, v1, ALU.subtract)
                R.act(ex, d21, AF.Exp)
                R.ts(dn_, ex, 1.0, None, ALU.add)
                R.add('dve', lambda e: e.reciprocal(dn_, dn_), reads=[dn_], writes=[dn_])
                R.tt(wts[:, :, 0], dn_, gsum, ALU.mult)
                R.tt(wts[:, :, 1], wts[:, :, 0], ex, ALU.mult)
                s12b3 = s12b.rearrange("p (b e) -> p b e", e=32)
                R.tt(s12b3, sel[:, :, 0, :], sel[:, :, 1, :], ALU.add)
                dump(f"sel{l}", sel[:, :, :, :], (128, NTB, 2, 32))
                dump(f"wts{l}", wts[:, :, :], (128, NTB, 2))
                if stop == 'route2':
                    break
                for tb in range(NTB):
                    R.mm(cnt_ps[:, tb * 32:(tb + 1) * 32], onesb, s12b3[:, tb, :])
                    R.mm(pos_ps[:, tb * 32:(tb + 1) * 32], tricb, s12b3[:, tb, :])
                cntb = tmpA[2][:, :].rearrange("p (b e) -> p b e", e=32)
                R.copy(cntb, cnt_ps[:, :].rearrange("p (b e) -> p b e", e=32))
                if stop == 'r3':
                    R.copy(tmpA[0][:, :], cnt_ps[:, :])
                    dump(f"c3_{l}", tmpA[0][:, :], (128, 512))
                    break
                R.memset(run[:, 0, :], 0.0)
                for b in range(NTB - 1):
                    R.tt(run[:, b + 1, :], run[:, b, :], cntb[:, b, :], ALU.add)
                ctot = sm[:, 0:32]
                R.tt(ctot, run[:, NTB - 1, :], cntb[:, NTB - 1, :], ALU.add)
                R.copy(posl, pos_ps[:, :].rearrange("p (b e) -> p b e", e=32))
                PS.unpin(cnt_ps)
                PS.unpin(pos_ps)
                cmpb = tmpA[1][:, 0:512].rearrange("p (e m) -> p e m", m=16)
                R.tt(cmpb, bcast(ctot, [[1, 32], [0, 16]]), bcast(cx[:, CX_THR:CX_THR + 16], [[0, 32], [1, 16]]), ALU.is_gt)
                blk = sm[:, 32:64]
                R.add('dve', lambda e, o=blk, i=cmpb: e.tensor_reduce(o, i, AX.X, ALU.add), reads=[tmpA[1][:, 0:512]], writes=[blk])
                pend = sm[:, 64:96]
                R.add('dve', lambda e: e.tensor_tensor_scan(pend, onesf[:, 0:32], blk, 0.0, ALU.mult, ALU.add),
                      reads=[onesf[:, 0:32], blk], writes=[pend])
                pst128 = sm[:, 96:128]
                R.tt(pst128, pend, blk, ALU.subtract)
                R.ts(pst128, pst128, 128.0, None, ALU.mult)
                R.tt(run, run, bcast(pst128, [[0, NTB], [1, 32]]), ALU.add)
                R.tt(posl, posl, run, ALU.add)
                ej = sm[:, 128:192]
                for jc in range(4):
                    cmpj = tmpA[0][:, 0:512].rearrange("p (j e) -> p j e", e=32)
                    R.tt(cmpj, bcast(cx[:, CX_IOTAJ + jc * 16:CX_IOTAJ + jc * 16 + 16], [[1, 16], [0, 32]]),
                         bcast(pend, [[0, 16], [1, 32]]), ALU.is_ge)
                    R.add('dve', lambda e, o=ej[:, jc * 16:(jc + 1) * 16], i=cmpj: e.tensor_reduce(o, i, AX.X, ALU.add),
                          reads=[tmpA[0][:, 0:512]], writes=[ej[:, jc * 16:(jc + 1) * 16]])
                oob = sm[:, 192:256]
                R.ts(oob, ej, 31.5, 1.0e6, ALU.is_gt, ALU.mult)
                R.ts(ej, ej, float(32 * l), 128.0, ALU.add, ALU.mult)
                R.tt(ej, ej, oob, ALU.add)
                R.ts(ej, ej, cx[:, CX_IOTAP:CX_IOTAP + 1], None, ALU.add)
                R.copy(idxw, ej)
                dump(f"ctot{l}", ctot, (128, 32))
                dump(f"ej{l}", ej, (128, 64))
                if stop == 'r4':
                    break
                for k in range(2):
                    tk = tmpA[k][:, :].rearrange("p (b e) -> p b e", e=32)
                    R.tt(tk, sel[:, :, k, :], posl, ALU.mult)
                    sf = sm[:, 320 + 16 * k:336 + 16 * k]
                    R.add('dve', lambda e, o=sf, i=tk: e.tensor_reduce(o, i, AX.X, ALU.add), reads=[tmpA[k][:, :]], writes=[sf])
                    R.copy(idx[:, :, k], sf)
                dump(f"idx{l}", idx[:, :, :], (128, NTB, 2), I32)
                dump(f"posl{l}", posl[:, :, :], (128, NTB, 32))
                if stop == 'route':
                    break
                for tb in range(NTB):
                    for k in range(2):
                        R.add('pool', lambda e, o=Xs.ap(), ix=idx[:, tb, k:k + 1], src=xn2[:, tb, :]: e.indirect_dma_start(
                            out=o, out_offset=bass.IndirectOffsetOnAxis(ap=ix, axis=0), in_=src, in_offset=None),
                            reads=[xn2[:, tb, :], idx[:, tb, k:k + 1]], writes=[("XsV", 0, 1, tb * 2 + k, tb * 2 + k + 1)], dma=True)
                AR.off = mark
                wbuf = [AR.alloc(WROW, BF16) for _ in range(3)]
                xg = [AR.alloc(DM, BF16) for _ in range(2)]
                xgT = [AR.alloc(8 * 128, BF16).rearrange("p (k r) -> p k r", r=128) for _ in range(2)]
                hact = AR.alloc(256, BF16)
                sgt = AR.alloc(256, F32)
                hTe = AR.alloc(256, BF16).rearrange("p (c r) -> p c r", r=128)
                ybuf = [AR.alloc(DM, BF16) for _ in range(2)]
                def issue_loads(jb):
                    R.add('pool', lambda e, o=wbuf[jb % 3], ix=idxw[:, jb:jb + 1]: gather_w(e, o, ix),
                          reads=[R.whole(Wall), idxw[:, jb:jb + 1]], writes=[wbuf[jb % 3]], dma=True)
                    R.dma('sp', xg[jb % 2], Xs[jb * 128:(jb + 1) * 128, :], extra_reads=[("XsV", 0, 1, 0, 1 << 30)])
                issue_loads(0)
                issue_loads(1)
                pe_warm()
                for jb in range(NBLK):
                    wb_, xg_, xgT_, yb_ = wbuf[jb % 3], xg[jb % 2], xgT[jb % 2], ybuf[jb % 2]
                    for half in range(2):
                        psb = PS.get().bitcast(BF16)
                        for i in range(4):
                            k = half * 4 + i
                            R.tr(psb[:, i * 128:(i + 1) * 128], xg_[:, k * 128:(k + 1) * 128], identb)
                        evac(xgT_[:, half * 4:half * 4 + 4, :], psb[:, 0:512].rearrange("p (a b) -> p a b", b=128))
                    psg = PS.get()
                    for k in range(8):
                        R.mm(psg[:, :], xgT_[:, k, :], wb_[:, k * 512:(k + 1) * 512], start=(k == 0), stop=(k == 7))
                    R.act(sgt, psg[:, 0:256], AF.Silu)
                    R.tt(hact, sgt, psg[:, 256:512], ALU.mult)
                    psb = PS.get().bitcast(BF16)
                    for c in range(2):
                        R.tr(psb[:, c * 128:(c + 1) * 128], hact[:, c * 128:(c + 1) * 128], identb)
                    evac(hTe, psb[:, 0:256].rearrange("p (a b) -> p a b", b=128))
                    for fh in range(2):
                        psy = PS.get()
                        for c in range(2):
                            R.mm(psy[:, :], hTe[:, c, :], wb_[:, 4096 + c * 1024 + fh * 512:4096 + c * 1024 + (fh + 1) * 512],
                                 start=(c == 0), stop=(c == 1))
                        evac(yb_[:, fh * 512:(fh + 1) * 512], psy[:, :])
                    R.dma('act', Yd[jb * 128:(jb + 1) * 128, :], yb_)
                    if jb + 2 < NBLK:
                        issue_loads(jb + 2)
                AR.off = mark
                yg = [[AR.alloc(DM, BF16) for _ in range(2)] for _ in range(3)]
                yacc = [AR.alloc(DM, F32) for _ in range(2)]
                for tb in range(NTB):
                    y1b, y2b = yg[tb % 3]
                    y1 = yacc[tb % 2]
                    for k, yy in enumerate((y1b, y2b)):
                        R.add('pool', lambda e, o=yy, ix=idx[:, tb, k:k + 1]: e.indirect_dma_start(
                            out=o, out_offset=None, in_=Yd.ap(), in_offset=bass.IndirectOffsetOnAxis(ap=ix, axis=0)),
                            reads=[R.whole(Yd), idx[:, tb, k:k + 1]], writes=[yy], dma=True)
                    R.act(y1, y1b, AF.Identity, scale=wts[:, tb, 0:1])
                    R.stt(y1, y2b, wts[:, tb, 1:2], y1, ALU.mult, ALU.add)
                    for half in range(2):
                        ps = PS.get()
                        for i in range(4):
                            k = half * 4 + i
                            R.tr(ps[:, i * 128:(i + 1) * 128], y1[:, k * 128:(k + 1) * 128], identf)
                        R.tt(hT[:, half * 4:half * 4 + 4, tb * 128:(tb + 1) * 128], hT[:, half * 4:half * 4 + 4, tb * 128:(tb + 1) * 128],
                             ps[:, :].rearrange("p (a b) -> p a b", b=128), ALU.add)
                dump(f"hmoe{l}", hT[:, :, 0:256], (128, 8, 256))
                if stop == 'moe':
                    break

                AR.reset()
                ptm = AR.alloc(NTB * 256, F32).rearrange("p (b c) -> p b c", c=256)
                pT = AR.alloc(2 * S, BF16).rearrange("p (c t) -> p c t", t=S)
                R.dma('sp', ptm, p_d[l, seq].rearrange("(b p) c -> p b c", p=128))
                for tb0 in range(0, NTB, 2):
                    ps = PS.get()
                    for c in range(2):
                        for tl in range(2):
                            R.tr(ps[:, (c * 2 + tl) * 128:(c * 2 + tl + 1) * 128], ptm[:, tb0 + tl, c * 128:(c + 1) * 128], identf)
                    evac(pT[:, :, tb0 * 128:(tb0 + 2) * 128], ps[:, :].rearrange("p (c t) -> p c t", t=256))
                norm_fm(16, l)
                for fo in range(8):
                    for g in range(NGR):
                        psg = PS.get()
                        for k in range(8):
                            wv = wgA if k < 4 else wgB
                            R.mm(psg[:, :], wv[:, k % 4, fo * 128:(fo + 1) * 128], hnT[:, k, g * GW:(g + 1) * GW],
                                 start=(k == 0), stop=(k == 7))
                        psp = PS.get()
                        for c in range(2):
                            R.mm(psp[:, :], wpp[:, c, fo * 128:(fo + 1) * 128], pT[:, c, g * GW:(g + 1) * GW],
                                 start=(c == 0), stop=(c == 1))
                        sg_ = tmpA[(fo * 4 + g) % 2]
                        R.act(sg_[:], psg[:, :], AF.Sigmoid)
                        R.tt(sg_[:], sg_[:], psp[:, :], ALU.mult)
                        R.tt(hT[:, fo, g * GW:(g + 1) * GW], hT[:, fo, g * GW:(g + 1) * GW], sg_[:], ALU.add, eng='pool')
                dump(f"hple{l}", hT[:, :, 0:256], (128, 8, 256))
            else:
                AR.reset()
                gfbc = AR.alloc(DM, F32)
                ot = [AR.alloc(DM, F32) for _ in range(2)]
                sm = AR.alloc(8, F32)
                R.dma('sp', gfbc, bass.AP(gfrow_d, 0, [[0, 128], [1, DM]]))
                for tb in range(NTB):
                    o_ = ot[tb % 2]
                    ssq = sm[:, 0:2]
                    pst = [PS.get(), PS.get()]
                    for half in range(2):
                        for i in range(4):
                            k = half * 4 + i
                            R.tr(pst[half][:, i * 128:(i + 1) * 128], hT[:, k, tb * 128:(tb + 1) * 128], identf)
                        R.act(tmpA[half][:], pst[half][:, :], AF.Square, accum_out=ssq[:, half:half + 1])
                    rs = sm[:, 2:3]
                    R.tt(rs, ssq[:, 0:1], ssq[:, 1:2], ALU.add)
                    R.act(rs, rs, AF.Ln, bias=epsc[:, 0:1], scale=1.0 / DM)
                    R.act(rs, rs, AF.Exp, scale=-0.5)
                    for half in range(2):
                        R.stt(o_[:, half * 512:(half + 1) * 512], pst[half][:, :], rs, gfbc[:, half * 512:(half + 1) * 512],
                              ALU.mult, ALU.mult)
                    R.dma('sp', out_d[seq, tb * 128:(tb + 1) * 128, :], o_)
                continue
            break
        toks = []
        for e in ('sp', 'pool', 'act'):
            toks += [(('d', e, s), 16 * c) for s, c in enumerate(R.slot_cnt[e]) if c > 0]
        R.wait_all('sp', toks)
        print("recorded ops:", {e: len(R.ops[e]) for e in R.ENG})
        R.emit()
    return nc, dbg_out


def prep_inputs(inputs, NLAYER=2):
    f = lambda a: np.ascontiguousarray(np.asarray(a, dtype=np.float32))
    wg, wu, wd = f(inputs["w_gate"]), f(inputs["w_up"]), f(inputs["w_down"])
    gu = np.concatenate([wg.reshape(2, 32, 8, 128, 256), wu.reshape(2, 32, 8, 128, 256)], axis=-1)
    gu = gu.transpose(0, 1, 3, 2, 4).reshape(2, 32, 128, 4096)
    dn = wd.reshape(2, 32, 2, 128, 1024).transpose(0, 1, 3, 2, 4).reshape(2, 32, 128, 2048)
    wall = np.ascontiguousarray(np.concatenate([gu, dn], axis=-1).reshape(2 * 32 * 128, WROW))
    rw = np.ascontiguousarray(np.concatenate([f(inputs["router_gw"]), f(inputs["router_ew"])], axis=-1))
    rb = np.ascontiguousarray(np.concatenate([f(inputs["router_gb"]), f(inputs["router_eb"])], axis=-1))
    vec = np.zeros((2, 128, NV), np.float32)
    for l in range(2):
        vec[l, :, 0:8] = f(inputs["norm1_g"])[l].reshape(8, 128).T
        vec[l, :, 8:16] = f(inputs["norm2_g"])[l].reshape(8, 128).T
        vec[l, :, 16:24] = f(inputs["ple_norm_g"])[l].reshape(8, 128).T
        vec[l, :, 24:32] = f(inputs["final_g"]).reshape(8, 128).T
        vec[l, :, 32:35] = f(inputs["sb_out_g"])[l].reshape(3, 128).T
        vec[l, 0:96, 35:39] = f(inputs["mnorm_g"])[l].reshape(4, 96).T
        vec[l, :, 39:41] = f(inputs["sgu_out_g"])[l].reshape(2, 128).T
        cw = f(inputs["conv_w"])[l].reshape(4, 2, 4, 96)
        vec[l, 0:96, 41:73] = cw.transpose(3, 1, 2, 0).reshape(96, 32)
        cbb = f(inputs["conv_b"])[l].reshape(2, 4, 96)
        vec[l, 0:96, 73:81] = cbb.transpose(2, 0, 1).reshape(96, 8)
        vec[l, 0:4, 81] = f(inputs["igate_b"])[l]
        vec[l, 0:4, 82] = f(inputs["fgate_b"])[l]
    sguln = np.ascontiguousarray(np.stack([f(inputs["sgu_ln_g"]), f(inputs["sgu_ln_b"])], axis=1))
    c, cx = make_consts()
    shared = dict(w_in=f(inputs["w_in"]), w_out=f(inputs["w_out"]), ple_gate_w=f(inputs["ple_gate_w"]),
                  ple_proj_w=f(inputs["ple_proj_w"]), wall_src=wall, rw=rw, rb=rb, vec=vec,
                  sgu_w=f(inputs["sgu_w"]), sgu_b=f(inputs["sgu_b"]), sgu_ln=sguln, g2row=f(inputs["norm2_g"]),
                  gfrow=f(inputs["final_g"]).reshape(1, DM), consts=c, constsx=cx)
    return shared


_CACHE = {}


def kernel(**inputs):
    NCORE = 8
    x = np.asarray(inputs["x"], dtype=np.float32)
    p = np.asarray(inputs["p"], dtype=np.float32)
    B = x.shape[0]
    nseq = B // NCORE
    shared = prep_inputs(inputs)
    if "nc" not in _CACHE:
        _CACHE["nc"] = build(NSEQ=nseq, NLAYER=2)[0]
    nc = _CACHE["nc"]
    in_maps = []
    for c in range(NCORE):
        m = dict(shared)
        m["x"] = np.ascontiguousarray(x[c * nseq:(c + 1) * nseq])
        m["p"] = np.ascontiguousarray(p[:, c * nseq:(c + 1) * nseq])
        in_maps.append(m)
    res = run_bass_kernel_spmd(nc, in_maps, core_ids=list(range(NCORE)))
    out = np.concatenate([np.asarray(r["out"]) for r in res.results], axis=0)
    return out.astype(np.float32)
```

```python
import contextlib
import numpy as np
import concourse.bass as bass
import concourse.mybir as mybir

F32 = mybir.dt.float32
BF16 = mybir.dt.bfloat16
I32 = mybir.dt.int32
U32 = mybir.dt.uint32
AF = mybir.ActivationFunctionType
ALU = mybir.AluOpType
AX = mybir.AxisListType


def _dsize(dt):
    return mybir.dt.size(dt)


class Rec:
    ENG = ['pe', 'act', 'dve', 'pool', 'sp']

    def __init__(self, nc, same_engine_sync=True):
        self.nc = nc
        self.ops = {e: [] for e in self.ENG}
        self.cnt = {e: 0 for e in self.ENG}
        self.waited = {e: {} for e in self.ENG}
        self.recs = {}
        self.nslot = {'sp': 8, 'pool': 8, 'act': 4}
        self.slot_cnt = {e: [0] * n for e, n in self.nslot.items()}
        self.slot_rr = {e: 0 for e in self.nslot}
        self.ses = same_engine_sync
        self.nops = 0

    def rng(self, ap):
        if isinstance(ap, tuple):
            return ap
        t = ap.tensor
        name = t.name
        sz = _dsize(ap.dtype)
        dims = list(ap.ap)
        if isinstance(t, bass.DRamTensorHandle):
            lo = ap.offset
            hi = lo + sum((c - 1) * abs(s) for s, c in dims) + 1
            return (name, 0, 1, lo * sz, hi * sz)
        pstride, pcount = dims[0]
        if pstride == 0:
            pstride = 1 << 40
        plo = ap.offset // pstride if pstride < (1 << 40) else 0
        flo = ap.offset - plo * pstride if pstride < (1 << 40) else ap.offset
        fhi = flo + sum((c - 1) * abs(s) for s, c in dims[1:]) + 1
        return (name, plo, plo + pcount, flo * sz, fhi * sz)

    def whole(self, t):
        return (t.name, 0, 1 << 20, 0, 1 << 60)

    def add(self, eng, fn, reads=(), writes=(), dma=False):
        deps = set()
        rr = [self.rng(a) for a in reads]
        ww = [self.rng(a) for a in writes]
        for (name, plo, phi, flo, fhi) in rr:
            for r in self.recs.get(name, ()):
                if r[4] and r[0] < phi and plo < r[1] and r[2] < fhi and flo < r[3]:
                    deps.add(r[5])
        for (name, plo, phi, flo, fhi) in ww:
            for r in self.recs.get(name, ()):
                if r[0] < phi and plo < r[1] and r[2] < fhi and flo < r[3]:
                    deps.add(r[5])
        waits = {}
        for (semkey, val) in deps:
            if semkey[0] == 'c' and semkey[1] == eng:
                if eng == 'pe' or not self.ses:
                    continue
            if self.waited[eng].get(semkey, 0) >= val:
                continue
            if waits.get(semkey, 0) < val:
                waits[semkey] = val
        if dma:
            s = self.slot_rr[eng]
            self.slot_rr[eng] = (s + 1) % self.nslot[eng]
            semkey = ('d', eng, s)
            prev = self.slot_cnt[eng][s]
            if prev > 0 and self.waited[eng].get(semkey, 0) < 16 * prev:
                if waits.get(semkey, 0) < 16 * prev:
                    waits[semkey] = 16 * prev
            self.slot_cnt[eng][s] = prev + 1
            token = (semkey, 16 * (prev + 1))
        else:
            self.cnt[eng] += 1
            token = (('c', eng), self.cnt[eng])
        for k, v in waits.items():
            self.waited[eng][k] = v
        self.ops[eng].append((waits, fn, token))
        self.nops += 1
        for (name, plo, phi, flo, fhi) in ww:
            lst = self.recs.setdefault(name, [])
            lst[:] = [r for r in lst if not (plo <= r[0] and r[1] <= phi and flo <= r[2] and r[3] <= fhi)]
            lst.append((plo, phi, flo, fhi, True, token))
        for (name, plo, phi, flo, fhi) in rr:
            lst = self.recs.setdefault(name, [])
            sk = token[0]
            lst[:] = [r for r in lst if not ((not r[4]) and r[5][0] == sk and r[0] == plo and r[1] == phi
                                             and r[2] == flo and r[3] == fhi)]
            lst.append((plo, phi, flo, fhi, False, token))
        return token

    def wait_all(self, eng, tokens):
        waits = {}
        for (semkey, val) in tokens:
            if waits.get(semkey, 0) < val:
                waits[semkey] = val
        for k, v in waits.items():
            self.waited[eng][k] = max(self.waited[eng].get(k, 0), v)
        self.ops[eng].append((waits, None, None))

    def emit(self):
        nc = self.nc
        needed = {e: set() for e in self.ENG}
        for e in self.ENG:
            for (waits, fn, token) in self.ops[e]:
                for (semkey, val) in waits.items():
                    if semkey[0] == 'c':
                        needed[semkey[1]].add(val)
        rank = {}
        for e in self.ENG:
            srt = sorted(needed[e])
            rank[e] = {v: i + 1 for i, v in enumerate(srt)}
        with contextlib.ExitStack() as st:
            sems = {}
            for e in self.ENG:
                sems[('c', e)] = st.enter_context(nc.semaphore(f"c_{e}"))
            for e, n in self.nslot.items():
                for s in range(n):
                    sems[('d', e, s)] = st.enter_context(nc.semaphore(f"d_{e}_{s}"))
            block = st.enter_context(nc.Block())
            rec = self

            def replay(e, eng):
                for (waits, fn, token) in rec.ops[e]:
                    for (semkey, val) in waits.items():
                        if semkey[0] == 'c':
                            val = rank[semkey[1]][val]
                        eng.wait_ge(sems[semkey], val)
                    if fn is None:
                        continue
                    inst = fn(eng)
                    semkey, val = token
                    if semkey[0] == 'd':
                        inst.then_inc(sems[semkey], 16)
                    elif val in rank[e]:
                        inst.then_inc(sems[semkey], 1)

            if self.ops['pe']:
                @block.tensor
                def _(eng):
                    replay('pe', eng)
            if self.ops['act']:
                @block.scalar
                def _(eng):
                    replay('act', eng)
            if self.ops['dve']:
                @block.vector
                def _(eng):
                    replay('dve', eng)
            if self.ops['pool']:
                @block.gpsimd
                def _(eng):
                    replay('pool', eng)
            if self.ops['sp']:
                @block.sync
                def _(eng):
                    replay('sp', eng)

    def dma(self, eng, out, in_, extra_reads=(), extra_writes=(), **kw):
        return self.add(eng, lambda e: e.dma_start(out=out, in_=in_, **kw),
                        reads=[in_] + list(extra_reads), writes=[out] + list(extra_writes), dma=True)

    def mm(self, out, lhsT, rhs, start=True, stop=True, **kw):
        return self.add('pe', lambda e: e.matmul(out, lhsT, rhs, start=start, stop=stop, **kw),
                        reads=[lhsT, rhs], writes=[out])

    def tr(self, out, in_, ident):
        return self.add('pe', lambda e: e.transpose(out, in_, ident), reads=[in_, ident], writes=[out])

    def act(self, out, in_, func, bias=None, scale=None, accum_out=None, eng='act'):
        reads = [in_]
        kw = {}
        if bias is not None:
            kw['bias'] = bias
            if not isinstance(bias, (int, float)):
                reads.append(bias)
        if scale is not None:
            kw['scale'] = scale
            if not isinstance(scale, (int, float)):
                reads.append(scale)
        writes = [out]
        if accum_out is not None:
            kw['accum_out'] = accum_out
            writes.append(accum_out)
        return self.add(eng, lambda e: e.activation(out, in_, func, **kw), reads=reads, writes=writes)

    def tt(self, out, in0, in1, op, eng='dve'):
        return self.add(eng, lambda e: e.tensor_tensor(out, in0, in1, op), reads=[in0, in1], writes=[out])

    def ts(self, out, in0, s1, s2, op0, op1=None, eng='dve', accum_out=None):
        reads = [in0]
        for s in (s1, s2):
            if s is not None and not isinstance(s, (int, float)):
                reads.append(s)
        writes = [out]
        kw = {}
        if accum_out is not None:
            kw['accum_out'] = accum_out
            writes.append(accum_out)
        if op1 is None:
            return self.add(eng, lambda e: e.tensor_scalar(out, in0, s1, None, op0, **kw), reads=reads, writes=writes)
        return self.add(eng, lambda e: e.tensor_scalar(out, in0, s1, s2, op0, op1, **kw), reads=reads, writes=writes)

    def stt(self, out, in0, scalar, in1, op0, op1, eng='dve'):
        reads = [in0, in1]
        if not isinstance(scalar, (int, float)):
            reads.append(scalar)
        return self.add(eng, lambda e: e.scalar_tensor_tensor(out, in0, scalar, in1, op0, op1), reads=reads, writes=[out])

    def copy(self, out, in_, eng='dve'):
        if eng == 'act':
            return self.add('act', lambda e: e.copy(out, in_), reads=[in_], writes=[out])
        return self.add(eng, lambda e: e.tensor_copy(out, in_), reads=[in_], writes=[out])

    def memset(self, ap, val, eng='dve'):
        return self.add(eng, lambda e: e.memset(ap, val), writes=[ap])


from concourse.bass_utils import run_bass_kernel_spmd

S = 2048
DM = 1024
NIN = 3208
NTB = 16
NGR = 4
GW = 512
OFF = dict(qa=0, ka=384, va=768, qb=1152, kb=1536, vb=1920, ob=2304, ib=2688, fb=2692, uc=2696, vc=2952)
NBLK = 64
WROW = 6144
EPS = 1e-6
NV = 96
C_ID = 0
C_ONE = 128
C_TRIS = 256
C_TRIC = 384
C_MSB = 512
C_MML = 512 + 2048
C_END = 512 + 4096
CX_IOTAP = 0
CX_IOTAJ = 1
CX_THR = 65
CX_SEL4 = 81
CX_SELR = 81 + 512
CX_END = 81 + 512 + 96


def make_consts():
    c = np.zeros((128, C_END), np.float32)
    c[:, C_ID:C_ID + 128] = np.eye(128)
    c[:, C_ONE:C_ONE + 128] = 1.0
    j = np.arange(128)[:, None]
    s = np.arange(128)[None, :]
    c[:, C_TRIS:C_TRIS + 128] = (j >= s)
    c[:, C_TRIC:C_TRIC + 128] = (j < s)
    for jj in range(4):
        m_sb = np.zeros((128, 4, 128), np.float32)
        m_ml = np.zeros((128, 4, 128), np.float32)
        for n in range(4):
            if n > jj:
                m_sb[:, n, :] = 1.0
                m_ml[:, n, :] = 1.0
            elif n == jj:
                m_sb[:, n, :] = (j < s)
                m_ml[:, n, :] = (j <= s)
        c[:, C_MSB + jj * 512:C_MSB + (jj + 1) * 512] = m_sb.reshape(128, 512)
        c[:, C_MML + jj * 512:C_MML + (jj + 1) * 512] = m_ml.reshape(128, 512)
    cx = np.zeros((128, CX_END), np.float32)
    cx[:, CX_IOTAP] = np.arange(128)
    cx[:, CX_IOTAJ:CX_IOTAJ + 64] = np.arange(64)[None, :]
    cx[:, CX_THR:CX_THR + 16] = (np.arange(16) * 128)[None, :]
    for h in range(4):
        cx[h, CX_SEL4 + h * 128:CX_SEL4 + (h + 1) * 128] = 1.0
    cx[96, CX_SELR:CX_SELR + 96] = 1.0
    return c, cx


class PSM:
    def __init__(self, banks):
        self.banks = banks
        self.pinned = set()
        self.i = 0

    def _next(self):
        for _ in range(16):
            b = self.i
            self.i = (self.i + 1) % len(self.banks)
            if b not in self.pinned:
                return b
        raise RuntimeError("no psum bank")

    def get(self):
        return self.banks[self._next()]

    def pin(self):
        b = self._next()
        self.pinned.add(b)
        return self.banks[b]

    def unpin(self, bank):
        self.pinned.discard(self.banks.index(bank))


def run_pipeline(groups):
    timeline = {}
    t0 = 0
    for gi, (n_r, fn, ov) in enumerate(groups):
        for r in range(n_r):
            timeline.setdefault(t0 + r, []).append((gi, r))
        t0 = t0 + n_r - ov
    for t in sorted(timeline):
        for (gi, r) in timeline[t]:
            groups[gi][1](r)


def bcast(ap, dims):
    p = list(ap.ap)[0]
    return bass.AP(ap.tensor, ap.offset, [[p[0], p[1]]] + [list(d) for d in dims])


SKIPMIX = False


def build(NSEQ=4, NLAYER=2, dbg=None, stop=None):
    nc = bass.Bass("TRN2", target_bir_lowering=False)
    R = Rec(nc)
    dt = nc.dram_tensor
    x_d = dt("x", [NSEQ, S, DM], F32, kind="ExternalInput")
    p_d = dt("p", [2, NSEQ, S, 256], F32, kind="ExternalInput")
    win_d = dt("w_in", [2, DM, NIN], F32, kind="ExternalInput")
    wout_d = dt("w_out", [2, DM, DM], F32, kind="ExternalInput")
    pgw_d = dt("ple_gate_w", [2, DM, DM], F32, kind="ExternalInput")
    ppw_d = dt("ple_proj_w", [2, 256, DM], F32, kind="ExternalInput")
    wall_d = dt("wall_src", [2 * 32 * 128, WROW], F32, kind="ExternalInput")
    rw_d = dt("rw", [2, DM, 36], F32, kind="ExternalInput")
    rb_d = dt("rb", [2, 36], F32, kind="ExternalInput")
    vec_d = dt("vec", [2, 128, NV], F32, kind="ExternalInput")
    sguw_d = dt("sgu_w", [2, 4, 128, 128], F32, kind="ExternalInput")
    sgub_d = dt("sgu_b", [2, 4, 128], F32, kind="ExternalInput")
    sguln_d = dt("sgu_ln", [2, 2, 256], F32, kind="ExternalInput")
    g2row_d = dt("g2row", [2, DM], F32, kind="ExternalInput")
    gfrow_d = dt("gfrow", [1, DM], F32, kind="ExternalInput")
    c_d = dt("consts", [128, C_END], F32, kind="ExternalInput")
    cx_d = dt("constsx", [128, CX_END], F32, kind="ExternalInput")
    out_d = dt("out", [NSEQ, S, DM], F32, kind="ExternalOutput")
    Xs = dt("Xs", [NBLK * 128, DM], BF16, kind="Internal")
    Yd = dt("Yd", [NBLK * 128, DM], BF16, kind="Internal")
    Wall = dt("Wall", [2 * 32 * 128, WROW], BF16, kind="Internal")
    dbg_out = {}

    st = contextlib.ExitStack()
    with st:
        def sb(name, shape, dtp):
            return st.enter_context(nc.sbuf_tensor(name, shape, dtp))
        hT = sb("hT", [128, 8, S], F32)
        hnT = sb("hnT", [128, 8, S], BF16)
        NSLAB = 4
        slabs = [sb(f"slab{i}", [128, 4096], BF16) for i in range(NSLAB)]
        cb = sb("cb", [128, C_END], BF16)
        cf = sb("cf", [128, 256], F32)
        cx = sb("cx", [128, CX_END], F32)
        vecs = [sb(f"vec{l}", [128, NV], F32) for l in range(2)]
        rstd_bc = sb("rstd_bc", [128, S], F32)
        tmpA = [sb(f"tmpA{i}", [128, 512], F32) for i in range(3)]
        epsc = sb("epsc", [128, 1], F32)
        AR_BYTES = 51 * 1024
        arena = sb("arena", [128, AR_BYTES // 4], F32)
        arena_b = arena.bitcast(BF16)
        arena_i = arena.bitcast(I32)
        banks = [st.enter_context(nc.psum_tensor(f"ps{i}", [128, 512], F32)) for i in range(8)]
        PS = PSM(banks)
        print("sbuf remaining", nc.sbuf_bytes_remaining)

        identf = cf[:, 0:128]
        onesf = cf[:, 128:256]
        identb = cb[:, C_ID:C_ID + 128]
        onesb = cb[:, C_ONE:C_ONE + 128]
        trisb = cb[:, C_TRIS:C_TRIS + 128]
        tricb = cb[:, C_TRIC:C_TRIC + 128]

        class Arena:
            def __init__(self):
                self.off = 0

            def reset(self):
                self.off = 0

            def alloc(self, n, dtp):
                sz = 4 if dtp in (F32, I32) else 2
                nbytes = (n * sz + 31) // 32 * 32
                assert self.off + nbytes <= AR_BYTES, (self.off, nbytes)
                start = self.off // sz
                self.off += nbytes
                h = arena if dtp == F32 else (arena_i if dtp == I32 else arena_b)
                return h[:, start:start + n]
        AR = Arena()

        slab_i = [0]

        def next_slab():
            s = slabs[slab_i[0] % NSLAB]
            slab_i[0] += 1
            return s

        def dump(name, ap, shape, dtp=F32):
            if dbg is None or name not in dbg:
                return
            d = dt("dbg_" + name, list(shape), dtp, kind="ExternalOutput")
            dbg_out[name] = d
            R.dma('sp', d.ap(), ap)

        flip = [0]
        _breg = {}

        def gather_w(e, o, ix):
            reg = e.to_reg(2 * 32 * 128 - 1)
            inst = e.indirect_dma_start(out=o, out_offset=None, in_=Wall.ap(),
                                        in_offset=bass.IndirectOffsetOnAxis(ap=ix, axis=0),
                                        bounds_check=reg, oob_is_err=False)
            e.free_register(reg)
            return inst

        def pe_warm(n=24):
            bank = PS.get()
            for _ in range(n):
                R.mm(bank[:, :], onesb, cb[:, C_MSB:C_MSB + 512], start=True, stop=True)

        def evac(out, in_, scale=None):
            flip[0] ^= 1
            if scale is not None:
                R.act(out, in_, AF.Copy, scale=scale)
            elif flip[0]:
                R.act(out, in_, AF.Copy)
            else:
                R.copy(out, in_)

        R.dma('pool', cb[:], c_d.ap())
        R.dma('sp', cf[:], c_d[:, 0:256])
        R.dma('sp', cx[:], cx_d.ap())
        for l in range(2):
            R.dma('sp', vecs[l][:], vec_d[l])
        R.memset(epsc[:], EPS)

        wall_jobs = list(range(NLAYER * 32)) if (stop is None or stop in ('moe', 'ple', 'all', 'route')) else []

        def wall_tick(n=1):
            for _ in range(n):
                if not wall_jobs:
                    return
                i = wall_jobs.pop(0)
                R.dma('pool', Wall[i * 128:(i + 1) * 128, :], wall_d[i * 128:(i + 1) * 128, :], max_dma_last_dim=4096)
        if stop is None or stop in ('moe', 'ple', 'all', 'route'):
            AR.reset()
            zt = AR.alloc(4096, BF16)
            R.memset(zt, 0.0)
            for i in range(NBLK * 128 // 512):
                R.dma('sp', Xs[i * 512:(i + 1) * 512, :].rearrange("(a p) d -> p a d", p=128),
                      zt.rearrange("p (a d) -> p a d", d=1024), extra_writes=[("XsV", 0, 1, 0, 1 << 30)])

        def norm_fm(gcol, l, src=None):
            for g in range(NGR):
                ps = PS.get()
                for k in range(8):
                    sq = tmpA[k % 2]
                    R.act(sq[:], hT[:, k, g * GW:(g + 1) * GW], AF.Square)
                    R.mm(ps[:, :], onesf, sq[:], start=(k == 0), stop=(k == 7))
                t = tmpA[2]
                R.act(t[:], ps[:, :], AF.Ln, bias=epsc[:, 0:1], scale=1.0 / DM)
                R.act(rstd_bc[:, g * GW:(g + 1) * GW], t[:], AF.Exp, scale=-0.5)
                for k in range(8):
                    R.stt(hnT[:, k, g * GW:(g + 1) * GW], hT[:, k, g * GW:(g + 1) * GW],
                          vecs[l][:, gcol + k:gcol + k + 1], rstd_bc[:, g * GW:(g + 1) * GW], ALU.mult, ALU.mult)

        def load_in_cols(slab, dst_col, l, col0, ncols, width):
            v = slab[:, 0:8 * width].rearrange("p (k n) -> p k n", n=width)
            src = win_d[l].rearrange("(k p) n -> p k n", p=128)[:, :, col0:col0 + ncols]
            R.dma('pool', v[:, :, dst_col:dst_col + ncols], src)
            return v

        def proj_fm(wv, c0, M, out_fn):
            for g in range(NGR):
                ps = PS.get()
                for k in range(8):
                    R.mm(ps[0:M, :], wv[:, k, c0:c0 + M], hnT[:, k, g * GW:(g + 1) * GW], start=(k == 0), stop=(k == 7))
                out_fn(g, ps)

        def proj_tm(wv, c0, N, out_fn):
            per = 4 if N <= 128 else 1
            for tb0 in range(0, NTB, per):
                ps = PS.get()
                for i in range(per):
                    tb = tb0 + i
                    for k in range(8):
                        R.mm(ps[:, i * N:(i + 1) * N], hnT[:, k, tb * 128:(tb + 1) * 128], wv[:, k, c0:c0 + N],
                             start=(k == 0), stop=(k == 7))
                out_fn(tb0, per, ps)

        def outproj_add(wv, kparts, rhs_fn):
            nk = len(kparts)
            for fo in range(8):
                for g in range(NGR):
                    ps = PS.get()
                    for j, K in enumerate(kparts):
                        R.mm(ps[:, :], wv[0:K, j, fo * 128:(fo + 1) * 128], rhs_fn(j, g), start=(j == 0), stop=(j == nk - 1))
                    R.tt(hT[:, fo, g * GW:(g + 1) * GW], hT[:, fo, g * GW:(g + 1) * GW], ps[:, :], ALU.add)

        for seq in range(NSEQ):
            AR.reset()
            xts = [AR.alloc(1024, F32) for _ in range(2)]
            for tb in range(NTB):
                xt = xts[tb % 2]
                R.dma('sp', xt, x_d[seq, tb * 128:(tb + 1) * 128, :])
                for half in range(2):
                    ps = PS.get()
                    for i in range(4):
                        k = half * 4 + i
                        R.tr(ps[:, i * 128:(i + 1) * 128], xt[:, k * 128:(k + 1) * 128], identf)
                    evac(hT[:, half * 4:half * 4 + 4, tb * 128:(tb + 1) * 128],
                         ps[:, :].rearrange("p (a b) -> p a b", b=128))

            for l in range(NLAYER):
                V = vecs[l]
                norm_fm(0, l)
                dump(f"hn{l}", hnT[:, 0, 0:512], (128, 512), BF16)
                if stop == 'norm1':
                    break
                if not SKIPMIX:
                    AR.reset()
                    yaT = AR.alloc(3 * S, BF16).rearrange("p (j t) -> p j t", t=S)
                    qT = AR.alloc(S, BF16)
                    kT = AR.alloc(S, BF16)
                    vtm = AR.alloc(NTB * 128, BF16).rearrange("p (b c) -> p b c", c=128)
                    e_t = [[AR.alloc(512, BF16) for _ in range(2)] for _ in range(2)]
                    sp_t = [[AR.alloc(512, BF16) for _ in range(3)] for _ in range(2)]
                    p_t = [[AR.alloc(512, BF16) for _ in range(2)] for _ in range(2)]
                    a_t = [[AR.alloc(512, BF16) for _ in range(2)] for _ in range(2)]
                    wA = [None, None, None]

                    def loadA(j):
                        sl = next_slab()
                        load_in_cols(sl, 0, l, OFF['qa'] + j * 128, 128, 384)
                        load_in_cols(sl, 128, l, OFF['ka'] + j * 128, 128, 384)
                        wA[j] = load_in_cols(sl, 256, l, OFF['va'] + j * 128, 128, 384)
                    loadA(0)
                    for j in range(3):
                        wv = wA[j]
                        if j + 1 < 3:
                            loadA(j + 1)
                        proj_fm(wv, 0, 128, lambda g, ps: R.act(qT[:, g * GW:(g + 1) * GW], ps[:, :], AF.Copy, scale=0.125))
                        proj_fm(wv, 128, 128, lambda g, ps: R.copy(kT[:, g * GW:(g + 1) * GW], ps[:, :]))
                        proj_tm(wv, 256, 128, lambda tb0, per, ps: evac(
                            vtm[:, tb0:tb0 + per, :], ps[:, :].rearrange("p (a b) -> p a b", b=128)))
                        groupsA = []
                        for G in range(NGR):
                            def mk(G=G, j=j):
                                nb = 4 * G + 4
                                its = list(range(nb - 1, -1, -1))
                                n = len(its)
                                stt_ = dict(accs=None, crs=None, psz={})

                                def c0f(i):
                                    return max(0, its[i] - 4 * G) * 128

                                def sZ(i):
                                    b = its[i]
                                    c0 = c0f(i)
                                    for hp in range(2):
                                        pl, ph = hp * 64, hp * 64 + 64
                                        psz = PS.get()
                                        stt_['psz'][(i, hp)] = psz
                                        R.mm(psz[:, c0:512], kT[pl:ph, b * 128:(b + 1) * 128], qT[pl:ph, G * GW + c0:(G + 1) * GW])

                                def sEL(i):
                                    b = its[i]
                                    c0 = c0f(i)
                                    for hp in range(2):
                                        et = e_t[hp][i % 2][:, c0:512]
                                        R.act(et, stt_['psz'].pop((i, hp))[:, c0:512], AF.Exp)
                                        if b >= 4 * G:
                                            jj = b - 4 * G
                                            R.tt(et, et, cb[:, C_MSB + jj * 512 + c0:C_MSB + (jj + 1) * 512], ALU.mult)
                                    for hp in range(2):
                                        et, spt = e_t[hp][i % 2][:, c0:512], sp_t[hp][i % 3][:, c0:512]
                                        R.act(spt, et, AF.Ln, bias=1.0)

                                def sT(i):
                                    if stt_['accs'] is None:
                                        stt_['accs'] = [PS.pin(), PS.pin()]
                                        stt_['crs'] = [PS.pin(), PS.pin()]
                                    c0 = c0f(i)
                                    for hp in range(2):
                                        R.mm(stt_['crs'][hp][:, c0:512], trisb, sp_t[hp][i % 3][:, c0:512], start=(i == 0), stop=False)

                                def sX(i):
                                    c0 = c0f(i)
                                    for hp in range(2):
                                        R.act(p_t[hp][i % 2][:, c0:512], stt_['crs'][hp][:, c0:512], AF.Exp, scale=-1.0)
                                        R.tt(a_t[hp][i % 2][:, c0:512], e_t[hp][i % 2][:, c0:512], p_t[hp][i % 2][:, c0:512], ALU.mult)

                                def sC(i):
                                    c0 = c0f(i)
                                    if i < n - 1:
                                        for hp in range(2):
                                            R.mm(stt_['crs'][hp][:, c0:512], tricb, sp_t[hp][i % 3][:, c0:512], start=False, stop=False)

                                def sA(i):
                                    b = its[i]
                                    c0 = c0f(i)
                                    for hp in range(2):
                                        pl, ph = hp * 64, hp * 64 + 64
                                        R.mm(stt_['accs'][hp][pl:ph, c0:512], vtm[:, b, pl:ph], a_t[hp][i % 2][:, c0:512],
                                             start=(i == 0), stop=(i == n - 1))

                                def rnd(r):
                                    if r % 2 == 0:
                                        wall_tick(1)
                                    if 0 <= r - 3 < n:
                                        sC(r - 3)
                                    if 0 <= r - 2 < n:
                                        sT(r - 2)
                                    if r < n:
                                        sZ(r)
                                    if 0 <= r - 3 < n:
                                        sA(r - 3)
                                    if 0 <= r - 1 < n:
                                        sEL(r - 1)
                                    if 0 <= r - 2 < n:
                                        sX(r - 2)
                                    if r == n + 2:
                                        for hp in range(2):
                                            pl, ph = hp * 64, hp * 64 + 64
                                            evac(yaT[pl:ph, j, G * GW:(G + 1) * GW], stt_['accs'][hp][pl:ph, :])
                                        for bk in stt_['accs'] + stt_['crs']:
                                            PS.unpin(bk)
                                return (n + 3, rnd, 2)
                            groupsA.append(mk())
                        run_pipeline(groupsA)
                    dump(f"yaT{l}", yaT[:, :, :], (128, 3, S), BF16)
                    if stop == 'attnA':
                        break
                    slo = next_slab()
                    wo = slo[:, 0:3 * 1024].rearrange("p (j n) -> p j n", n=1024)
                    R.dma('pool', wo, wout_d[l, 0:384, :].rearrange("(j p) n -> p j n", p=128))
                    for g in range(NGR):
                        ps = PS.get()
                        for j in range(3):
                            sq = tmpA[j % 2]
                            R.act(sq[:], yaT[:, j, g * GW:(g + 1) * GW], AF.Square)
                            R.mm(ps[:, :], onesf, sq[:], start=(j == 0), stop=(j == 2))
                        t = tmpA[2]
                        R.act(t[:], ps[:, :], AF.Ln, bias=epsc[:, 0:1], scale=1.0 / 384)
                        R.act(rstd_bc[:, g * GW:(g + 1) * GW], t[:], AF.Exp, scale=-0.5)
                        for j in range(3):
                            R.stt(yaT[:, j, g * GW:(g + 1) * GW], yaT[:, j, g * GW:(g + 1) * GW],
                                  V[:, 32 + j:33 + j], rstd_bc[:, g * GW:(g + 1) * GW], ALU.mult, ALU.mult)
                    dump(f"yanT{l}", yaT[:, :, :], (128, 3, S), BF16)
                    outproj_add(wo, [128, 128, 128], lambda j, g: yaT[:, j, g * GW:(g + 1) * GW])
                    if stop == 'outA':
                        break

                    AR.reset()
                    t1 = AR.alloc(S, F32)
                    AR.off -= S * 4
                    ybT = AR.alloc(4 * S, BF16).rearrange("p (h t) -> p h t", t=S)
                    Bf = rstd_bc
                    utm = AR.alloc(NTB * 4, F32).rearrange("p (b h) -> p b h", h=4)
                    nfb = AR.alloc(8, F32)[0:4, 0:1]
                    qb = AR.alloc(S, BF16)
                    kb = AR.alloc(S, BF16)
                    sgo = AR.alloc(S, BF16)
                    vbt = AR.alloc(NTB * 97, BF16).rearrange("p (b c) -> p b c", c=97)
                    R.memset(vbt[:, :, 96:97], 1.0)
                    numS = AR.alloc(GW, F32)
                    raw = [AR.alloc(GW + 4, BF16) for _ in range(3)]
                    dgw = AR.alloc(8 * 96, BF16).rearrange("p (m c) -> p m c", c=96)
                    d_t = [AR.alloc(512, BF16) for _ in range(3)]
                    w_t = [AR.alloc(512, BF16) for _ in range(2)]
                    slg = next_slab()
                    wg_ = load_in_cols(slg, 0, l, OFF['ib'], 8, 8)
                    R.ts(nfb, V[0:4, 82:83], -1.0, None, ALU.mult)
                    proj_fm(wg_, 4, 4, lambda g, ps: R.act(t1[0:4, g * GW:(g + 1) * GW], ps[0:4, :], AF.Exp, bias=nfb, scale=-1.0))
                    R.act(t1[0:4, :], t1[0:4, :], AF.Ln, bias=1.0)
                    R.add('dve', lambda e: e.tensor_tensor_scan(Bf[0:4, :], bcast(onesf[0:4, 0:1], [[0, S]]), t1[0:4, :], 0.0,
                                                                 ALU.mult, ALU.subtract),
                          reads=[onesf[0:4, 0:1], t1[0:4, :]], writes=[Bf[0:4, :]])
                    LNS = float(np.log(96.0 ** -0.5))

                    def u_out(g, ps):
                        uu = tmpA[0][0:4, :]
                        R.stt(uu, ps[0:4, :], V[0:4, 81:82], Bf[0:4, g * GW:(g + 1) * GW], ALU.add, ALU.subtract)
                        R.ts(uu, uu, LNS, None, ALU.add)
                        ps2 = PS.get()
                        for i in range(4):
                            R.tr(ps2[:, i * 4:(i + 1) * 4], uu[:, i * 128:(i + 1) * 128], identf[0:4, 0:4])
                        R.copy(utm[:, 4 * g:4 * g + 4, :], ps2[:, 0:16].rearrange("p (a b) -> p a b", b=4))
                    proj_fm(wg_, 0, 4, u_out)
                    dump(f"Bf{l}", Bf[0:4, :], (4, S))
                    dump(f"utm{l}", utm[:, :, :], (128, NTB, 4))
                    wB = [None] * 4

                    def loadB(h):
                        sl = next_slab()
                        load_in_cols(sl, 0, l, OFF['qb'] + h * 96, 96, 384)
                        load_in_cols(sl, 96, l, OFF['kb'] + h * 96, 96, 384)
                        load_in_cols(sl, 192, l, OFF['vb'] + h * 96, 96, 384)
                        wB[h] = load_in_cols(sl, 288, l, OFF['ob'] + h * 96, 96, 384)
                    loadB(0)
                    for h in range(4):
                        wv = wB[h]
                        if h + 1 < 4:
                            loadB(h + 1)
                        for qk in range(2):
                            cw = 41 + qk * 16 + h * 4
                            for tap in range(4):
                                R.ts(dgw[0:96, qk * 4 + tap, :], identf[0:96, 0:96], V[0:96, cw + tap:cw + tap + 1], None, ALU.mult)
                        for qk in range(2):
                            cbias = 73 + qk * 4 + h
                            dst = qb if qk == 0 else kb
                            R.memset(raw[0][0:96, 1:4], 0.0)

                            def conv_out(g, ps, qk=qk, cbias=cbias, dst=dst):
                                rw_ = raw[g % 3]
                                R.act(rw_[0:96, 4:4 + GW], ps[0:96, :], AF.Copy)
                                if g + 1 < NGR:
                                    R.copy(raw[(g + 1) % 3][0:96, 1:4], rw_[0:96, GW + 1:GW + 4])
                                pc = PS.get()
                                for tap in range(4):
                                    R.mm(pc[0:96, :], dgw[0:96, qk * 4 + tap, :], rw_[0:96, 1 + tap:1 + tap + GW],
                                         start=(tap == 0), stop=(tap == 3))
                                R.act(dst[0:96, g * GW:(g + 1) * GW], pc[0:96, :], AF.Silu, bias=V[0:96, cbias:cbias + 1])
                            proj_fm(wv, qk * 96, 96, conv_out)
                        if h == 0:
                            dump(f"qb0_{l}", qb[0:96, :], (96, S), BF16)
                        proj_tm(wv, 192, 96, lambda tb0, per, ps: evac(
                            vbt[:, tb0:tb0 + per, 0:96], ps[:, 0:per * 96].rearrange("p (a b) -> p a b", b=96)))
                        proj_fm(wv, 288, 96, lambda g, ps: R.act(sgo[0:96, g * GW:(g + 1) * GW], ps[0:96, :], AF.Sigmoid))
                        groupsB = []
                        pend_epi = []
                        for G in range(NGR):
                            def mkB(G=G, h=h):
                                nb = 4 * G + 4
                                n = nb
                                stb = dict(psB=None, num=None, den=None, pss={})

                                def sS(i):
                                    b = i
                                    if stb['psB'] is None:
                                        stb['psB'] = PS.pin()
                                        R.mm(stb['psB'][:, :], cx[0:4, CX_SEL4 + h * 128:CX_SEL4 + (h + 1) * 128], Bf[0:4, G * GW:(G + 1) * GW])
                                    pss = PS.get()
                                    stb['pss'][i] = pss
                                    c0 = max(0, b - 4 * G) * 128
                                    R.mm(pss[:, c0:512], kb[0:96, b * 128:(b + 1) * 128], qb[0:96, G * GW + c0:(G + 1) * GW])
                                    dt_ = d_t[i % 3][:, c0:512]
                                    R.act(dt_, stb['psB'][:, c0:512], AF.Exp, bias=utm[:, b, h:h + 1])
                                    if b >= 4 * G:
                                        jj = b - 4 * G
                                        R.tt(dt_, dt_, cb[:, C_MML + jj * 512 + c0:C_MML + (jj + 1) * 512], ALU.mult)
                                    if i == n - 1:
                                        PS.unpin(stb['psB'])

                                def sW(i):
                                    c0 = max(0, i - 4 * G) * 128
                                    R.tt(w_t[i % 2][:, c0:512], stb['pss'].pop(i)[:, c0:512], d_t[i % 3][:, c0:512], ALU.mult)

                                def sN(i):
                                    b = i
                                    c0 = max(0, i - 4 * G) * 128
                                    if stb['num'] is None:
                                        stb['num'] = PS.pin()
                                    R.mm(stb['num'][0:97, c0:512], vbt[:, b, :], w_t[i % 2][:, c0:512], start=(i == 0), stop=(i == n - 1))

                                def epi_steps():
                                    num = stb['num']
                                    dn, hh, sq = tmpA[0], tmpA[1], tmpA[2]
                                    box = {}

                                    def s0():
                                        R.act(numS[0:97, :], num[0:97, :], AF.Copy)
                                        PS.unpin(num)

                                    def s1():
                                        box['den'] = PS.get()
                                        R.mm(box['den'][0:96, :], cx[0:97, CX_SELR:CX_SELR + 96], numS[0:97, :])

                                    def s5():
                                        R.tt(hh[0:96, :], numS[0:96, :], dn[0:96, :], ALU.mult)
                                        if h == 0 and G == 0:
                                            dump(f"hb00_{l}", hh[0:96, :], (96, 512))

                                    def s7():
                                        box['ps'] = PS.get()
                                        R.mm(box['ps'][0:96, :], onesf[0:96, 0:96], sq[0:96, :])
                                    return [
                                        s0,
                                        s1,
                                        lambda: R.act(dn[0:96, :], box['den'][0:96, :], AF.Abs),
                                        lambda: R.ts(dn[0:96, :], dn[0:96, :], 1.0, None, ALU.max),
                                        lambda: R.act(dn[0:96, :], dn[0:96, :], AF.Ln),
                                        lambda: R.act(dn[0:96, :], dn[0:96, :], AF.Exp, scale=-1.0),
                                        s5,
                                        lambda: R.act(sq[0:96, :], hh[0:96, :], AF.Square),
                                        s7,
                                        lambda: R.act(sq[0:96, :], box['ps'][0:96, :], AF.Ln, bias=epsc[0:96, 0:1], scale=1.0 / 96),
                                        lambda: R.act(sq[0:96, :], sq[0:96, :], AF.Exp, scale=-0.5),
                                        lambda: R.stt(hh[0:96, :], hh[0:96, :], V[0:96, 35 + h:36 + h], sq[0:96, :], ALU.mult, ALU.mult),
                                        lambda: R.tt(ybT[0:96, h, G * GW:(G + 1) * GW], hh[0:96, :], sgo[0:96, G * GW:(G + 1) * GW], ALU.mult),
                                    ]

                                def rnd(r):
                                    if r % 4 == 0:
                                        wall_tick(1)
                                    if 0 <= r - 2 < n:
                                        sN(r - 2)
                                    if r < n:
                                        sS(r)
                                    if 0 <= r - 1 < n:
                                        sW(r - 1)
                                    if r >= 2 and pend_epi:
                                        pend_epi.pop(0)()
                                    if r == n + 1:
                                        while pend_epi:
                                            pend_epi.pop(0)()
                                        pend_epi.extend(epi_steps())
                                return (n + 2, rnd, 2)
                            groupsB.append(mkB())
                        run_pipeline(groupsB)
                        while pend_epi:
                            pend_epi.pop(0)()
                    dump(f"ybT{l}", ybT[0:96, :, :], (96, 4, S), BF16)
                    if stop == 'mlstm':
                        break
                    slo = next_slab()
                    wo = slo[:, 0:4 * 1024].rearrange("p (j n) -> p j n", n=1024)
                    R.dma('pool', wo[0:96, :, :], wout_d[l, 384:768, :].rearrange("(j p) n -> p j n", p=96))
                    outproj_add(wo, [96, 96, 96, 96], lambda j, g: ybT[0:96, j, g * GW:(g + 1) * GW])

                    AR.reset()
                    ycT = AR.alloc(2 * S, BF16).rearrange("p (j t) -> p j t", t=S)
                    uT = AR.alloc(S, BF16)
                    vtmC = AR.alloc(NTB * 128, F32).rearrange("p (b c) -> p b c", c=128)
                    vnb = AR.alloc(NTB * 128, BF16).rearrange("p (b c) -> p b c", c=128)
                    wsf = AR.alloc(4 * 128, F32).rearrange("p (g s) -> p g s", s=128)
                    wsT = AR.alloc(4 * 128, BF16).rearrange("p (g s) -> p g s", s=128)
                    bsT = AR.alloc(512, F32)
                    lng = AR.alloc(128, F32)
                    lnb = AR.alloc(128, F32)
                    st8 = AR.alloc(64, F32)
                    st8b = AR.alloc(64, F32)
                    gx = [AR.alloc(512, F32) for _ in range(3)]
                    R.dma('sp', wsf, sguw_d[l].rearrange("g t s -> t g s"))
                    R.memset(wsf[0:64, :, 64:128], 0.0)
                    ps = PS.get()
                    for gq in range(4):
                        R.tr(ps[:, gq * 128:(gq + 1) * 128], wsf[:, gq, :], identf)
                    R.copy(wsT, ps[:, :].rearrange("p (g s) -> p g s", s=128))

                    def gelu_chain(dst, src_ps, np_, n):
                        R.act(dst, src_ps, AF.Gelu_apprx_tanh)

                    for j in range(2):
                        slc = next_slab()
                        load_in_cols(slc, 0, l, OFF['uc'] + j * 128, 128, 256)
                        wv = load_in_cols(slc, 128, l, OFF['vc'] + j * 128, 128, 256)
                        R.dma('sp', lng, bass.AP(sguln_d, (l * 2 + 0) * 256 + j * 128, [[0, 128], [1, 128]]))
                        R.dma('sp', lnb, bass.AP(sguln_d, (l * 2 + 1) * 256 + j * 128, [[0, 128], [1, 128]]))
                        for gi in range(2):
                            for rep in range(4):
                                R.dma('sp', bsT[gi * 64:(gi + 1) * 64, rep * 128:(rep + 1) * 128],
                                      bass.AP(sgub_d, (l * 4 + 2 * j + gi) * 128, [[0, 64], [1, 128]]))
                        proj_fm(wv, 0, 128, lambda g, ps: gelu_chain(uT[:, g * GW:(g + 1) * GW], ps[:, :], 128, 512))

                        def v_out(tb0, per, ps):
                            gelu_chain(vtmC[:, tb0:tb0 + per, :].rearrange("p a b -> p (a b)"), ps[:, :], 128, 512)
                        proj_tm(wv, 128, 128, v_out)
                        for tb0 in range(0, NTB, 4):
                            vv = vtmC[:, tb0:tb0 + 4, :].rearrange("p a (g c) -> p (a g) c", c=64)
                            mu = st8[:, 0:8]
                            R.add('dve', lambda e, o=mu, i=vv: e.tensor_reduce(o, i, AX.X, ALU.add), reads=[vv], writes=[mu])
                            R.ts(mu, mu, 1.0 / 64, None, ALU.mult)
                            cen = gx[0].rearrange("p (a c) -> p a c", c=64)
                            R.tt(cen, vv, bcast(mu, [[1, 8], [0, 64]]), ALU.subtract)
                            sqv = gx[1].rearrange("p (a c) -> p a c", c=64)
                            R.act(sqv, cen, AF.Square)
                            var = st8b[:, 0:8]
                            R.add('dve', lambda e, o=var, i=sqv: e.tensor_reduce(o, i, AX.X, ALU.add), reads=[sqv], writes=[var])
                            R.act(var, var, AF.Ln, bias=epsc[:, 0:1], scale=1.0 / 64)
                            R.act(var, var, AF.Exp, scale=-0.5)
                            R.tt(cen, cen, bcast(var, [[1, 8], [0, 64]]), ALU.mult)
                            cen4 = gx[0].rearrange("p (a c) -> p a c", c=128)
                            R.tt(cen4, cen4, bcast(lng, [[0, 4], [1, 128]]), ALU.mult)
                            R.tt(vnb[:, tb0:tb0 + 4, :], cen4, bcast(lnb, [[0, 4], [1, 128]]), ALU.add)
                        for tb0 in range(0, NTB, 4):
                            ps = PS.get()
                            for i in range(4):
                                tb = tb0 + i
                                for gi in range(2):
                                    R.mm(ps[gi * 64:(gi + 1) * 64, i * 128:(i + 1) * 128], vnb[:, tb, gi * 64:(gi + 1) * 64],
                                         wsT[:, 2 * j + gi, :])
                            t = gx[2]
                            R.tt(t, ps[:, :], bsT, ALU.add)
                            R.tt(ycT[:, j, tb0 * 128:(tb0 + 4) * 128], t, uT[:, tb0 * 128:(tb0 + 4) * 128], ALU.mult)
                    dump(f"ycT{l}", ycT[:, :, :], (128, 2, S), BF16)
                    if stop == 'sgu':
                        break
                    ycn = ycT
                    slo = next_slab()
                    wo = slo[:, 0:2 * 1024].rearrange("p (j n) -> p j n", n=1024)
                    R.dma('pool', wo, wout_d[l, 768:1024, :].rearrange("(j p) n -> p j n", p=128))
                    for g in range(NGR):
                        ps = PS.get()
                        for j in range(2):
                            sq = tmpA[j % 2]
                            R.act(sq[:], ycT[:, j, g * GW:(g + 1) * GW], AF.Square)
                            R.mm(ps[:, :], onesf, sq[:], start=(j == 0), stop=(j == 1))
                        t = tmpA[2]
                        R.act(t[:], ps[:, :], AF.Ln, bias=epsc[:, 0:1], scale=1.0 / 256)
                        R.act(t[:], t[:], AF.Exp, scale=-0.5)
                        for j in range(2):
                            R.stt(ycn[:, j, g * GW:(g + 1) * GW], ycT[:, j, g * GW:(g + 1) * GW], V[:, 39 + j:40 + j], t[:],
                                  ALU.mult, ALU.mult)
                    outproj_add(wo, [128, 128], lambda j, g: ycn[:, j, g * GW:(g + 1) * GW])
                    dump(f"hmix{l}", hT[:, :, 0:256], (128, 8, 256))
                    if stop == 'mix':
                        break

                wall_tick(len(wall_jobs))
                slg0, slg1, slp = next_slab(), next_slab(), next_slab()
                wgA = slg0[:, 0:4096].rearrange("p (k n) -> p k n", n=1024)
                wgB = slg1[:, 0:4096].rearrange("p (k n) -> p k n", n=1024)
                wpp = slp[:, 0:2048].rearrange("p (k n) -> p k n", n=1024)
                R.dma('pool', wgA, pgw_d[l, 0:512, :].rearrange("(k p) n -> p k n", p=128))
                R.dma('pool', wgB, pgw_d[l, 512:1024, :].rearrange("(k p) n -> p k n", p=128))
                R.dma('pool', wpp, ppw_d[l].rearrange("(k p) n -> p k n", p=128))

                AR.reset()
                wts = AR.alloc(NTB * 2, F32).rearrange("p (b k) -> p b k", k=2)
                idx = AR.alloc(NTB * 2, I32).rearrange("p (b k) -> p b k", k=2)
                idxw = AR.alloc(NBLK, I32)
                mark = AR.off
                g2bc = AR.alloc(DM, F32)
                rws = AR.alloc(8 * 36, F32).rearrange("p (k n) -> p k n", n=36)
                rbb = AR.alloc(36, F32)
                sel = AR.alloc(NTB * 64, F32).rearrange("p (b k e) -> p b k e", k=2, e=32)
                Lall = AR.alloc(NTB * 36, F32)
                AR.off -= (NTB * 36 * 4 + 31) // 32 * 32
                posl = AR.alloc(512, F32).rearrange("p (b e) -> p b e", e=32)
                run = AR.alloc(512, F32).rearrange("p (b e) -> p b e", e=32)
                sm = AR.alloc(512, F32)
                s12b = AR.alloc(NTB * 32, BF16)
                xn2 = AR.alloc(NTB * DM, BF16).rearrange("p (b d) -> p b d", d=DM)
                R.dma('sp', g2bc, bass.AP(g2row_d, l * DM, [[0, 128], [1, DM]]))
                R.dma('sp', rbb, bass.AP(rb_d, l * 36, [[0, 128], [1, 36]]))
                R.dma('sp', rws, rw_d[l].rearrange("(k p) n -> p k n", p=128))
                for k in range(8):
                    R.ts(rws[:, k, :], rws[:, k, :], V[:, 8 + k:9 + k], None, ALU.mult)
                cnt_ps = PS.pin()
                pos_ps = PS.pin()
                L3 = Lall.rearrange("p (b n) -> p b n", n=36)
                for tb in range(NTB):
                    ssq = sm[:, 480 + 2 * (tb % 4):482 + 2 * (tb % 4)]
                    pst = [PS.get(), PS.get()]
                    for half in range(2):
                        for i in range(4):
                            k = half * 4 + i
                            R.tr(pst[half][:, i * 128:(i + 1) * 128], hT[:, k, tb * 128:(tb + 1) * 128], identf)
                        R.act(tmpA[half][:], pst[half][:, :], AF.Square, accum_out=ssq[:, half:half + 1])
                    rs = sm[:, 496 + (tb % 4):497 + (tb % 4)]
                    R.tt(rs, ssq[:, 0:1], ssq[:, 1:2], ALU.add)
                    R.act(rs, rs, AF.Ln, bias=epsc[:, 0:1], scale=1.0 / DM)
                    R.act(rs, rs, AF.Exp, scale=-0.5)
                    for half in range(2):
                        R.stt(xn2[:, tb, half * 512:(half + 1) * 512], pst[half][:, :], rs, g2bc[:, half * 512:(half + 1) * 512],
                              ALU.mult, ALU.mult)
                    psl = PS.get()
                    for k in range(8):
                        R.mm(psl[:, 0:36], hT[:, k, tb * 128:(tb + 1) * 128], rws[:, k, :], start=(k == 0), stop=(k == 7))
                    R.stt(L3[:, tb, :], psl[:, 0:36], rs, rbb, ALU.mult, ALU.add)
                dump(f"Lall{l}", Lall, (128, NTB * 36))
                if stop == 'route1':
                    break
                gl = L3[:, :, 0:4]
                el4 = bass.AP(Lall.tensor, Lall.offset + 4, [list(list(Lall.ap)[0]), [36, NTB], [8, 4], [1, 8]])
                gmax, gsum, v1, v2 = sm[:, 0:16], sm[:, 16:32], sm[:, 32:48], sm[:, 48:64]
                d21, ex, dn_ = sm[:, 64:80], sm[:, 80:96], sm[:, 96:112]
                goh = sm[:, 128:192].rearrange("p (b g) -> p b g", g=4)
                gex = sm[:, 192:256].rearrange("p (b g) -> p b g", g=4)
                pen = sm[:, 256:320].rearrange("p (b g) -> p b g", g=4)
                R.add('dve', lambda e: e.tensor_reduce(gmax, gl, AX.X, ALU.max), reads=[gl], writes=[gmax])
                R.tt(goh, gl, bcast(gmax, [[1, NTB], [0, 4]]), ALU.is_equal)
                R.tt(gex, gl, bcast(gmax, [[1, NTB], [0, 4]]), ALU.subtract)
                R.act(gex, gex, AF.Exp)
                R.add('dve', lambda e: e.tensor_reduce(gsum, gex, AX.X, ALU.add), reads=[gex], writes=[gsum])
                R.add('dve', lambda e: e.reciprocal(gsum, gsum), reads=[gsum], writes=[gsum])
                R.ts(pen, goh, 1e30, -1e30, ALU.mult, ALU.add)
                em = tmpA[0][:, :]
                em3 = em.rearrange("p (b e) -> p b e", e=32)
                R.tt(em.rearrange("p (b g e) -> p b g e", g=4, e=8), el4, bcast(sm[:, 256:320], [[4, NTB], [1, 4], [0, 8]]), ALU.add)
                R.add('dve', lambda e: e.tensor_reduce(v1, em3, AX.X, ALU.max), reads=[em], writes=[v1])
                R.tt(sel[:, :, 0, :], em3, bcast(v1, [[1, NTB], [0, 32]]), ALU.is_equal)
                em2 = tmpA[1][:, :]
                em23 = em2.rearrange("p (b e) -> p b e", e=32)
                R.stt(em23, sel[:, :, 0, :], -1e30, em3, ALU.mult, ALU.add)
                R.add('dve', lambda e: e.tensor_reduce(v2, em23, AX.X, ALU.max), reads=[em2], writes=[v2])
                R.tt(sel[:, :, 1, :], em23, bcast(v2, [[1, NTB], [0, 32]]), ALU.is_equal)
                R.tt(d21, v2, v1, ALU.subtract)
                R.act(ex, d21, AF.Exp)
                R.ts(dn_, ex, 1.0, None, ALU.add)
                R.add('dve', lambda e: e.reciprocal(dn_, dn_), reads=[dn_], writes=[dn_])
                R.tt(wts[:, :, 0], dn_, gsum, ALU.mult)
                R.tt(wts[:, :, 1], wts[:, :, 0], ex, ALU.mult)
                s12b3 = s12b.rearrange("p (b e) -> p b e", e=32)
                R.tt(s12b3, sel[:, :, 0, :], sel[:, :, 1, :], ALU.add)
                dump(f"sel{l}", sel[:, :, :, :], (128, NTB, 2, 32))
                dump(f"wts{l}", wts[:, :, :], (128, NTB, 2))
                if stop == 'route2':
                    break
                for tb in range(NTB):
                    R.mm(cnt_ps[:, tb * 32:(tb + 1) * 32], onesb, s12b3[:, tb, :])
                    R.mm(pos_ps[:, tb * 32:(tb + 1) * 32], tricb, s12b3[:, tb, :])
                cntb = tmpA[2][:, :].rearrange("p (b e) -> p b e", e=32)
                R.copy(cntb, cnt_ps[:, :].rearrange("p (b e) -> p b e", e=32))
                if stop == 'r3':
                    R.copy(tmpA[0][:, :], cnt_ps[:, :])
                    dump(f"c3_{l}", tmpA[0][:, :], (128, 512))
                    break
                R.memset(run[:, 0, :], 0.0)
                for b in range(NTB - 1):
                    R.tt(run[:, b + 1, :], run[:, b, :], cntb[:, b, :], ALU.add)
                ctot = sm[:, 0:32]
                R.tt(ctot, run[:, NTB - 1, :], cntb[:, NTB - 1, :], ALU.add)
                R.copy(posl, pos_ps[:, :].rearrange("p (b e) -> p b e", e=32))
                PS.unpin(cnt_ps)
                PS.unpin(pos_ps)
                cmpb = tmpA[1][:, 0:512].rearrange("p (e m) -> p e m", m=16)
                R.tt(cmpb, bcast(ctot, [[1, 32], [0, 16]]), bcast(cx[:, CX_THR:CX_THR + 16], [[0, 32], [1, 16]]), ALU.is_gt)
                blk = sm[:, 32:64]
                R.add('dve', lambda e, o=blk, i=cmpb: e.tensor_reduce(o, i, AX.X, ALU.add), reads=[tmpA[1][:, 0:512]], writes=[blk])
                pend = sm[:, 64:96]
                R.add('dve', lambda e: e.tensor_tensor_scan(pend, onesf[:, 0:32], blk, 0.0, ALU.mult, ALU.add),
                      reads=[onesf[:, 0:32], blk], writes=[pend])
                pst128 = sm[:, 96:128]
                R.tt(pst128, pend, blk, ALU.subtract)
                R.ts(pst128, pst128, 128.0, None, ALU.mult)
                R.tt(run, run, bcast(pst128, [[0, NTB], [1, 32]]), ALU.add)
                R.tt(posl, posl, run, ALU.add)
                ej = sm[:, 128:192]
                for jc in range(4):
                    cmpj = tmpA[0][:, 0:512].rearrange("p (j e) -> p j e", e=32)
                    R.tt(cmpj, bcast(cx[:, CX_IOTAJ + jc * 16:CX_IOTAJ + jc * 16 + 16], [[1, 16], [0, 32]]),
                         bcast(pend, [[0, 16], [1, 32]]), ALU.is_ge)
                    R.add('dve', lambda e, o=ej[:, jc * 16:(jc + 1) * 16], i=cmpj: e.tensor_reduce(o, i, AX.X, ALU.add),
                          reads=[tmpA[0][:, 0:512]], writes=[ej[:, jc * 16:(jc + 1) * 16]])
                oob = sm[:, 192:256]
                R.ts(oob, ej, 31.5, 1.0e6, ALU.is_gt, ALU.mult)
                R.ts(ej, ej, float(32 * l), 128.0, ALU.add, ALU.mult)
                R.tt(ej, ej, oob, ALU.add)
                R.ts(ej, ej, cx[:, CX_IOTAP:CX_IOTAP + 1], None, ALU.add)
                R.copy(idxw, ej)
                dump(f"ctot{l}", ctot, (128, 32))
                dump(f"ej{l}", ej, (128, 64))
                if stop == 'r4':
                    break
                for k in range(2):
                    tk = tmpA[k][:, :].rearrange("p (b e) -> p b e", e=32)
                    R.tt(tk, sel[:, :, k, :], posl, ALU.mult)
                    sf = sm[:, 320 + 16 * k:336 + 16 * k]
                    R.add('dve', lambda e, o=sf, i=tk: e.tensor_reduce(o, i, AX.X, ALU.add), reads=[tmpA[k][:, :]], writes=[sf])
                    R.copy(idx[:, :, k], sf)
                dump(f"idx{l}", idx[:, :, :], (128, NTB, 2), I32)
                dump(f"posl{l}", posl[:, :, :], (128, NTB, 32))
                if stop == 'route':
                    break
                for tb in range(NTB):
                    for k in range(2):
                        R.add('pool', lambda e, o=Xs.ap(), ix=idx[:, tb, k:k + 1], src=xn2[:, tb, :]: e.indirect_dma_start(
                            out=o, out_offset=bass.IndirectOffsetOnAxis(ap=ix, axis=0), in_=src, in_offset=None),
                            reads=[xn2[:, tb, :], idx[:, tb, k:k + 1]], writes=[("XsV", 0, 1, tb * 2 + k, tb * 2 + k + 1)], dma=True)
                AR.off = mark
                wbuf = [AR.alloc(WROW, BF16) for _ in range(3)]
                xg = [AR.alloc(DM, BF16) for _ in range(2)]
                xgT = [AR.alloc(8 * 128, BF16).rearrange("p (k r) -> p k r", r=128) for _ in range(2)]
                hact = AR.alloc(256, BF16)
                sgt = AR.alloc(256, F32)
                hTe = AR.alloc(256, BF16).rearrange("p (c r) -> p c r", r=128)
                ybuf = [AR.alloc(DM, BF16) for _ in range(2)]
                def issue_loads(jb):
                    R.add('pool', lambda e, o=wbuf[jb % 3], ix=idxw[:, jb:jb + 1]: gather_w(e, o, ix),
                          reads=[R.whole(Wall), idxw[:, jb:jb + 1]], writes=[wbuf[jb % 3]], dma=True)
                    R.dma('sp', xg[jb % 2], Xs[jb * 128:(jb + 1) * 128, :], extra_reads=[("XsV", 0, 1, 0, 1 << 30)])
                issue_loads(0)
                issue_loads(1)
                for jb in range(NBLK):
                    wb_, xg_, xgT_, yb_ = wbuf[jb % 3], xg[jb % 2], xgT[jb % 2], ybuf[jb % 2]
                    for half in range(2):
                        psb = PS.get().bitcast(BF16)
                        for i in range(4):
                            k = half * 4 + i
                            R.tr(psb[:, i * 128:(i + 1) * 128], xg_[:, k * 128:(k + 1) * 128], identb)
                        evac(xgT_[:, half * 4:half * 4 + 4, :], psb[:, 0:512].rearrange("p (a b) -> p a b", b=128))
                    psg = PS.get()
                    for k in range(8):
                        R.mm(psg[:, :], xgT_[:, k, :], wb_[:, k * 512:(k + 1) * 512], start=(k == 0), stop=(k == 7))
                    R.act(sgt, psg[:, 0:256], AF.Silu)
                    R.tt(hact, sgt, psg[:, 256:512], ALU.mult)
                    psb = PS.get().bitcast(BF16)
                    for c in range(2):
                        R.tr(psb[:, c * 128:(c + 1) * 128], hact[:, c * 128:(c + 1) * 128], identb)
                    evac(hTe, psb[:, 0:256].rearrange("p (a b) -> p a b", b=128))
                    for fh in range(2):
                        psy = PS.get()
                        for c in range(2):
                            R.mm(psy[:, :], hTe[:, c, :], wb_[:, 4096 + c * 1024 + fh * 512:4096 + c * 1024 + (fh + 1) * 512],
                                 start=(c == 0), stop=(c == 1))
                        evac(yb_[:, fh * 512:(fh + 1) * 512], psy[:, :])
                    R.dma('act', Yd[jb * 128:(jb + 1) * 128, :], yb_)
                    if jb + 2 < NBLK:
                        issue_loads(jb + 2)
                AR.off = mark
                yg = [[AR.alloc(DM, BF16) for _ in range(2)] for _ in range(3)]
                yacc = [AR.alloc(DM, F32) for _ in range(2)]
                for tb in range(NTB):
                    y1b, y2b = yg[tb % 3]
                    y1 = yacc[tb % 2]
                    for k, yy in enumerate((y1b, y2b)):
                        R.add('pool', lambda e, o=yy, ix=idx[:, tb, k:k + 1]: e.indirect_dma_start(
                            out=o, out_offset=None, in_=Yd.ap(), in_offset=bass.IndirectOffsetOnAxis(ap=ix, axis=0)),
                            reads=[R.whole(Yd), idx[:, tb, k:k + 1]], writes=[yy], dma=True)
                    R.act(y1, y1b, AF.Identity, scale=wts[:, tb, 0:1])
                    R.stt(y1, y2b, wts[:, tb, 1:2], y1, ALU.mult, ALU.add)
                    for half in range(2):
                        ps = PS.get()
                        for i in range(4):
                            k = half * 4 + i
                            R.tr(ps[:, i * 128:(i + 1) * 128], y1[:, k * 128:(k + 1) * 128], identf)
                        R.tt(hT[:, half * 4:half * 4 + 4, tb * 128:(tb + 1) * 128], hT[:, half * 4:half * 4 + 4, tb * 128:(tb + 1) * 128],
                             ps[:, :].rearrange("p (a b) -> p a b", b=128), ALU.add)
                dump(f"hmoe{l}", hT[:, :, 0:256], (128, 8, 256))
                if stop == 'moe':
                    break

                AR.reset()
                ptm = AR.alloc(NTB * 256, F32).rearrange("p (b c) -> p b c", c=256)
                pT = AR.alloc(2 * S, BF16).rearrange("p (c t) -> p c t", t=S)
                R.dma('sp', ptm, p_d[l, seq].rearrange("(b p) c -> p b c", p=128))
                for tb0 in range(0, NTB, 2):
                    ps = PS.get()
                    for c in range(2):
                        for tl in range(2):
                            R.tr(ps[:, (c * 2 + tl) * 128:(c * 2 + tl + 1) * 128], ptm[:, tb0 + tl, c * 128:(c + 1) * 128], identf)
                    evac(pT[:, :, tb0 * 128:(tb0 + 2) * 128], ps[:, :].rearrange("p (c t) -> p c t", t=256))
                norm_fm(16, l)
                for fo in range(8):
                    for g in range(NGR):
                        psg = PS.get()
                        for k in range(8):
                            wv = wgA if k < 4 else wgB
                            R.mm(psg[:, :], wv[:, k % 4, fo * 128:(fo + 1) * 128], hnT[:, k, g * GW:(g + 1) * GW],
                                 start=(k == 0), stop=(k == 7))
                        psp = PS.get()
                        for c in range(2):
                            R.mm(psp[:, :], wpp[:, c, fo * 128:(fo + 1) * 128], pT[:, c, g * GW:(g + 1) * GW],
                                 start=(c == 0), stop=(c == 1))
                        sg_ = tmpA[(fo * 4 + g) % 2]
                        R.act(sg_[:], psg[:, :], AF.Sigmoid)
                        R.tt(sg_[:], sg_[:], psp[:, :], ALU.mult)
                        R.tt(hT[:, fo, g * GW:(g + 1) * GW], hT[:, fo, g * GW:(g + 1) * GW], sg_[:], ALU.add, eng='pool')
                dump(f"hple{l}", hT[:, :, 0:256], (128, 8, 256))
            else:
                AR.reset()
                gfbc = AR.alloc(DM, F32)
                ot = [AR.alloc(DM, F32) for _ in range(2)]
                sm = AR.alloc(8, F32)
                R.dma('sp', gfbc, bass.AP(gfrow_d, 0, [[0, 128], [1, DM]]))
                for tb in range(NTB):
                    o_ = ot[tb % 2]
                    ssq = sm[:, 0:2]
                    pst = [PS.get(), PS.get()]
                    for half in range(2):
                        for i in range(4):
                            k = half * 4 + i
                            R.tr(pst[half][:, i * 128:(i + 1) * 128], hT[:, k, tb * 128:(tb + 1) * 128], identf)
                        R.act(tmpA[half][:], pst[half][:, :], AF.Square, accum_out=ssq[:, half:half + 1])
                    rs = sm[:, 2:3]
                    R.tt(rs, ssq[:, 0:1], ssq[:, 1:2], ALU.add)
                    R.act(rs, rs, AF.Ln, bias=epsc[:, 0:1], scale=1.0 / DM)
                    R.act(rs, rs, AF.Exp, scale=-0.5)
                    for half in range(2):
                        R.stt(o_[:, half * 512:(half + 1) * 512], pst[half][:, :], rs, gfbc[:, half * 512:(half + 1) * 512],
                              ALU.mult, ALU.mult)
                    R.dma('sp', out_d[seq, tb * 128:(tb + 1) * 128, :], o_)
                continue
            break
        toks = []
        for e in ('sp', 'pool', 'act'):
            toks += [(('d', e, s), 16 * c) for s, c in enumerate(R.slot_cnt[e]) if c > 0]
        R.wait_all('sp', toks)
        print("recorded ops:", {e: len(R.ops[e]) for e in R.ENG})
        R.emit()
    return nc, dbg_out


def prep_inputs(inputs, NLAYER=2):
    f = lambda a: np.ascontiguousarray(np.asarray(a, dtype=np.float32))
    wg, wu, wd = f(inputs["w_gate"]), f(inputs["w_up"]), f(inputs["w_down"])
    gu = np.concatenate([wg.reshape(2, 32, 8, 128, 256), wu.reshape(2, 32, 8, 128, 256)], axis=-1)
    gu = gu.transpose(0, 1, 3, 2, 4).reshape(2, 32, 128, 4096)
    dn = wd.reshape(2, 32, 2, 128, 1024).transpose(0, 1, 3, 2, 4).reshape(2, 32, 128, 2048)
    wall = np.ascontiguousarray(np.concatenate([gu, dn], axis=-1).reshape(2 * 32 * 128, WROW))
    rw = np.ascontiguousarray(np.concatenate([f(inputs["router_gw"]), f(inputs["router_ew"])], axis=-1))
    rb = np.ascontiguousarray(np.concatenate([f(inputs["router_gb"]), f(inputs["router_eb"])], axis=-1))
    vec = np.zeros((2, 128, NV), np.float32)
    for l in range(2):
        vec[l, :, 0:8] = f(inputs["norm1_g"])[l].reshape(8, 128).T
        vec[l, :, 8:16] = f(inputs["norm2_g"])[l].reshape(8, 128).T
        vec[l, :, 16:24] = f(inputs["ple_norm_g"])[l].reshape(8, 128).T
        vec[l, :, 24:32] = f(inputs["final_g"]).reshape(8, 128).T
        vec[l, :, 32:35] = f(inputs["sb_out_g"])[l].reshape(3, 128).T
        vec[l, 0:96, 35:39] = f(inputs["mnorm_g"])[l].reshape(4, 96).T
        vec[l, :, 39:41] = f(inputs["sgu_out_g"])[l].reshape(2, 128).T
        cw = f(inputs["conv_w"])[l].reshape(4, 2, 4, 96)
        vec[l, 0:96, 41:73] = cw.transpose(3, 1, 2, 0).reshape(96, 32)
        cbb = f(inputs["conv_b"])[l].reshape(2, 4, 96)
        vec[l, 0:96, 73:81] = cbb.transpose(2, 0, 1).reshape(96, 8)
        vec[l, 0:4, 81] = f(inputs["igate_b"])[l]
        vec[l, 0:4, 82] = f(inputs["fgate_b"])[l]
    sguln = np.ascontiguousarray(np.stack([f(inputs["sgu_ln_g"]), f(inputs["sgu_ln_b"])], axis=1))
    c, cx = make_consts()
    shared = dict(w_in=f(inputs["w_in"]), w_out=f(inputs["w_out"]), ple_gate_w=f(inputs["ple_gate_w"]),
                  ple_proj_w=f(inputs["ple_proj_w"]), wall_src=wall, rw=rw, rb=rb, vec=vec,
                  sgu_w=f(inputs["sgu_w"]), sgu_b=f(inputs["sgu_b"]), sgu_ln=sguln, g2row=f(inputs["norm2_g"]),
                  gfrow=f(inputs["final_g"]).reshape(1, DM), consts=c, constsx=cx)
    return shared


_CACHE = {}


def kernel(**inputs):
    NCORE = 8
    x = np.asarray(inputs["x"], dtype=np.float32)
    p = np.asarray(inputs["p"], dtype=np.float32)
    B = x.shape[0]
    nseq = B // NCORE
    shared = prep_inputs(inputs)
    if "nc" not in _CACHE:
        _CACHE["nc"] = build(NSEQ=nseq, NLAYER=2)[0]
    nc = _CACHE["nc"]
    in_maps = []
    for c in range(NCORE):
        m = dict(shared)
        m["x"] = np.ascontiguousarray(x[c * nseq:(c + 1) * nseq])
        m["p"] = np.ascontiguousarray(p[:, c * nseq:(c + 1) * nseq])
        in_maps.append(m)
    res = run_bass_kernel_spmd(nc, in_maps, core_ids=list(range(NCORE)))
    out = np.concatenate([np.asarray(r["out"]) for r in res.results], axis=0)
    return out.astype(np.float32)
```

```python
import contextlib
import numpy as np
import concourse.bass as bass
import concourse.mybir as mybir

F32 = mybir.dt.float32
BF16 = mybir.dt.bfloat16
I32 = mybir.dt.int32
U32 = mybir.dt.uint32
AF = mybir.ActivationFunctionType
ALU = mybir.AluOpType
AX = mybir.AxisListType


def _dsize(dt):
    return mybir.dt.size(dt)


class Rec:
    ENG = ['pe', 'act', 'dve', 'pool', 'sp']

    def __init__(self, nc, same_engine_sync=True):
        self.nc = nc
        self.ops = {e: [] for e in self.ENG}
        self.cnt = {e: 0 for e in self.ENG}
        self.waited = {e: {} for e in self.ENG}
        self.recs = {}
        self.nslot = {'sp': 8, 'pool': 8, 'act': 4}
        self.slot_cnt = {e: [0] * n for e, n in self.nslot.items()}
        self.slot_rr = {e: 0 for e in self.nslot}
        self.ses = same_engine_sync
        self.nops = 0

    def rng(self, ap):
        if isinstance(ap, tuple):
            return ap
        t = ap.tensor
        name = t.name
        sz = _dsize(ap.dtype)
        dims = list(ap.ap)
        if isinstance(t, bass.DRamTensorHandle):
            lo = ap.offset
            hi = lo + sum((c - 1) * abs(s) for s, c in dims) + 1
            return (name, 0, 1, lo * sz, hi * sz)
        pstride, pcount = dims[0]
        if pstride == 0:
            pstride = 1 << 40
        plo = ap.offset // pstride if pstride < (1 << 40) else 0
        flo = ap.offset - plo * pstride if pstride < (1 << 40) else ap.offset
        fhi = flo + sum((c - 1) * abs(s) for s, c in dims[1:]) + 1
        return (name, plo, plo + pcount, flo * sz, fhi * sz)

    def whole(self, t):
        return (t.name, 0, 1 << 20, 0, 1 << 60)

    def add(self, eng, fn, reads=(), writes=(), dma=False):
        deps = set()
        rr = [self.rng(a) for a in reads]
        ww = [self.rng(a) for a in writes]
        for (name, plo, phi, flo, fhi) in rr:
            for r in self.recs.get(name, ()):
                if r[4] and r[0] < phi and plo < r[1] and r[2] < fhi and flo < r[3]:
                    deps.add(r[5])
        for (name, plo, phi, flo, fhi) in ww:
            for r in self.recs.get(name, ()):
                if r[0] < phi and plo < r[1] and r[2] < fhi and flo < r[3]:
                    deps.add(r[5])
        waits = {}
        for (semkey, val) in deps:
            if semkey[0] == 'c' and semkey[1] == eng:
                if eng == 'pe' or not self.ses:
                    continue
            if self.waited[eng].get(semkey, 0) >= val:
                continue
            if waits.get(semkey, 0) < val:
                waits[semkey] = val
        if dma:
            s = self.slot_rr[eng]
            self.slot_rr[eng] = (s + 1) % self.nslot[eng]
            semkey = ('d', eng, s)
            prev = self.slot_cnt[eng][s]
            if prev > 0 and self.waited[eng].get(semkey, 0) < 16 * prev:
                if waits.get(semkey, 0) < 16 * prev:
                    waits[semkey] = 16 * prev
            self.slot_cnt[eng][s] = prev + 1
            token = (semkey, 16 * (prev + 1))
        else:
            self.cnt[eng] += 1
            token = (('c', eng), self.cnt[eng])
        for k, v in waits.items():
            self.waited[eng][k] = v
        self.ops[eng].append((waits, fn, token))
        self.nops += 1
        for (name, plo, phi, flo, fhi) in ww:
            lst = self.recs.setdefault(name, [])
            lst[:] = [r for r in lst if not (plo <= r[0] and r[1] <= phi and flo <= r[2] and r[3] <= fhi)]
            lst.append((plo, phi, flo, fhi, True, token))
        for (name, plo, phi, flo, fhi) in rr:
            lst = self.recs.setdefault(name, [])
            sk = token[0]
            lst[:] = [r for r in lst if not ((not r[4]) and r[5][0] == sk and r[0] == plo and r[1] == phi
                                             and r[2] == flo and r[3] == fhi)]
            lst.append((plo, phi, flo, fhi, False, token))
        return token

    def wait_all(self, eng, tokens):
        waits = {}
        for (semkey, val) in tokens:
            if waits.get(semkey, 0) < val:
                waits[semkey] = val
        for k, v in waits.items():
            self.waited[eng][k] = max(self.waited[eng].get(k, 0), v)
        self.ops[eng].append((waits, None, None))

    def emit(self):
        nc = self.nc
        needed = {e: set() for e in self.ENG}
        for e in self.ENG:
            for (waits, fn, token) in self.ops[e]:
                for (semkey, val) in waits.items():
                    if semkey[0] == 'c':
                        needed[semkey[1]].add(val)
        rank = {}
        for e in self.ENG:
            srt = sorted(needed[e])
            rank[e] = {v: i + 1 for i, v in enumerate(srt)}
        with contextlib.ExitStack() as st:
            sems = {}
            for e in self.ENG:
                sems[('c', e)] = st.enter_context(nc.semaphore(f"c_{e}"))
            for e, n in self.nslot.items():
                for s in range(n):
                    sems[('d', e, s)] = st.enter_context(nc.semaphore(f"d_{e}_{s}"))
            block = st.enter_context(nc.Block())
            rec = self

            def replay(e, eng):
                for (waits, fn, token) in rec.ops[e]:
                    for (semkey, val) in waits.items():
                        if semkey[0] == 'c':
                            val = rank[semkey[1]][val]
                        eng.wait_ge(sems[semkey], val)
                    if fn is None:
                        continue
                    inst = fn(eng)
                    semkey, val = token
                    if semkey[0] == 'd':
                        inst.then_inc(sems[semkey], 16)
                    elif val in rank[e]:
                        inst.then_inc(sems[semkey], 1)

            if self.ops['pe']:
                @block.tensor
                def _(eng):
                    replay('pe', eng)
            if self.ops['act']:
                @block.scalar
                def _(eng):
                    replay('act', eng)
            if self.ops['dve']:
                @block.vector
                def _(eng):
                    replay('dve', eng)
            if self.ops['pool']:
                @block.gpsimd
                def _(eng):
                    replay('pool', eng)
            if self.ops['sp']:
                @block.sync
                def _(eng):
                    replay('sp', eng)

    def dma(self, eng, out, in_, extra_reads=(), extra_writes=(), **kw):
        return self.add(eng, lambda e: e.dma_start(out=out, in_=in_, **kw),
                        reads=[in_] + list(extra_reads), writes=[out] + list(extra_writes), dma=True)

    def mm(self, out, lhsT, rhs, start=True, stop=True, **kw):
        return self.add('pe', lambda e: e.matmul(out, lhsT, rhs, start=start, stop=stop, **kw),
                        reads=[lhsT, rhs], writes=[out])

    def tr(self, out, in_, ident):
        return self.add('pe', lambda e: e.transpose(out, in_, ident), reads=[in_, ident], writes=[out])

    def act(self, out, in_, func, bias=None, scale=None, accum_out=None, eng='act'):
        reads = [in_]
        kw = {}
        if bias is not None:
            kw['bias'] = bias
            if not isinstance(bias, (int, float)):
                reads.append(bias)
        if scale is not None:
            kw['scale'] = scale
            if not isinstance(scale, (int, float)):
                reads.append(scale)
        writes = [out]
        if accum_out is not None:
            kw['accum_out'] = accum_out
            writes.append(accum_out)
        return self.add(eng, lambda e: e.activation(out, in_, func, **kw), reads=reads, writes=writes)

    def tt(self, out, in0, in1, op, eng='dve'):
        return self.add(eng, lambda e: e.tensor_tensor(out, in0, in1, op), reads=[in0, in1], writes=[out])

    def ts(self, out, in0, s1, s2, op0, op1=None, eng='dve', accum_out=None):
        reads = [in0]
        for s in (s1, s2):
            if s is not None and not isinstance(s, (int, float)):
                reads.append(s)
        writes = [out]
        kw = {}
        if accum_out is not None:
            kw['accum_out'] = accum_out
            writes.append(accum_out)
        if op1 is None:
            return self.add(eng, lambda e: e.tensor_scalar(out, in0, s1, None, op0, **kw), reads=reads, writes=writes)
        return self.add(eng, lambda e: e.tensor_scalar(out, in0, s1, s2, op0, op1, **kw), reads=reads, writes=writes)

    def stt(self, out, in0, scalar, in1, op0, op1, eng='dve'):
        reads = [in0, in1]
        if not isinstance(scalar, (int, float)):
            reads.append(scalar)
        return self.add(eng, lambda e: e.scalar_tensor_tensor(out, in0, scalar, in1, op0, op1), reads=reads, writes=[out])

    def copy(self, out, in_, eng='dve'):
        if eng == 'act':
            return self.add('act', lambda e: e.copy(out, in_), reads=[in_], writes=[out])
        return self.add(eng, lambda e: e.tensor_copy(out, in_), reads=[in_], writes=[out])

    def memset(self, ap, val, eng='dve'):
        return self.add(eng, lambda e: e.memset(ap, val), writes=[ap])


from concourse.bass_utils import run_bass_kernel_spmd

S = 2048
DM = 1024
NIN = 3208
NTB = 16
NGR = 4
GW = 512
OFF = dict(qa=0, ka=384, va=768, qb=1152, kb=1536, vb=1920, ob=2304, ib=2688, fb=2692, uc=2696, vc=2952)
NBLK = 64
WROW = 6144
EPS = 1e-6
NV = 96
C_ID = 0
C_ONE = 128
C_TRIS = 256
C_TRIC = 384
C_MSB = 512
C_MML = 512 + 2048
C_END = 512 + 4096
CX_IOTAP = 0
CX_IOTAJ = 1
CX_THR = 65
CX_SEL4 = 81
CX_SELR = 81 + 512
CX_END = 81 + 512 + 96


def make_consts():
    c = np.zeros((128, C_END), np.float32)
    c[:, C_ID:C_ID + 128] = np.eye(128)
    c[:, C_ONE:C_ONE + 128] = 1.0
    j = np.arange(128)[:, None]
    s = np.arange(128)[None, :]
    c[:, C_TRIS:C_TRIS + 128] = (j >= s)
    c[:, C_TRIC:C_TRIC + 128] = (j < s)
    for jj in range(4):
        m_sb = np.zeros((128, 4, 128), np.float32)
        m_ml = np.zeros((128, 4, 128), np.float32)
        for n in range(4):
            if n > jj:
                m_sb[:, n, :] = 1.0
                m_ml[:, n, :] = 1.0
            elif n == jj:
                m_sb[:, n, :] = (j < s)
                m_ml[:, n, :] = (j <= s)
        c[:, C_MSB + jj * 512:C_MSB + (jj + 1) * 512] = m_sb.reshape(128, 512)
        c[:, C_MML + jj * 512:C_MML + (jj + 1) * 512] = m_ml.reshape(128, 512)
    cx = np.zeros((128, CX_END), np.float32)
    cx[:, CX_IOTAP] = np.arange(128)
    cx[:, CX_IOTAJ:CX_IOTAJ + 64] = np.arange(64)[None, :]
    cx[:, CX_THR:CX_THR + 16] = (np.arange(16) * 128)[None, :]
    for h in range(4):
        cx[h, CX_SEL4 + h * 128:CX_SEL4 + (h + 1) * 128] = 1.0
    cx[96, CX_SELR:CX_SELR + 96] = 1.0
    return c, cx


class PSM:
    def __init__(self, banks):
        self.banks = banks
        self.pinned = set()
        self.i = 0

    def _next(self):
        for _ in range(16):
            b = self.i
            self.i = (self.i + 1) % len(self.banks)
            if b not in self.pinned:
                return b
        raise RuntimeError("no psum bank")

    def get(self):
        return self.banks[self._next()]

    def try_get(self):
        if len(self.pinned) >= len(self.banks):
            return None
        return self.banks[self._next()]

    def pin(self):
        b = self._next()
        self.pinned.add(b)
        return self.banks[b]

    def unpin(self, bank):
        self.pinned.discard(self.banks.index(bank))


def run_pipeline(groups):
    timeline = {}
    t0 = 0
    for gi, (n_r, fn, ov) in enumerate(groups):
        for r in range(n_r):
            timeline.setdefault(t0 + r, []).append((gi, r))
        t0 = t0 + n_r - ov
    for t in sorted(timeline):
        for (gi, r) in timeline[t]:
            groups[gi][1](r)


def bcast(ap, dims):
    p = list(ap.ap)[0]
    return bass.AP(ap.tensor, ap.offset, [[p[0], p[1]]] + [list(d) for d in dims])


SKIPMIX = False


def build(NSEQ=4, NLAYER=2, dbg=None, stop=None):
    nc = bass.Bass("TRN2", target_bir_lowering=False)
    R = Rec(nc)
    dt = nc.dram_tensor
    x_d = dt("x", [NSEQ, S, DM], F32, kind="ExternalInput")
    p_d = dt("p", [2, NSEQ, S, 256], F32, kind="ExternalInput")
    win_d = dt("w_in", [2, DM, NIN], F32, kind="ExternalInput")
    wout_d = dt("w_out", [2, DM, DM], F32, kind="ExternalInput")
    pgw_d = dt("ple_gate_w", [2, DM, DM], F32, kind="ExternalInput")
    ppw_d = dt("ple_proj_w", [2, 256, DM], F32, kind="ExternalInput")
    wall_d = dt("wall_src", [2 * 32 * 128, WROW], F32, kind="ExternalInput")
    rw_d = dt("rw", [2, DM, 36], F32, kind="ExternalInput")
    rb_d = dt("rb", [2, 36], F32, kind="ExternalInput")
    vec_d = dt("vec", [2, 128, NV], F32, kind="ExternalInput")
    sguw_d = dt("sgu_w", [2, 4, 128, 128], F32, kind="ExternalInput")
    sgub_d = dt("sgu_b", [2, 4, 128], F32, kind="ExternalInput")
    sguln_d = dt("sgu_ln", [2, 2, 256], F32, kind="ExternalInput")
    g2row_d = dt("g2row", [2, DM], F32, kind="ExternalInput")
    gfrow_d = dt("gfrow", [1, DM], F32, kind="ExternalInput")
    c_d = dt("consts", [128, C_END], F32, kind="ExternalInput")
    cx_d = dt("constsx", [128, CX_END], F32, kind="ExternalInput")
    out_d = dt("out", [NSEQ, S, DM], F32, kind="ExternalOutput")
    Xs = dt("Xs", [NBLK * 128, DM], BF16, kind="Internal")
    Yd = dt("Yd", [NBLK * 128, DM], BF16, kind="Internal")
    Wall = dt("Wall", [2 * 32 * 128, WROW], BF16, kind="Internal")
    dbg_out = {}

    st = contextlib.ExitStack()
    with st:
        def sb(name, shape, dtp):
            return st.enter_context(nc.sbuf_tensor(name, shape, dtp))
        hT = sb("hT", [128, 8, S], F32)
        hnT = sb("hnT", [128, 8, S], BF16)
        NSLAB = 4
        slabs = [sb(f"slab{i}", [128, 4096], BF16) for i in range(NSLAB)]
        cb = sb("cb", [128, C_END], BF16)
        cf = sb("cf", [128, 256], F32)
        cx = sb("cx", [128, CX_END], F32)
        vecs = [sb(f"vec{l}", [128, NV], F32) for l in range(2)]
        rstd_bc = sb("rstd_bc", [128, S], F32)
        tmpA = [sb(f"tmpA{i}", [128, 512], F32) for i in range(3)]
        epsc = sb("epsc", [128, 1], F32)
        AR_BYTES = 52 * 1024
        arena = sb("arena", [128, AR_BYTES // 4], F32)
        arena_b = arena.bitcast(BF16)
        arena_i = arena.bitcast(I32)
        banks = [st.enter_context(nc.psum_tensor(f"ps{i}", [128, 512], F32)) for i in range(8)]
        PS = PSM(banks)
        print("sbuf remaining", nc.sbuf_bytes_remaining)

        identf = cf[:, 0:128]
        onesf = cf[:, 128:256]
        identb = cb[:, C_ID:C_ID + 128]
        onesb = cb[:, C_ONE:C_ONE + 128]
        trisb = cb[:, C_TRIS:C_TRIS + 128]
        tricb = cb[:, C_TRIC:C_TRIC + 128]

        class Arena:
            def __init__(self):
                self.off = 0

            def reset(self):
                self.off = 0

            def alloc(self, n, dtp):
                sz = 4 if dtp in (F32, I32) else 2
                nbytes = (n * sz + 31) // 32 * 32
                assert self.off + nbytes <= AR_BYTES, (self.off, nbytes)
                start = self.off // sz
                self.off += nbytes
                h = arena if dtp == F32 else (arena_i if dtp == I32 else arena_b)
                return h[:, start:start + n]
        AR = Arena()

        slab_i = [0]

        def next_slab():
            s = slabs[slab_i[0] % NSLAB]
            slab_i[0] += 1
            return s

        def dump(name, ap, shape, dtp=F32):
            if dbg is None or name not in dbg:
                return
            d = dt("dbg_" + name, list(shape), dtp, kind="ExternalOutput")
            dbg_out[name] = d
            R.dma('sp', d.ap(), ap)

        flip = [0]
        _breg = {}

        def gather_w(e, o, ix):
            reg = e.to_reg(2 * 32 * 128 - 1)
            inst = e.indirect_dma_start(out=o, out_offset=None, in_=Wall.ap(),
                                        in_offset=bass.IndirectOffsetOnAxis(ap=ix, axis=0),
                                        bounds_check=reg, oob_is_err=False)
            e.free_register(reg)
            return inst

        def pe_warm(n=24):
            bank = PS.get()
            for _ in range(n):
                R.mm(bank[:, :], onesb, cb[:, C_MSB:C_MSB + 512], start=True, stop=True)

        def evac(out, in_, scale=None):
            flip[0] ^= 1
            if scale is not None:
                R.act(out, in_, AF.Copy, scale=scale)
            elif flip[0]:
                R.act(out, in_, AF.Copy)
            else:
                R.copy(out, in_)

        R.dma('pool', cb[:], c_d.ap())
        R.dma('sp', cf[:], c_d[:, 0:256])
        R.dma('sp', cx[:], cx_d.ap())
        for l in range(2):
            R.dma('sp', vecs[l][:], vec_d[l])
        R.memset(epsc[:], EPS)

        wall_jobs = list(range(NLAYER * 32)) if (stop is None or stop in ('moe', 'ple', 'all', 'route')) else []

        def wall_tick(n=1):
            for _ in range(n):
                if not wall_jobs:
                    return
                i = wall_jobs.pop(0)
                R.dma('pool', Wall[i * 128:(i + 1) * 128, :], wall_d[i * 128:(i + 1) * 128, :], max_dma_last_dim=4096)
        if stop is None or stop in ('moe', 'ple', 'all', 'route'):
            AR.reset()
            zt = AR.alloc(4096, BF16)
            R.memset(zt, 0.0)
            for i in range(NBLK * 128 // 512):
                R.dma('sp', Xs[i * 512:(i + 1) * 512, :].rearrange("(a p) d -> p a d", p=128),
                      zt.rearrange("p (a d) -> p a d", d=1024), extra_writes=[("XsV", 0, 1, 0, 1 << 30)])

        def norm_fm(gcol, l, src=None):
            for g in range(NGR):
                ps = PS.get()
                for k in range(8):
                    sq = tmpA[k % 2]
                    R.act(sq[:], hT[:, k, g * GW:(g + 1) * GW], AF.Square)
                    R.mm(ps[:, :], onesf, sq[:], start=(k == 0), stop=(k == 7))
                t = tmpA[2]
                R.act(t[:], ps[:, :], AF.Ln, bias=epsc[:, 0:1], scale=1.0 / DM)
                R.act(rstd_bc[:, g * GW:(g + 1) * GW], t[:], AF.Exp, scale=-0.5)
                for k in range(8):
                    R.stt(hnT[:, k, g * GW:(g + 1) * GW], hT[:, k, g * GW:(g + 1) * GW],
                          vecs[l][:, gcol + k:gcol + k + 1], rstd_bc[:, g * GW:(g + 1) * GW], ALU.mult, ALU.mult)

        def load_in_cols(slab, dst_col, l, col0, ncols, width):
            v = slab[:, 0:8 * width].rearrange("p (k n) -> p k n", n=width)
            src = win_d[l].rearrange("(k p) n -> p k n", p=128)[:, :, col0:col0 + ncols]
            R.dma('pool', v[:, :, dst_col:dst_col + ncols], src)
            return v

        def proj_fm(wv, c0, M, out_fn):
            for g in range(NGR):
                ps = PS.get()
                for k in range(8):
                    R.mm(ps[0:M, :], wv[:, k, c0:c0 + M], hnT[:, k, g * GW:(g + 1) * GW], start=(k == 0), stop=(k == 7))
                out_fn(g, ps)

        def proj_tm(wv, c0, N, out_fn):
            per = 4 if N <= 128 else 1
            for tb0 in range(0, NTB, per):
                ps = PS.get()
                for i in range(per):
                    tb = tb0 + i
                    for k in range(8):
                        R.mm(ps[:, i * N:(i + 1) * N], hnT[:, k, tb * 128:(tb + 1) * 128], wv[:, k, c0:c0 + N],
                             start=(k == 0), stop=(k == 7))
                out_fn(tb0, per, ps)

        def outproj_add(wv, kparts, rhs_fn):
            nk = len(kparts)
            for fo in range(8):
                for g in range(NGR):
                    ps = PS.get()
                    for j, K in enumerate(kparts):
                        R.mm(ps[:, :], wv[0:K, j, fo * 128:(fo + 1) * 128], rhs_fn(j, g), start=(j == 0), stop=(j == nk - 1))
                    R.tt(hT[:, fo, g * GW:(g + 1) * GW], hT[:, fo, g * GW:(g + 1) * GW], ps[:, :], ALU.add)

        for seq in range(NSEQ):
            AR.reset()
            xts = [AR.alloc(1024, F32) for _ in range(2)]
            for tb in range(NTB):
                xt = xts[tb % 2]
                R.dma('sp', xt, x_d[seq, tb * 128:(tb + 1) * 128, :])
                for half in range(2):
                    ps = PS.get()
                    for i in range(4):
                        k = half * 4 + i
                        R.tr(ps[:, i * 128:(i + 1) * 128], xt[:, k * 128:(k + 1) * 128], identf)
                    evac(hT[:, half * 4:half * 4 + 4, tb * 128:(tb + 1) * 128],
                         ps[:, :].rearrange("p (a b) -> p a b", b=128))

            for l in range(NLAYER):
                V = vecs[l]
                norm_fm(0, l)
                dump(f"hn{l}", hnT[:, 0, 0:512], (128, 512), BF16)
                if stop == 'norm1':
                    break
                if not SKIPMIX:
                    AR.reset()
                    yaT = AR.alloc(3 * S, BF16).rearrange("p (j t) -> p j t", t=S)
                    qT2 = [AR.alloc(S, BF16) for _ in range(2)]
                    kT2 = [AR.alloc(S, BF16) for _ in range(2)]
                    vtm2 = [AR.alloc(NTB * 128, BF16).rearrange("p (b c) -> p b c", c=128) for _ in range(2)]
                    e_t = [[AR.alloc(512, BF16) for _ in range(2)] for _ in range(2)]
                    sp_t = [[AR.alloc(512, BF16) for _ in range(2)] for _ in range(2)]
                    p_t = [[AR.alloc(512, BF16) for _ in range(2)] for _ in range(2)]
                    a_t = [[AR.alloc(512, BF16) for _ in range(2)] for _ in range(2)]
                    wA = [None, None, None]

                    def loadA(j):
                        sl = next_slab()
                        load_in_cols(sl, 0, l, OFF['qa'] + j * 128, 128, 384)
                        load_in_cols(sl, 128, l, OFF['ka'] + j * 128, 128, 384)
                        wA[j] = load_in_cols(sl, 256, l, OFF['va'] + j * 128, 128, 384)
                    loadA(0)

                    def proj_chunks(jn):
                        wvn = wA[jn]
                        qn, kn, vn = qT2[jn % 2], kT2[jn % 2], vtm2[jn % 2]
                        chunks = []
                        for g in range(NGR):
                            def cq(ps, g=g):
                                for k in range(8):
                                    R.mm(ps[:, :], wvn[:, k, 0:128], hnT[:, k, g * GW:(g + 1) * GW], start=(k == 0), stop=(k == 7))
                                R.ts(qn[:, g * GW:(g + 1) * GW], ps[:, :], 0.125, None, ALU.mult)

                            def ck(ps, g=g):
                                for k in range(8):
                                    R.mm(ps[:, :], wvn[:, k, 128:256], hnT[:, k, g * GW:(g + 1) * GW], start=(k == 0), stop=(k == 7))
                                R.copy(kn[:, g * GW:(g + 1) * GW], ps[:, :])

                            def cv(ps, g=g):
                                for i in range(4):
                                    tb = 4 * g + i
                                    for k in range(8):
                                        R.mm(ps[:, i * 128:(i + 1) * 128], hnT[:, k, tb * 128:(tb + 1) * 128], wvn[:, k, 256:384],
                                             start=(k == 0), stop=(k == 7))
                                R.copy(vn[:, 4 * g:4 * g + 4, :], ps[:, :].rearrange("p (a b) -> p a b", b=128))
                            chunks += [cq, ck, cv]
                        return chunks
                    for c_ in proj_chunks(0):
                        c_(PS.get())
                    for j in range(3):
                        qT, kT, vtm = qT2[j % 2], kT2[j % 2], vtm2[j % 2]
                        pendA = []
                        if j + 1 < 3:
                            loadA(j + 1)
                            pendA = proj_chunks(j + 1)
                        groupsA = []
                        for G in range(NGR):
                            def mk(G=G, j=j):
                                nb = 4 * G + 4
                                its = list(range(nb - 1, -1, -1))
                                n = len(its)
                                stt_ = dict(accs=None, crs=None, psz={})

                                def c0f(i):
                                    return max(0, its[i] - 4 * G) * 128

                                def sZ(i):
                                    b = its[i]
                                    c0 = c0f(i)
                                    for hp in range(2):
                                        pl, ph = hp * 64, hp * 64 + 64
                                        psz = PS.pin()
                                        stt_['psz'][(i, hp)] = psz
                                        R.mm(psz[:, c0:512], kT[pl:ph, b * 128:(b + 1) * 128], qT[pl:ph, G * GW + c0:(G + 1) * GW])

                                def sEL(i):
                                    b = its[i]
                                    c0 = c0f(i)
                                    for hp in range(2):
                                        et = e_t[hp][i % 2][:, c0:512]
                                        pz_ = stt_['psz'].pop((i, hp))
                                        R.act(et, pz_[:, c0:512], AF.Exp)
                                        PS.unpin(pz_)
                                        if b >= 4 * G:
                                            jj = b - 4 * G
                                            R.tt(et, et, cb[:, C_MSB + jj * 512 + c0:C_MSB + (jj + 1) * 512], ALU.mult)
                                    for hp in range(2):
                                        et, spt = e_t[hp][i % 2][:, c0:512], sp_t[hp][i % 2][:, c0:512]
                                        R.act(spt, et, AF.Ln, bias=1.0)

                                def sT(i):
                                    if stt_['accs'] is None:
                                        stt_['accs'] = [PS.pin(), PS.pin()]
                                        stt_['crs'] = [PS.pin(), PS.pin()]
                                    c0 = c0f(i)
                                    for hp in range(2):
                                        R.mm(stt_['crs'][hp][:, c0:512], trisb, sp_t[hp][i % 2][:, c0:512], start=(i == 0), stop=False)

                                def sX(i):
                                    c0 = c0f(i)
                                    for hp in range(2):
                                        R.act(p_t[hp][i % 2][:, c0:512], stt_['crs'][hp][:, c0:512], AF.Exp, scale=-1.0)
                                        R.tt(a_t[hp][i % 2][:, c0:512], e_t[hp][i % 2][:, c0:512], p_t[hp][i % 2][:, c0:512], ALU.mult)

                                def sC(i):
                                    c0 = c0f(i)
                                    if i < n - 1:
                                        for hp in range(2):
                                            R.mm(stt_['crs'][hp][:, c0:512], tricb, sp_t[hp][i % 2][:, c0:512], start=False, stop=False)

                                def sA(i):
                                    b = its[i]
                                    c0 = c0f(i)
                                    for hp in range(2):
                                        pl, ph = hp * 64, hp * 64 + 64
                                        R.mm(stt_['accs'][hp][pl:ph, c0:512], vtm[:, b, pl:ph], a_t[hp][i % 2][:, c0:512],
                                             start=(i == 0), stop=(i == n - 1))

                                def rnd(r):
                                    if r % 2 == 0:
                                        wall_tick(1)
                                    if 0 <= r - 3 < n:
                                        sC(r - 3)
                                    if 0 <= r - 2 < n:
                                        sT(r - 2)
                                    if r < n:
                                        sZ(r)
                                    if 0 <= r - 3 < n:
                                        sA(r - 3)
                                    if 0 <= r - 1 < n:
                                        sEL(r - 1)
                                    if 0 <= r - 2 < n:
                                        sX(r - 2)
                                    if pendA and r % 2 == 1 and r < n:
                                        bk_ = PS.try_get()
                                        if bk_ is not None:
                                            pendA.pop(0)(bk_)
                                    if r == n + 2:
                                        for hp in range(2):
                                            pl, ph = hp * 64, hp * 64 + 64
                                            evac(yaT[pl:ph, j, G * GW:(G + 1) * GW], stt_['accs'][hp][pl:ph, :])
                                        for bk in stt_['accs'] + stt_['crs']:
                                            PS.unpin(bk)
                                return (n + 3, rnd, 2)
                            groupsA.append(mk())
                        run_pipeline(groupsA)
                        while pendA:
                            pendA.pop(0)(PS.get())
                    dump(f"yaT{l}", yaT[:, :, :], (128, 3, S), BF16)
                    if stop == 'attnA':
                        break
                    slo = next_slab()
                    wo = slo[:, 0:3 * 1024].rearrange("p (j n) -> p j n", n=1024)
                    R.dma('pool', wo, wout_d[l, 0:384, :].rearrange("(j p) n -> p j n", p=128))
                    for g in range(NGR):
                        ps = PS.get()
                        for j in range(3):
                            sq = tmpA[j % 2]
                            R.act(sq[:], yaT[:, j, g * GW:(g + 1) * GW], AF.Square)
                            R.mm(ps[:, :], onesf, sq[:], start=(j == 0), stop=(j == 2))
                        t = tmpA[2]
                        R.act(t[:], ps[:, :], AF.Ln, bias=epsc[:, 0:1], scale=1.0 / 384)
                        R.act(rstd_bc[:, g * GW:(g + 1) * GW], t[:], AF.Exp, scale=-0.5)
                        for j in range(3):
                            R.stt(yaT[:, j, g * GW:(g + 1) * GW], yaT[:, j, g * GW:(g + 1) * GW],
                                  V[:, 32 + j:33 + j], rstd_bc[:, g * GW:(g + 1) * GW], ALU.mult, ALU.mult)
                    dump(f"yanT{l}", yaT[:, :, :], (128, 3, S), BF16)
                    outproj_add(wo, [128, 128, 128], lambda j, g: yaT[:, j, g * GW:(g + 1) * GW])
                    if stop == 'outA':
                        break

                    AR.reset()
                    t1 = AR.alloc(S, F32)
                    AR.off -= S * 4
                    ybT = AR.alloc(4 * S, BF16).rearrange("p (h t) -> p h t", t=S)
                    Bf = rstd_bc
                    utm = AR.alloc(NTB * 4, F32).rearrange("p (b h) -> p b h", h=4)
                    nfb = AR.alloc(8, F32)[0:4, 0:1]
                    qb = AR.alloc(S, BF16)
                    kb = AR.alloc(S, BF16)
                    sgo = AR.alloc(S, BF16)
                    vbt = AR.alloc(NTB * 97, BF16).rearrange("p (b c) -> p b c", c=97)
                    R.memset(vbt[:, :, 96:97], 1.0)
                    numS = AR.alloc(GW, F32)
                    raw = [AR.alloc(GW + 4, BF16) for _ in range(3)]
                    dgw = AR.alloc(8 * 96, BF16).rearrange("p (m c) -> p m c", c=96)
                    d_t = [AR.alloc(512, BF16) for _ in range(3)]
                    w_t = [AR.alloc(512, BF16) for _ in range(2)]
                    slg = next_slab()
                    wg_ = load_in_cols(slg, 0, l, OFF['ib'], 8, 8)
                    R.ts(nfb, V[0:4, 82:83], -1.0, None, ALU.mult)
                    proj_fm(wg_, 4, 4, lambda g, ps: R.act(t1[0:4, g * GW:(g + 1) * GW], ps[0:4, :], AF.Exp, bias=nfb, scale=-1.0))
                    R.act(t1[0:4, :], t1[0:4, :], AF.Ln, bias=1.0)
                    R.add('dve', lambda e: e.tensor_tensor_scan(Bf[0:4, :], bcast(onesf[0:4, 0:1], [[0, S]]), t1[0:4, :], 0.0,
                                                                 ALU.mult, ALU.subtract),
                          reads=[onesf[0:4, 0:1], t1[0:4, :]], writes=[Bf[0:4, :]])
                    LNS = float(np.log(96.0 ** -0.5))

                    def u_out(g, ps):
                        uu = tmpA[0][0:4, :]
                        R.stt(uu, ps[0:4, :], V[0:4, 81:82], Bf[0:4, g * GW:(g + 1) * GW], ALU.add, ALU.subtract)
                        R.ts(uu, uu, LNS, None, ALU.add)
                        ps2 = PS.get()
                        for i in range(4):
                            R.tr(ps2[:, i * 4:(i + 1) * 4], uu[:, i * 128:(i + 1) * 128], identf[0:4, 0:4])
                        R.copy(utm[:, 4 * g:4 * g + 4, :], ps2[:, 0:16].rearrange("p (a b) -> p a b", b=4))
                    proj_fm(wg_, 0, 4, u_out)
                    dump(f"Bf{l}", Bf[0:4, :], (4, S))
                    dump(f"utm{l}", utm[:, :, :], (128, NTB, 4))
                    wB = [None] * 4

                    def loadB(h):
                        sl = next_slab()
                        load_in_cols(sl, 0, l, OFF['qb'] + h * 96, 96, 384)
                        load_in_cols(sl, 96, l, OFF['kb'] + h * 96, 96, 384)
                        load_in_cols(sl, 192, l, OFF['vb'] + h * 96, 96, 384)
                        wB[h] = load_in_cols(sl, 288, l, OFF['ob'] + h * 96, 96, 384)
                    loadB(0)
                    for h in range(4):
                        wv = wB[h]
                        if h + 1 < 4:
                            loadB(h + 1)
                        for qk in range(2):
                            cw = 41 + qk * 16 + h * 4
                            for tap in range(4):
                                R.ts(dgw[0:96, qk * 4 + tap, :], identf[0:96, 0:96], V[0:96, cw + tap:cw + tap + 1], None, ALU.mult)
                        for qk in range(2):
                            cbias = 73 + qk * 4 + h
                            dst = qb if qk == 0 else kb
                            R.memset(raw[0][0:96, 1:4], 0.0)

                            def conv_out(g, ps, qk=qk, cbias=cbias, dst=dst):
                                rw_ = raw[g % 3]
                                R.act(rw_[0:96, 4:4 + GW], ps[0:96, :], AF.Copy)
                                if g + 1 < NGR:
                                    R.copy(raw[(g + 1) % 3][0:96, 1:4], rw_[0:96, GW + 1:GW + 4])
                                pc = PS.get()
                                for tap in range(4):
                                    R.mm(pc[0:96, :], dgw[0:96, qk * 4 + tap, :], rw_[0:96, 1 + tap:1 + tap + GW],
                                         start=(tap == 0), stop=(tap == 3))
                                R.act(dst[0:96, g * GW:(g + 1) * GW], pc[0:96, :], AF.Silu, bias=V[0:96, cbias:cbias + 1])
                            proj_fm(wv, qk * 96, 96, conv_out)
                        if h == 0:
                            dump(f"qb0_{l}", qb[0:96, :], (96, S), BF16)
                        proj_tm(wv, 192, 96, lambda tb0, per, ps: evac(
                            vbt[:, tb0:tb0 + per, 0:96], ps[:, 0:per * 96].rearrange("p (a b) -> p a b", b=96)))
                        proj_fm(wv, 288, 96, lambda g, ps: R.act(sgo[0:96, g * GW:(g + 1) * GW], ps[0:96, :], AF.Sigmoid))
                        groupsB = []
                        pend_epi = []
                        for G in range(NGR):
                            def mkB(G=G, h=h):
                                nb = 4 * G + 4
                                n = nb
                                stb = dict(psB=None, num=None, den=None, pss={})

                                def sS(i):
                                    b = i
                                    if stb['psB'] is None:
                                        stb['psB'] = PS.pin()
                                        R.mm(stb['psB'][:, :], cx[0:4, CX_SEL4 + h * 128:CX_SEL4 + (h + 1) * 128], Bf[0:4, G * GW:(G + 1) * GW])
                                    pss = PS.get()
                                    stb['pss'][i] = pss
                                    c0 = max(0, b - 4 * G) * 128
                                    R.mm(pss[:, c0:512], kb[0:96, b * 128:(b + 1) * 128], qb[0:96, G * GW + c0:(G + 1) * GW])
                                    dt_ = d_t[i % 3][:, c0:512]
                                    R.act(dt_, stb['psB'][:, c0:512], AF.Exp, bias=utm[:, b, h:h + 1])
                                    if b >= 4 * G:
                                        jj = b - 4 * G
                                        R.tt(dt_, dt_, cb[:, C_MML + jj * 512 + c0:C_MML + (jj + 1) * 512], ALU.mult)
                                    if i == n - 1:
                                        PS.unpin(stb['psB'])

                                def sW(i):
                                    c0 = max(0, i - 4 * G) * 128
                                    R.tt(w_t[i % 2][:, c0:512], stb['pss'].pop(i)[:, c0:512], d_t[i % 3][:, c0:512], ALU.mult)

                                def sN(i):
                                    b = i
                                    c0 = max(0, i - 4 * G) * 128
                                    if stb['num'] is None:
                                        stb['num'] = PS.pin()
                                    R.mm(stb['num'][0:97, c0:512], vbt[:, b, :], w_t[i % 2][:, c0:512], start=(i == 0), stop=(i == n - 1))

                                def epi_steps():
                                    num = stb['num']
                                    dn, hh, sq = tmpA[0], tmpA[1], tmpA[2]
                                    box = {}

                                    def s0():
                                        R.act(numS[0:97, :], num[0:97, :], AF.Copy)
                                        PS.unpin(num)

                                    def s1():
                                        box['den'] = PS.get()
                                        R.mm(box['den'][0:96, :], cx[0:97, CX_SELR:CX_SELR + 96], numS[0:97, :])

                                    def s5():
                                        R.tt(hh[0:96, :], numS[0:96, :], dn[0:96, :], ALU.mult)
                                        if h == 0 and G == 0:
                                            dump(f"hb00_{l}", hh[0:96, :], (96, 512))

                                    def s7():
                                        box['ps'] = PS.get()
                                        R.mm(box['ps'][0:96, :], onesf[0:96, 0:96], sq[0:96, :])
                                    return [
                                        s0,
                                        s1,
                                        lambda: R.act(dn[0:96, :], box['den'][0:96, :], AF.Abs),
                                        lambda: R.ts(dn[0:96, :], dn[0:96, :], 1.0, None, ALU.max),
                                        lambda: R.act(dn[0:96, :], dn[0:96, :], AF.Ln),
                                        lambda: R.act(dn[0:96, :], dn[0:96, :], AF.Exp, scale=-1.0),
                                        s5,
                                        lambda: R.act(sq[0:96, :], hh[0:96, :], AF.Square),
                                        s7,
                                        lambda: R.act(sq[0:96, :], box['ps'][0:96, :], AF.Ln, bias=epsc[0:96, 0:1], scale=1.0 / 96),
                                        lambda: R.act(sq[0:96, :], sq[0:96, :], AF.Exp, scale=-0.5),
                                        lambda: R.stt(hh[0:96, :], hh[0:96, :], V[0:96, 35 + h:36 + h], sq[0:96, :], ALU.mult, ALU.mult),
                                        lambda: R.tt(ybT[0:96, h, G * GW:(G + 1) * GW], hh[0:96, :], sgo[0:96, G * GW:(G + 1) * GW], ALU.mult),
                                    ]

                                def rnd(r):
                                    if r % 4 == 0:
                                        wall_tick(1)
                                    if 0 <= r - 2 < n:
                                        sN(r - 2)
                                    if r < n:
                                        sS(r)
                                    if 0 <= r - 1 < n:
                                        sW(r - 1)
                                    if r >= 2 and pend_epi:
                                        pend_epi.pop(0)()
                                    if r == n + 1:
                                        while pend_epi:
                                            pend_epi.pop(0)()
                                        pend_epi.extend(epi_steps())
                                return (n + 2, rnd, 2)
                            groupsB.append(mkB())
                        run_pipeline(groupsB)
                        while pend_epi:
                            pend_epi.pop(0)()
                    dump(f"ybT{l}", ybT[0:96, :, :], (96, 4, S), BF16)
                    if stop == 'mlstm':
                        break
                    slo = next_slab()
                    wo = slo[:, 0:4 * 1024].rearrange("p (j n) -> p j n", n=1024)
                    R.dma('pool', wo[0:96, :, :], wout_d[l, 384:768, :].rearrange("(j p) n -> p j n", p=96))
                    outproj_add(wo, [96, 96, 96, 96], lambda j, g: ybT[0:96, j, g * GW:(g + 1) * GW])

                    AR.reset()
                    ycT = AR.alloc(2 * S, BF16).rearrange("p (j t) -> p j t", t=S)
                    uT = AR.alloc(S, BF16)
                    vtmC = AR.alloc(NTB * 128, F32).rearrange("p (b c) -> p b c", c=128)
                    vnb = AR.alloc(NTB * 128, BF16).rearrange("p (b c) -> p b c", c=128)
                    wsf = AR.alloc(4 * 128, F32).rearrange("p (g s) -> p g s", s=128)
                    wsT = AR.alloc(4 * 128, BF16).rearrange("p (g s) -> p g s", s=128)
                    bsT = AR.alloc(512, F32)
                    lng = AR.alloc(128, F32)
                    lnb = AR.alloc(128, F32)
                    st8 = AR.alloc(64, F32)
                    st8b = AR.alloc(64, F32)
                    gx = [AR.alloc(512, F32) for _ in range(3)]
                    R.dma('sp', wsf, sguw_d[l].rearrange("g t s -> t g s"))
                    R.memset(wsf[0:64, :, 64:128], 0.0)
                    ps = PS.get()
                    for gq in range(4):
                        R.tr(ps[:, gq * 128:(gq + 1) * 128], wsf[:, gq, :], identf)
                    R.copy(wsT, ps[:, :].rearrange("p (g s) -> p g s", s=128))

                    def gelu_chain(dst, src_ps, np_, n):
                        R.act(dst, src_ps, AF.Gelu_apprx_tanh)

                    for j in range(2):
                        slc = next_slab()
                        load_in_cols(slc, 0, l, OFF['uc'] + j * 128, 128, 256)
                        wv = load_in_cols(slc, 128, l, OFF['vc'] + j * 128, 128, 256)
                        R.dma('sp', lng, bass.AP(sguln_d, (l * 2 + 0) * 256 + j * 128, [[0, 128], [1, 128]]))
                        R.dma('sp', lnb, bass.AP(sguln_d, (l * 2 + 1) * 256 + j * 128, [[0, 128], [1, 128]]))
                        for gi in range(2):
                            for rep in range(4):
                                R.dma('sp', bsT[gi * 64:(gi + 1) * 64, rep * 128:(rep + 1) * 128],
                                      bass.AP(sgub_d, (l * 4 + 2 * j + gi) * 128, [[0, 64], [1, 128]]))
                        proj_fm(wv, 0, 128, lambda g, ps: gelu_chain(uT[:, g * GW:(g + 1) * GW], ps[:, :], 128, 512))

                        def v_out(tb0, per, ps):
                            gelu_chain(vtmC[:, tb0:tb0 + per, :].rearrange("p a b -> p (a b)"), ps[:, :], 128, 512)
                        proj_tm(wv, 128, 128, v_out)
                        for tb0 in range(0, NTB, 4):
                            vv = vtmC[:, tb0:tb0 + 4, :].rearrange("p a (g c) -> p (a g) c", c=64)
                            mu = st8[:, 0:8]
                            R.add('dve', lambda e, o=mu, i=vv: e.tensor_reduce(o, i, AX.X, ALU.add), reads=[vv], writes=[mu])
                            R.ts(mu, mu, 1.0 / 64, None, ALU.mult)
                            cen = gx[0].rearrange("p (a c) -> p a c", c=64)
                            R.tt(cen, vv, bcast(mu, [[1, 8], [0, 64]]), ALU.subtract)
                            sqv = gx[1].rearrange("p (a c) -> p a c", c=64)
                            R.act(sqv, cen, AF.Square)
                            var = st8b[:, 0:8]
                            R.add('dve', lambda e, o=var, i=sqv: e.tensor_reduce(o, i, AX.X, ALU.add), reads=[sqv], writes=[var])
                            R.act(var, var, AF.Ln, bias=epsc[:, 0:1], scale=1.0 / 64)
                            R.act(var, var, AF.Exp, scale=-0.5)
                            R.tt(cen, cen, bcast(var, [[1, 8], [0, 64]]), ALU.mult)
                            cen4 = gx[0].rearrange("p (a c) -> p a c", c=128)
                            R.tt(cen4, cen4, bcast(lng, [[0, 4], [1, 128]]), ALU.mult)
                            R.tt(vnb[:, tb0:tb0 + 4, :], cen4, bcast(lnb, [[0, 4], [1, 128]]), ALU.add)
                        for tb0 in range(0, NTB, 4):
                            ps = PS.get()
                            for i in range(4):
                                tb = tb0 + i
                                for gi in range(2):
                                    R.mm(ps[gi * 64:(gi + 1) * 64, i * 128:(i + 1) * 128], vnb[:, tb, gi * 64:(gi + 1) * 64],
                                         wsT[:, 2 * j + gi, :])
                            t = gx[2]
                            R.tt(t, ps[:, :], bsT, ALU.add)
                            R.tt(ycT[:, j, tb0 * 128:(tb0 + 4) * 128], t, uT[:, tb0 * 128:(tb0 + 4) * 128], ALU.mult)
                    dump(f"ycT{l}", ycT[:, :, :], (128, 2, S), BF16)
                    if stop == 'sgu':
                        break
                    ycn = ycT
                    slo = next_slab()
                    wo = slo[:, 0:2 * 1024].rearrange("p (j n) -> p j n", n=1024)
                    R.dma('pool', wo, wout_d[l, 768:1024, :].rearrange("(j p) n -> p j n", p=128))
                    for g in range(NGR):
                        ps = PS.get()
                        for j in range(2):
                            sq = tmpA[j % 2]
                            R.act(sq[:], ycT[:, j, g * GW:(g + 1) * GW], AF.Square)
                            R.mm(ps[:, :], onesf, sq[:], start=(j == 0), stop=(j == 1))
                        t = tmpA[2]
                        R.act(t[:], ps[:, :], AF.Ln, bias=epsc[:, 0:1], scale=1.0 / 256)
                        R.act(t[:], t[:], AF.Exp, scale=-0.5)
                        for j in range(2):
                            R.stt(ycn[:, j, g * GW:(g + 1) * GW], ycT[:, j, g * GW:(g + 1) * GW], V[:, 39 + j:40 + j], t[:],
                                  ALU.mult, ALU.mult)
                    outproj_add(wo, [128, 128], lambda j, g: ycn[:, j, g * GW:(g + 1) * GW])
                    dump(f"hmix{l}", hT[:, :, 0:256], (128, 8, 256))
                    if stop == 'mix':
                        break

                wall_tick(len(wall_jobs))
                slg0, slg1, slp = next_slab(), next_slab(), next_slab()
                wgA = slg0[:, 0:4096].rearrange("p (k n) -> p k n", n=1024)
                wgB = slg1[:, 0:4096].rearrange("p (k n) -> p k n", n=1024)
                wpp = slp[:, 0:2048].rearrange("p (k n) -> p k n", n=1024)
                R.dma('pool', wgA, pgw_d[l, 0:512, :].rearrange("(k p) n -> p k n", p=128))
                R.dma('pool', wgB, pgw_d[l, 512:1024, :].rearrange("(k p) n -> p k n", p=128))
                R.dma('pool', wpp, ppw_d[l].rearrange("(k p) n -> p k n", p=128))

                AR.reset()
                wts = AR.alloc(NTB * 2, F32).rearrange("p (b k) -> p b k", k=2)
                idx = AR.alloc(NTB * 2, I32).rearrange("p (b k) -> p b k", k=2)
                idxw = AR.alloc(NBLK, I32)
                mark = AR.off
                g2bc = AR.alloc(DM, F32)
                rws = AR.alloc(8 * 36, F32).rearrange("p (k n) -> p k n", n=36)
                rbb = AR.alloc(36, F32)
                sel = AR.alloc(NTB * 64, F32).rearrange("p (b k e) -> p b k e", k=2, e=32)
                Lall = AR.alloc(NTB * 36, F32)
                AR.off -= (NTB * 36 * 4 + 31) // 32 * 32
                posl = AR.alloc(512, F32).rearrange("p (b e) -> p b e", e=32)
                run = AR.alloc(512, F32).rearrange("p (b e) -> p b e", e=32)
                sm = AR.alloc(512, F32)
                s12b = AR.alloc(NTB * 32, BF16)
                xn2 = AR.alloc(NTB * DM, BF16).rearrange("p (b d) -> p b d", d=DM)
                R.dma('sp', g2bc, bass.AP(g2row_d, l * DM, [[0, 128], [1, DM]]))
                R.dma('sp', rbb, bass.AP(rb_d, l * 36, [[0, 128], [1, 36]]))
                R.dma('sp', rws, rw_d[l].rearrange("(k p) n -> p k n", p=128))
                for k in range(8):
                    R.ts(rws[:, k, :], rws[:, k, :], V[:, 8 + k:9 + k], None, ALU.mult)
                cnt_ps = PS.pin()
                pos_ps = PS.pin()
                L3 = Lall.rearrange("p (b n) -> p b n", n=36)
                for tb in range(NTB):
                    ssq = sm[:, 480 + 2 * (tb % 4):482 + 2 * (tb % 4)]
                    pst = [PS.get(), PS.get()]
                    for half in range(2):
                        for i in range(4):
                            k = half * 4 + i
                            R.tr(pst[half][:, i * 128:(i + 1) * 128], hT[:, k, tb * 128:(tb + 1) * 128], identf)
                        R.act(tmpA[half][:], pst[half][:, :], AF.Square, accum_out=ssq[:, half:half + 1])
                    rs = sm[:, 496 + (tb % 4):497 + (tb % 4)]
                    R.tt(rs, ssq[:, 0:1], ssq[:, 1:2], ALU.add)
                    R.act(rs, rs, AF.Ln, bias=epsc[:, 0:1], scale=1.0 / DM)
                    R.act(rs, rs, AF.Exp, scale=-0.5)
                    for half in range(2):
                        R.stt(xn2[:, tb, half * 512:(half + 1) * 512], pst[half][:, :], rs, g2bc[:, half * 512:(half + 1) * 512],
                              ALU.mult, ALU.mult)
                    psl = PS.get()
                    for k in range(8):
                        R.mm(psl[:, 0:36], hT[:, k, tb * 128:(tb + 1) * 128], rws[:, k, :], start=(k == 0), stop=(k == 7))
                    R.stt(L3[:, tb, :], psl[:, 0:36], rs, rbb, ALU.mult, ALU.add)
                dump(f"Lall{l}", Lall, (128, NTB * 36))
                if stop == 'route1':
                    break
                gl = L3[:, :, 0:4]
                el4 = bass.AP(Lall.tensor, Lall.offset + 4, [list(list(Lall.ap)[0]), [36, NTB], [8, 4], [1, 8]])
                gmax, gsum, v1, v2 = sm[:, 0:16], sm[:, 16:32], sm[:, 32:48], sm[:, 48:64]
                d21, ex, dn_ = sm[:, 64:80], sm[:, 80:96], sm[:, 96:112]
                goh = sm[:, 128:192].rearrange("p (b g) -> p b g", g=4)
                gex = sm[:, 192:256].rearrange("p (b g) -> p b g", g=4)
                pen = sm[:, 256:320].rearrange("p (b g) -> p b g", g=4)
                R.add('dve', lambda e: e.tensor_reduce(gmax, gl, AX.X, ALU.max), reads=[gl], writes=[gmax])
                R.tt(goh, gl, bcast(gmax, [[1, NTB], [0, 4]]), ALU.is_equal)
                R.tt(gex, gl, bcast(gmax, [[1, NTB], [0, 4]]), ALU.subtract)
                R.act(gex, gex, AF.Exp)
                R.add('dve', lambda e: e.tensor_reduce(gsum, gex, AX.X, ALU.add), reads=[gex], writes=[gsum])
                R.add('dve', lambda e: e.reciprocal(gsum, gsum), reads=[gsum], writes=[gsum])
                R.ts(pen, goh, 1e30, -1e30, ALU.mult, ALU.add)
                em = tmpA[0][:, :]
                em3 = em.rearrange("p (b e) -> p b e", e=32)
                R.tt(em.rearrange("p (b g e) -> p b g e", g=4, e=8), el4, bcast(sm[:, 256:320], [[4, NTB], [1, 4], [0, 8]]), ALU.add)
                R.add('dve', lambda e: e.tensor_reduce(v1, em3, AX.X, ALU.max), reads=[em], writes=[v1])
                R.tt(sel[:, :, 0, :], em3, bcast(v1, [[1, NTB], [0, 32]]), ALU.is_equal)
                em2 = tmpA[1][:, :]
                em23 = em2.rearrange("p (b e) -> p b e", e=32)
                R.stt(em23, sel[:, :, 0, :], -1e30, em3, ALU.mult, ALU.add)
                R.add('dve', lambda e: e.tensor_reduce(v2, em23, AX.X, ALU.max), reads=[em2], writes=[v2])
                R.tt(sel[:, :, 1, :], em23, bcast(v2, [[1, NTB], [0, 32]]), ALU.is_equal)
                R.tt(d21, v2, v1, ALU.subtract)
                R.act(ex, d21, AF.Exp)
                R.ts(dn_, ex, 1.0, None, ALU.add)
                R.add('dve', lambda e: e.reciprocal(dn_, dn_), reads=[dn_], writes=[dn_])
                R.tt(wts[:, :, 0], dn_, gsum, ALU.mult)
                R.tt(wts[:, :, 1], wts[:, :, 0], ex, ALU.mult)
                s12b3 = s12b.rearrange("p (b e) -> p b e", e=32)
                R.tt(s12b3, sel[:, :, 0, :], sel[:, :, 1, :], ALU.add)
                dump(f"sel{l}", sel[:, :, :, :], (128, NTB, 2, 32))
                dump(f"wts{l}", wts[:, :, :], (128, NTB, 2))
                if stop == 'route2':
                    break
                for tb in range(NTB):
                    R.mm(cnt_ps[:, tb * 32:(tb + 1) * 32], onesb, s12b3[:, tb, :])
                    R.mm(pos_ps[:, tb * 32:(tb + 1) * 32], tricb, s12b3[:, tb, :])
                cntb = tmpA[2][:, :].rearrange("p (b e) -> p b e", e=32)
                R.copy(cntb, cnt_ps[:, :].rearrange("p (b e) -> p b e", e=32))
                if stop == 'r3':
                    R.copy(tmpA[0][:, :], cnt_ps[:, :])
                    dump(f"c3_{l}", tmpA[0][:, :], (128, 512))
                    break
                R.memset(run[:, 0, :], 0.0)
                for b in range(NTB - 1):
                    R.tt(run[:, b + 1, :], run[:, b, :], cntb[:, b, :], ALU.add)
                ctot = sm[:, 0:32]
                R.tt(ctot, run[:, NTB - 1, :], cntb[:, NTB - 1, :], ALU.add)
                R.copy(posl, pos_ps[:, :].rearrange("p (b e) -> p b e", e=32))
                PS.unpin(cnt_ps)
                PS.unpin(pos_ps)
                cmpb = tmpA[1][:, 0:512].rearrange("p (e m) -> p e m", m=16)
                R.tt(cmpb, bcast(ctot, [[1, 32], [0, 16]]), bcast(cx[:, CX_THR:CX_THR + 16], [[0, 32], [1, 16]]), ALU.is_gt)
                blk = sm[:, 32:64]
                R.add('dve', lambda e, o=blk, i=cmpb: e.tensor_reduce(o, i, AX.X, ALU.add), reads=[tmpA[1][:, 0:512]], writes=[blk])
                pend = sm[:, 64:96]
                R.add('dve', lambda e: e.tensor_tensor_scan(pend, onesf[:, 0:32], blk, 0.0, ALU.mult, ALU.add),
                      reads=[onesf[:, 0:32], blk], writes=[pend])
                pst128 = sm[:, 96:128]
                R.tt(pst128, pend, blk, ALU.subtract)
                R.ts(pst128, pst128, 128.0, None, ALU.mult)
                R.tt(run, run, bcast(pst128, [[0, NTB], [1, 32]]), ALU.add)
                R.tt(posl, posl, run, ALU.add)
                ej = sm[:, 128:192]
                for jc in range(4):
                    cmpj = tmpA[0][:, 0:512].rearrange("p (j e) -> p j e", e=32)
                    R.tt(cmpj, bcast(cx[:, CX_IOTAJ + jc * 16:CX_IOTAJ + jc * 16 + 16], [[1, 16], [0, 32]]),
                         bcast(pend, [[0, 16], [1, 32]]), ALU.is_ge)
                    R.add('dve', lambda e, o=ej[:, jc * 16:(jc + 1) * 16], i=cmpj: e.tensor_reduce(o, i, AX.X, ALU.add),
                          reads=[tmpA[0][:, 0:512]], writes=[ej[:, jc * 16:(jc + 1) * 16]])
                oob = sm[:, 192:256]
                R.ts(oob, ej, 31.5, 1.0e6, ALU.is_gt, ALU.mult)
                R.ts(ej, ej, float(32 * l), 128.0, ALU.add, ALU.mult)
                R.tt(ej, ej, oob, ALU.add)
                R.ts(ej, ej, cx[:, CX_IOTAP:CX_IOTAP + 1], None, ALU.add)
                R.copy(idxw, ej)
                dump(f"ctot{l}", ctot, (128, 32))
                dump(f"ej{l}", ej, (128, 64))
                if stop == 'r4':
                    break
                for k in range(2):
                    tk = tmpA[k][:, :].rearrange("p (b e) -> p b e", e=32)
                    R.tt(tk, sel[:, :, k, :], posl, ALU.mult)
                    sf = sm[:, 320 + 16 * k:336 + 16 * k]
                    R.add('dve', lambda e, o=sf, i=tk: e.tensor_reduce(o, i, AX.X, ALU.add), reads=[tmpA[k][:, :]], writes=[sf])
                    R.copy(idx[:, :, k], sf)
                dump(f"idx{l}", idx[:, :, :], (128, NTB, 2), I32)
                dump(f"posl{l}", posl[:, :, :], (128, NTB, 32))
                if stop == 'route':
                    break
                for tb in range(NTB):
                    for k in range(2):
                        R.add('pool', lambda e, o=Xs.ap(), ix=idx[:, tb, k:k + 1], src=xn2[:, tb, :]: e.indirect_dma_start(
                            out=o, out_offset=bass.IndirectOffsetOnAxis(ap=ix, axis=0), in_=src, in_offset=None),
                            reads=[xn2[:, tb, :], idx[:, tb, k:k + 1]], writes=[("XsV", 0, 1, tb * 2 + k, tb * 2 + k + 1)], dma=True)
                AR.off = mark
                wbuf = [AR.alloc(WROW, BF16) for _ in range(3)]
                xg = [AR.alloc(DM, BF16) for _ in range(2)]
                xgT = [AR.alloc(8 * 128, BF16).rearrange("p (k r) -> p k r", r=128) for _ in range(2)]
                hact = AR.alloc(256, BF16)
                sgt = AR.alloc(256, F32)
                hTe = AR.alloc(256, BF16).rearrange("p (c r) -> p c r", r=128)
                ybuf = [AR.alloc(DM, BF16) for _ in range(2)]
                def issue_loads(jb):
                    R.add('pool', lambda e, o=wbuf[jb % 3], ix=idxw[:, jb:jb + 1]: gather_w(e, o, ix),
                          reads=[R.whole(Wall), idxw[:, jb:jb + 1]], writes=[wbuf[jb % 3]], dma=True)
                    R.dma('sp', xg[jb % 2], Xs[jb * 128:(jb + 1) * 128, :], extra_reads=[("XsV", 0, 1, 0, 1 << 30)])
                issue_loads(0)
                issue_loads(1)
                for jb in range(NBLK):
                    wb_, xg_, xgT_, yb_ = wbuf[jb % 3], xg[jb % 2], xgT[jb % 2], ybuf[jb % 2]
                    for half in range(2):
                        psb = PS.get().bitcast(BF16)
                        for i in range(4):
                            k = half * 4 + i
                            R.tr(psb[:, i * 128:(i + 1) * 128], xg_[:, k * 128:(k + 1) * 128], identb)
                        evac(xgT_[:, half * 4:half * 4 + 4, :], psb[:, 0:512].rearrange("p (a b) -> p a b", b=128))
                    psg = PS.get()
                    for k in range(8):
                        R.mm(psg[:, :], xgT_[:, k, :], wb_[:, k * 512:(k + 1) * 512], start=(k == 0), stop=(k == 7))
                    R.act(sgt, psg[:, 0:256], AF.Silu)
                    R.tt(hact, sgt, psg[:, 256:512], ALU.mult)
                    psb = PS.get().bitcast(BF16)
                    for c in range(2):
                        R.tr(psb[:, c * 128:(c + 1) * 128], hact[:, c * 128:(c + 1) * 128], identb)
                    evac(hTe, psb[:, 0:256].rearrange("p (a b) -> p a b", b=128))
                    for fh in range(2):
                        psy = PS.get()
                        for c in range(2):
                            R.mm(psy[:, :], hTe[:, c, :], wb_[:, 4096 + c * 1024 + fh * 512:4096 + c * 1024 + (fh + 1) * 512],
                                 start=(c == 0), stop=(c == 1))
                        evac(yb_[:, fh * 512:(fh + 1) * 512], psy[:, :])
                    R.dma('act', Yd[jb * 128:(jb + 1) * 128, :], yb_)
                    if jb + 2 < NBLK:
                        issue_loads(jb + 2)
                AR.off = mark
                yg = [[AR.alloc(DM, BF16) for _ in range(2)] for _ in range(3)]
                yacc = [AR.alloc(DM, F32) for _ in range(2)]
                for tb in range(NTB):
                    y1b, y2b = yg[tb % 3]
                    y1 = yacc[tb % 2]
                    for k, yy in enumerate((y1b, y2b)):
                        R.add('pool', lambda e, o=yy, ix=idx[:, tb, k:k + 1]: e.indirect_dma_start(
                            out=o, out_offset=None, in_=Yd.ap(), in_offset=bass.IndirectOffsetOnAxis(ap=ix, axis=0)),
                            reads=[R.whole(Yd), idx[:, tb, k:k + 1]], writes=[yy], dma=True)
                    R.act(y1, y1b, AF.Identity, scale=wts[:, tb, 0:1])
                    R.stt(y1, y2b, wts[:, tb, 1:2], y1, ALU.mult, ALU.add)
                    for half in range(2):
                        ps = PS.get()
                        for i in range(4):
                            k = half * 4 + i
                            R.tr(ps[:, i * 128:(i + 1) * 128], y1[:, k * 128:(k + 1) * 128], identf)
                        R.tt(hT[:, half * 4:half * 4 + 4, tb * 128:(tb + 1) * 128], hT[:, half * 4:half * 4 + 4, tb * 128:(tb + 1) * 128],
                             ps[:, :].rearrange("p (a b) -> p a b", b=128), ALU.add)
                dump(f"hmoe{l}", hT[:, :, 0:256], (128, 8, 256))
                if stop == 'moe':
                    break

                AR.reset()
                ptm = AR.alloc(NTB * 256, F32).rearrange("p (b c) -> p b c", c=256)
                pT = AR.alloc(2 * S, BF16).rearrange("p (c t) -> p c t", t=S)
                R.dma('sp', ptm, p_d[l, seq].rearrange("(b p) c -> p b c", p=128))
                for tb0 in range(0, NTB, 2):
                    ps = PS.get()
                    for c in range(2):
                        for tl in range(2):
                            R.tr(ps[:, (c * 2 + tl) * 128:(c * 2 + tl + 1) * 128], ptm[:, tb0 + tl, c * 128:(c + 1) * 128], identf)
                    evac(pT[:, :, tb0 * 128:(tb0 + 2) * 128], ps[:, :].rearrange("p (c t) -> p c t", t=256))
                norm_fm(16, l)
                for fo in range(8):
                    for g in range(NGR):
                        psg = PS.get()
                        for k in range(8):
                            wv = wgA if k < 4 else wgB
                            R.mm(psg[:, :], wv[:, k % 4, fo * 128:(fo + 1) * 128], hnT[:, k, g * GW:(g + 1) * GW],
                                 start=(k == 0), stop=(k == 7))
                        psp = PS.get()
                        for c in range(2):
                            R.mm(psp[:, :], wpp[:, c, fo * 128:(fo + 1) * 128], pT[:, c, g * GW:(g + 1) * GW],
                                 start=(c == 0), stop=(c == 1))
                        sg_ = tmpA[(fo * 4 + g) % 2]
                        R.act(sg_[:], psg[:, :], AF.Sigmoid)
                        R.tt(sg_[:], sg_[:], psp[:, :], ALU.mult)
                        R.tt(hT[:, fo, g * GW:(g + 1) * GW], hT[:, fo, g * GW:(g + 1) * GW], sg_[:], ALU.add, eng='pool')
                dump(f"hple{l}", hT[:, :, 0:256], (128, 8, 256))
            else:
                AR.reset()
                gfbc = AR.alloc(DM, F32)
                ot = [AR.alloc(DM, F32) for _ in range(2)]
                sm = AR.alloc(8, F32)
                R.dma('sp', gfbc, bass.AP(gfrow_d, 0, [[0, 128], [1, DM]]))
                for tb in range(NTB):
                    o_ = ot[tb % 2]
                    ssq = sm[:, 0:2]
                    pst = [PS.get(), PS.get()]
                    for half in range(2):
                        for i in range(4):
                            k = half * 4 + i
                            R.tr(pst[half][:, i * 128:(i + 1) * 128], hT[:, k, tb * 128:(tb + 1) * 128], identf)
                        R.act(tmpA[half][:], pst[half][:, :], AF.Square, accum_out=ssq[:, half:half + 1])
                    rs = sm[:, 2:3]
                    R.tt(rs, ssq[:, 0:1], ssq[:, 1:2], ALU.add)
                    R.act(rs, rs, AF.Ln, bias=epsc[:, 0:1], scale=1.0 / DM)
                    R.act(rs, rs, AF.Exp, scale=-0.5)
                    for half in range(2):
                        R.stt(o_[:, half * 512:(half + 1) * 512], pst[half][:, :], rs, gfbc[:, half * 512:(half + 1) * 512],
                              ALU.mult, ALU.mult)
                    R.dma('sp', out_d[seq, tb * 128:(tb + 1) * 128, :], o_)
                continue
            break
        toks = []
        for e in ('sp', 'pool', 'act'):
            toks += [(('d', e, s), 16 * c) for s, c in enumerate(R.slot_cnt[e]) if c > 0]
        R.wait_all('sp', toks)
        print("recorded ops:", {e: len(R.ops[e]) for e in R.ENG})
        R.emit()
    return nc, dbg_out


def prep_inputs(inputs, NLAYER=2):
    f = lambda a: np.ascontiguousarray(np.asarray(a, dtype=np.float32))
    wg, wu, wd = f(inputs["w_gate"]), f(inputs["w_up"]), f(inputs["w_down"])
    gu = np.concatenate([wg.reshape(2, 32, 8, 128, 256), wu.reshape(2, 32, 8, 128, 256)], axis=-1)
    gu = gu.transpose(0, 1, 3, 2, 4).reshape(2, 32, 128, 4096)
    dn = wd.reshape(2, 32, 2, 128, 1024).transpose(0, 1, 3, 2, 4).reshape(2, 32, 128, 2048)
    wall = np.ascontiguousarray(np.concatenate([gu, dn], axis=-1).reshape(2 * 32 * 128, WROW))
    rw = np.ascontiguousarray(np.concatenate([f(inputs["router_gw"]), f(inputs["router_ew"])], axis=-1))
    rb = np.ascontiguousarray(np.concatenate([f(inputs["router_gb"]), f(inputs["router_eb"])], axis=-1))
    vec = np.zeros((2, 128, NV), np.float32)
    for l in range(2):
        vec[l, :, 0:8] = f(inputs["norm1_g"])[l].reshape(8, 128).T
        vec[l, :, 8:16] = f(inputs["norm2_g"])[l].reshape(8, 128).T
        vec[l, :, 16:24] = f(inputs["ple_norm_g"])[l].reshape(8, 128).T
        vec[l, :, 24:32] = f(inputs["final_g"]).reshape(8, 128).T
        vec[l, :, 32:35] = f(inputs["sb_out_g"])[l].reshape(3, 128).T
        vec[l, 0:96, 35:39] = f(inputs["mnorm_g"])[l].reshape(4, 96).T
        vec[l, :, 39:41] = f(inputs["sgu_out_g"])[l].reshape(2, 128).T
        cw = f(inputs["conv_w"])[l].reshape(4, 2, 4, 96)
        vec[l, 0:96, 41:73] = cw.transpose(3, 1, 2, 0).reshape(96, 32)
        cbb = f(inputs["conv_b"])[l].reshape(2, 4, 96)
        vec[l, 0:96, 73:81] = cbb.transpose(2, 0, 1).reshape(96, 8)
        vec[l, 0:4, 81] = f(inputs["igate_b"])[l]
        vec[l, 0:4, 82] = f(inputs["fgate_b"])[l]
    sguln = np.ascontiguousarray(np.stack([f(inputs["sgu_ln_g"]), f(inputs["sgu_ln_b"])], axis=1))
    c, cx = make_consts()
    shared = dict(w_in=f(inputs["w_in"]), w_out=f(inputs["w_out"]), ple_gate_w=f(inputs["ple_gate_w"]),
                  ple_proj_w=f(inputs["ple_proj_w"]), wall_src=wall, rw=rw, rb=rb, vec=vec,
                  sgu_w=f(inputs["sgu_w"]), sgu_b=f(inputs["sgu_b"]), sgu_ln=sguln, g2row=f(inputs["norm2_g"]),
                  gfrow=f(inputs["final_g"]).reshape(1, DM), consts=c, constsx=cx)
    return shared


_CACHE = {}


def kernel(**inputs):
    NCORE = 8
    x = np.asarray(inputs["x"], dtype=np.float32)
    p = np.asarray(inputs["p"], dtype=np.float32)
    B = x.shape[0]
    nseq = B // NCORE
    shared = prep_inputs(inputs)
    if "nc" not in _CACHE:
        _CACHE["nc"] = build(NSEQ=nseq, NLAYER=2)[0]
    nc = _CACHE["nc"]
    in_maps = []
    for c in range(NCORE):
        m = dict(shared)
        m["x"] = np.ascontiguousarray(x[c * nseq:(c + 1) * nseq])
        m["p"] = np.ascontiguousarray(p[:, c * nseq:(c + 1) * nseq])
        in_maps.append(m)
    res = run_bass_kernel_spmd(nc, in_maps, core_ids=list(range(NCORE)))
    out = np.concatenate([np.asarray(r["out"]) for r in res.results], axis=0)
    return out.astype(np.float32)
```

```python
import contextlib
import numpy as np
import concourse.bass as bass
import concourse.mybir as mybir

F32 = mybir.dt.float32
BF16 = mybir.dt.bfloat16
I32 = mybir.dt.int32
U32 = mybir.dt.uint32
AF = mybir.ActivationFunctionType
ALU = mybir.AluOpType
AX = mybir.AxisListType


def _dsize(dt):
    return mybir.dt.size(dt)


class Rec:
    ENG = ['pe', 'act', 'dve', 'pool', 'sp']

    def __init__(self, nc, same_engine_sync=True):
        self.nc = nc
        self.ops = {e: [] for e in self.ENG}
        self.cnt = {e: 0 for e in self.ENG}
        self.waited = {e: {} for e in self.ENG}
        self.recs = {}
        self.nslot = {'sp': 8, 'pool': 8, 'act': 4}
        self.slot_cnt = {e: [0] * n for e, n in self.nslot.items()}
        self.slot_rr = {e: 0 for e in self.nslot}
        self.ses = same_engine_sync
        self.nops = 0

    def rng(self, ap):
        if isinstance(ap, tuple):
            return ap
        t = ap.tensor
        name = t.name
        sz = _dsize(ap.dtype)
        dims = list(ap.ap)
        if isinstance(t, bass.DRamTensorHandle):
            lo = ap.offset
            hi = lo + sum((c - 1) * abs(s) for s, c in dims) + 1
            return (name, 0, 1, lo * sz, hi * sz)
        pstride, pcount = dims[0]
        if pstride == 0:
            pstride = 1 << 40
        plo = ap.offset // pstride if pstride < (1 << 40) else 0
        flo = ap.offset - plo * pstride if pstride < (1 << 40) else ap.offset
        fhi = flo + sum((c - 1) * abs(s) for s, c in dims[1:]) + 1
        return (name, plo, plo + pcount, flo * sz, fhi * sz)

    def whole(self, t):
        return (t.name, 0, 1 << 20, 0, 1 << 60)

    def add(self, eng, fn, reads=(), writes=(), dma=False):
        deps = set()
        rr = [self.rng(a) for a in reads]
        ww = [self.rng(a) for a in writes]
        for (name, plo, phi, flo, fhi) in rr:
            for r in self.recs.get(name, ()):
                if r[4] and r[0] < phi and plo < r[1] and r[2] < fhi and flo < r[3]:
                    deps.add(r[5])
        for (name, plo, phi, flo, fhi) in ww:
            for r in self.recs.get(name, ()):
                if r[0] < phi and plo < r[1] and r[2] < fhi and flo < r[3]:
                    deps.add(r[5])
        waits = {}
        for (semkey, val) in deps:
            if semkey[0] == 'c' and semkey[1] == eng:
                if eng == 'pe' or not self.ses:
                    continue
            if self.waited[eng].get(semkey, 0) >= val:
                continue
            if waits.get(semkey, 0) < val:
                waits[semkey] = val
        if dma:
            s = self.slot_rr[eng]
            self.slot_rr[eng] = (s + 1) % self.nslot[eng]
            semkey = ('d', eng, s)
            prev = self.slot_cnt[eng][s]
            if prev > 0 and self.waited[eng].get(semkey, 0) < 16 * prev:
                if waits.get(semkey, 0) < 16 * prev:
                    waits[semkey] = 16 * prev
            self.slot_cnt[eng][s] = prev + 1
            token = (semkey, 16 * (prev + 1))
        else:
            self.cnt[eng] += 1
            token = (('c', eng), self.cnt[eng])
        for k, v in waits.items():
            self.waited[eng][k] = v
        self.ops[eng].append((waits, fn, token))
        self.nops += 1
        for (name, plo, phi, flo, fhi) in ww:
            lst = self.recs.setdefault(name, [])
            lst[:] = [r for r in lst if not (plo <= r[0] and r[1] <= phi and flo <= r[2] and r[3] <= fhi)]
            lst.append((plo, phi, flo, fhi, True, token))
        for (name, plo, phi, flo, fhi) in rr:
            lst = self.recs.setdefault(name, [])
            sk = token[0]
            lst[:] = [r for r in lst if not ((not r[4]) and r[5][0] == sk and r[0] == plo and r[1] == phi
                                             and r[2] == flo and r[3] == fhi)]
            lst.append((plo, phi, flo, fhi, False, token))
        return token

    def wait_all(self, eng, tokens):
        waits = {}
        for (semkey, val) in tokens:
            if waits.get(semkey, 0) < val:
                waits[semkey] = val
        for k, v in waits.items():
            self.waited[eng][k] = max(self.waited[eng].get(k, 0), v)
        self.ops[eng].append((waits, None, None))

    def emit(self):
        nc = self.nc
        needed = {e: set() for e in self.ENG}
        for e in self.ENG:
            for (waits, fn, token) in self.ops[e]:
                for (semkey, val) in waits.items():
                    if semkey[0] == 'c':
                        needed[semkey[1]].add(val)
        rank = {}
        for e in self.ENG:
            srt = sorted(needed[e])
            rank[e] = {v: i + 1 for i, v in enumerate(srt)}
        with contextlib.ExitStack() as st:
            sems = {}
            for e in self.ENG:
                sems[('c', e)] = st.enter_context(nc.semaphore(f"c_{e}"))
            for e, n in self.nslot.items():
                for s in range(n):
                    sems[('d', e, s)] = st.enter_context(nc.semaphore(f"d_{e}_{s}"))
            block = st.enter_context(nc.Block())
            rec = self

            def replay(e, eng):
                for (waits, fn, token) in rec.ops[e]:
                    for (semkey, val) in waits.items():
                        if semkey[0] == 'c':
                            val = rank[semkey[1]][val]
                        eng.wait_ge(sems[semkey], val)
                    if fn is None:
                        continue
                    inst = fn(eng)
                    semkey, val = token
                    if semkey[0] == 'd':
                        inst.then_inc(sems[semkey], 16)
                    elif val in rank[e]:
                        inst.then_inc(sems[semkey], 1)

            if self.ops['pe']:
                @block.tensor
                def _(eng):
                    replay('pe', eng)
            if self.ops['act']:
                @block.scalar
                def _(eng):
                    replay('act', eng)
            if self.ops['dve']:
                @block.vector
                def _(eng):
                    replay('dve', eng)
            if self.ops['pool']:
                @block.gpsimd
                def _(eng):
                    replay('pool', eng)
            if self.ops['sp']:
                @block.sync
                def _(eng):
                    replay('sp', eng)

    def dma(self, eng, out, in_, extra_reads=(), extra_writes=(), **kw):
        return self.add(eng, lambda e: e.dma_start(out=out, in_=in_, **kw),
                        reads=[in_] + list(extra_reads), writes=[out] + list(extra_writes), dma=True)

    def mm(self, out, lhsT, rhs, start=True, stop=True, **kw):
        return self.add('pe', lambda e: e.matmul(out, lhsT, rhs, start=start, stop=stop, **kw),
                        reads=[lhsT, rhs], writes=[out])

    def tr(self, out, in_, ident):
        return self.add('pe', lambda e: e.transpose(out, in_, ident), reads=[in_, ident], writes=[out])

    def act(self, out, in_, func, bias=None, scale=None, accum_out=None, eng='act'):
        reads = [in_]
        kw = {}
        if bias is not None:
            kw['bias'] = bias
            if not isinstance(bias, (int, float)):
                reads.append(bias)
        if scale is not None:
            kw['scale'] = scale
            if not isinstance(scale, (int, float)):
                reads.append(scale)
        writes = [out]
        if accum_out is not None:
            kw['accum_out'] = accum_out
            writes.append(accum_out)
        return self.add(eng, lambda e: e.activation(out, in_, func, **kw), reads=reads, writes=writes)

    def tt(self, out, in0, in1, op, eng='dve'):
        return self.add(eng, lambda e: e.tensor_tensor(out, in0, in1, op), reads=[in0, in1], writes=[out])

    def ts(self, out, in0, s1, s2, op0, op1=None, eng='dve', accum_out=None):
        reads = [in0]
        for s in (s1, s2):
            if s is not None and not isinstance(s, (int, float)):
                reads.append(s)
        writes = [out]
        kw = {}
        if accum_out is not None:
            kw['accum_out'] = accum_out
            writes.append(accum_out)
        if op1 is None:
            return self.add(eng, lambda e: e.tensor_scalar(out, in0, s1, None, op0, **kw), reads=reads, writes=writes)
        return self.add(eng, lambda e: e.tensor_scalar(out, in0, s1, s2, op0, op1, **kw), reads=reads, writes=writes)

    def stt(self, out, in0, scalar, in1, op0, op1, eng='dve'):
        reads = [in0, in1]
        if not isinstance(scalar, (int, float)):
            reads.append(scalar)
        return self.add(eng, lambda e: e.scalar_tensor_tensor(out, in0, scalar, in1, op0, op1), reads=reads, writes=[out])

    def copy(self, out, in_, eng='dve'):
        if eng == 'act':
            return self.add('act', lambda e: e.copy(out, in_), reads=[in_], writes=[out])
        return self.add(eng, lambda e: e.tensor_copy(out, in_), reads=[in_], writes=[out])

    def memset(self, ap, val, eng='dve'):
        return self.add(eng, lambda e: e.memset(ap, val), writes=[ap])


from concourse.bass_utils import run_bass_kernel_spmd

S = 2048
DM = 1024
NIN = 3208
NTB = 16
NGR = 4
GW = 512
OFF = dict(qa=0, ka=384, va=768, qb=1152, kb=1536, vb=1920, ob=2304, ib=2688, fb=2692, uc=2696, vc=2952)
NBLK = 64
WROW = 6144
EPS = 1e-6
NV = 96
C_ID = 0
C_ONE = 128
C_TRIS = 256
C_TRIC = 384
C_MSB = 512
C_MML = 512 + 2048
C_END = 512 + 4096
CX_IOTAP = 0
CX_IOTAJ = 1
CX_THR = 65
CX_SEL4 = 81
CX_SELR = 81 + 512
CX_END = 81 + 512 + 96


def make_consts():
    c = np.zeros((128, C_END), np.float32)
    c[:, C_ID:C_ID + 128] = np.eye(128)
    c[:, C_ONE:C_ONE + 128] = 1.0
    j = np.arange(128)[:, None]
    s = np.arange(128)[None, :]
    c[:, C_TRIS:C_TRIS + 128] = (j >= s)
    c[:, C_TRIC:C_TRIC + 128] = (j < s)
    for jj in range(4):
        m_sb = np.zeros((128, 4, 128), np.float32)
        m_ml = np.zeros((128, 4, 128), np.float32)
        for n in range(4):
            if n > jj:
                m_sb[:, n, :] = 1.0
                m_ml[:, n, :] = 1.0
            elif n == jj:
                m_sb[:, n, :] = (j < s)
                m_ml[:, n, :] = (j <= s)
        c[:, C_MSB + jj * 512:C_MSB + (jj + 1) * 512] = m_sb.reshape(128, 512)
        c[:, C_MML + jj * 512:C_MML + (jj + 1) * 512] = m_ml.reshape(128, 512)
    cx = np.zeros((128, CX_END), np.float32)
    cx[:, CX_IOTAP] = np.arange(128)
    cx[:, CX_IOTAJ:CX_IOTAJ + 64] = np.arange(64)[None, :]
    cx[:, CX_THR:CX_THR + 16] = (np.arange(16) * 128)[None, :]
    for h in range(4):
        cx[h, CX_SEL4 + h * 128:CX_SEL4 + (h + 1) * 128] = 1.0
    cx[96, CX_SELR:CX_SELR + 96] = 1.0
    return c, cx


class PSM:
    def __init__(self, banks):
        self.banks = banks
        self.pinned = set()
        self.i = 0

    def _next(self):
        for _ in range(16):
            b = self.i
            self.i = (self.i + 1) % len(self.banks)
            if b not in self.pinned:
                return b
        raise RuntimeError("no psum bank")

    def get(self):
        return self.banks[self._next()]

    def try_get(self):
        if len(self.pinned) >= len(self.banks):
            return None
        return self.banks[self._next()]

    def pin(self):
        b = self._next()
        self.pinned.add(b)
        return self.banks[b]

    def unpin(self, bank):
        self.pinned.discard(self.banks.index(bank))


def run_pipeline(groups):
    timeline = {}
    t0 = 0
    for gi, (n_r, fn, ov) in enumerate(groups):
        for r in range(n_r):
            timeline.setdefault(t0 + r, []).append((gi, r))
        t0 = t0 + n_r - ov
    for t in sorted(timeline):
        for (gi, r) in timeline[t]:
            groups[gi][1](r)


def bcast(ap, dims):
    p = list(ap.ap)[0]
    return bass.AP(ap.tensor, ap.offset, [[p[0], p[1]]] + [list(d) for d in dims])


SKIPMIX = False


def build(NSEQ=4, NLAYER=2, dbg=None, stop=None):
    nc = bass.Bass("TRN2", target_bir_lowering=False)
    R = Rec(nc)
    dt = nc.dram_tensor
    x_d = dt("x", [NSEQ, S, DM], F32, kind="ExternalInput")
    p_d = dt("p", [2, NSEQ, S, 256], F32, kind="ExternalInput")
    win_d = dt("w_in", [2, DM, NIN], F32, kind="ExternalInput")
    wout_d = dt("w_out", [2, DM, DM], F32, kind="ExternalInput")
    pgw_d = dt("ple_gate_w", [2, DM, DM], F32, kind="ExternalInput")
    ppw_d = dt("ple_proj_w", [2, 256, DM], F32, kind="ExternalInput")
    wall_d = dt("wall_src", [2 * 32 * 128, WROW], F32, kind="ExternalInput")
    rw_d = dt("rw", [2, DM, 36], F32, kind="ExternalInput")
    rb_d = dt("rb", [2, 36], F32, kind="ExternalInput")
    vec_d = dt("vec", [2, 128, NV], F32, kind="ExternalInput")
    sguw_d = dt("sgu_w", [2, 4, 128, 128], F32, kind="ExternalInput")
    sgub_d = dt("sgu_b", [2, 4, 128], F32, kind="ExternalInput")
    sguln_d = dt("sgu_ln", [2, 2, 256], F32, kind="ExternalInput")
    g2row_d = dt("g2row", [2, DM], F32, kind="ExternalInput")
    gfrow_d = dt("gfrow", [1, DM], F32, kind="ExternalInput")
    c_d = dt("consts", [128, C_END], F32, kind="ExternalInput")
    cx_d = dt("constsx", [128, CX_END], F32, kind="ExternalInput")
    out_d = dt("out", [NSEQ, S, DM], F32, kind="ExternalOutput")
    Xs = dt("Xs", [NBLK * 128, DM], BF16, kind="Internal")
    Yd = dt("Yd", [NBLK * 128, DM], BF16, kind="Internal")
    Wall = dt("Wall", [2 * 32 * 128, WROW], BF16, kind="Internal")
    dbg_out = {}

    st = contextlib.ExitStack()
    with st:
        def sb(name, shape, dtp):
            return st.enter_context(nc.sbuf_tensor(name, shape, dtp))
        hT = sb("hT", [128, 8, S], F32)
        hnT = sb("hnT", [128, 8, S], BF16)
        NSLAB = 4
        slabs = [sb(f"slab{i}", [128, 4096], BF16) for i in range(NSLAB)]
        cb = sb("cb", [128, C_END], BF16)
        cf = sb("cf", [128, 256], F32)
        cx = sb("cx", [128, CX_END], F32)
        vecs = [sb(f"vec{l}", [128, NV], F32) for l in range(2)]
        rstd_bc = sb("rstd_bc", [128, S], F32)
        tmpA = [sb(f"tmpA{i}", [128, 512], F32) for i in range(3)]
        epsc = sb("epsc", [128, 1], F32)
        AR_BYTES = 52 * 1024
        arena = sb("arena", [128, AR_BYTES // 4], F32)
        arena_b = arena.bitcast(BF16)
        arena_i = arena.bitcast(I32)
        banks = [st.enter_context(nc.psum_tensor(f"ps{i}", [128, 512], F32)) for i in range(8)]
        PS = PSM(banks)
        print("sbuf remaining", nc.sbuf_bytes_remaining)

        identf = cf[:, 0:128]
        onesf = cf[:, 128:256]
        identb = cb[:, C_ID:C_ID + 128]
        onesb = cb[:, C_ONE:C_ONE + 128]
        trisb = cb[:, C_TRIS:C_TRIS + 128]
        tricb = cb[:, C_TRIC:C_TRIC + 128]

        class Arena:
            def __init__(self):
                self.off = 0

            def reset(self):
                self.off = 0

            def alloc(self, n, dtp):
                sz = 4 if dtp in (F32, I32) else 2
                nbytes = (n * sz + 31) // 32 * 32
                assert self.off + nbytes <= AR_BYTES, (self.off, nbytes)
                start = self.off // sz
                self.off += nbytes
                h = arena if dtp == F32 else (arena_i if dtp == I32 else arena_b)
                return h[:, start:start + n]
        AR = Arena()

        slab_i = [0]

        def next_slab():
            s = slabs[slab_i[0] % NSLAB]
            slab_i[0] += 1
            return s

        def dump(name, ap, shape, dtp=F32):
            if dbg is None or name not in dbg:
                return
            d = dt("dbg_" + name, list(shape), dtp, kind="ExternalOutput")
            dbg_out[name] = d
            R.dma('sp', d.ap(), ap)

        flip = [0]
        _breg = {}

        def gather_w(e, o, ix):
            reg = e.to_reg(2 * 32 * 128 - 1)
            inst = e.indirect_dma_start(out=o, out_offset=None, in_=Wall.ap(),
                                        in_offset=bass.IndirectOffsetOnAxis(ap=ix, axis=0),
                                        bounds_check=reg, oob_is_err=False)
            e.free_register(reg)
            return inst

        def pe_warm(n=24):
            bank = PS.get()
            for _ in range(n):
                R.mm(bank[:, :], onesb, cb[:, C_MSB:C_MSB + 512], start=True, stop=True)

        def evac(out, in_, scale=None):
            flip[0] ^= 1
            if scale is not None:
                R.act(out, in_, AF.Copy, scale=scale)
            elif flip[0]:
                R.act(out, in_, AF.Copy)
            else:
                R.copy(out, in_)

        R.dma('pool', cb[:], c_d.ap())
        R.dma('sp', cf[:], c_d[:, 0:256])
        R.dma('sp', cx[:], cx_d.ap())
        for l in range(2):
            R.dma('sp', vecs[l][:], vec_d[l])
        R.memset(epsc[:], EPS)

        wall_jobs = list(range(NLAYER * 32)) if (stop is None or stop in ('moe', 'ple', 'all', 'route')) else []

        def wall_tick(n=1):
            for _ in range(n):
                if not wall_jobs:
                    return
                i = wall_jobs.pop(0)
                R.dma('pool', Wall[i * 128:(i + 1) * 128, :], wall_d[i * 128:(i + 1) * 128, :], max_dma_last_dim=4096)
        if stop is None or stop in ('moe', 'ple', 'all', 'route'):
            AR.reset()
            zt = AR.alloc(4096, BF16)
            R.memset(zt, 0.0)
            for i in range(NBLK * 128 // 512):
                R.dma('sp', Xs[i * 512:(i + 1) * 512, :].rearrange("(a p) d -> p a d", p=128),
                      zt.rearrange("p (a d) -> p a d", d=1024), extra_writes=[("XsV", 0, 1, 0, 1 << 30)])

        def norm_fm(gcol, l, src=None):
            for g in range(NGR):
                ps = PS.get()
                for k in range(8):
                    sq = tmpA[k % 2]
                    R.act(sq[:], hT[:, k, g * GW:(g + 1) * GW], AF.Square)
                    R.mm(ps[:, :], onesf, sq[:], start=(k == 0), stop=(k == 7))
                t = tmpA[2]
                R.act(t[:], ps[:, :], AF.Ln, bias=epsc[:, 0:1], scale=1.0 / DM)
                R.act(rstd_bc[:, g * GW:(g + 1) * GW], t[:], AF.Exp, scale=-0.5)
                for k in range(8):
                    R.stt(hnT[:, k, g * GW:(g + 1) * GW], hT[:, k, g * GW:(g + 1) * GW],
                          vecs[l][:, gcol + k:gcol + k + 1], rstd_bc[:, g * GW:(g + 1) * GW], ALU.mult, ALU.mult)

        def load_in_cols(slab, dst_col, l, col0, ncols, width):
            v = slab[:, 0:8 * width].rearrange("p (k n) -> p k n", n=width)
            src = win_d[l].rearrange("(k p) n -> p k n", p=128)[:, :, col0:col0 + ncols]
            R.dma('pool', v[:, :, dst_col:dst_col + ncols], src)
            return v

        def proj_fm(wv, c0, M, out_fn):
            for g in range(NGR):
                ps = PS.get()
                for k in range(8):
                    R.mm(ps[0:M, :], wv[:, k, c0:c0 + M], hnT[:, k, g * GW:(g + 1) * GW], start=(k == 0), stop=(k == 7))
                out_fn(g, ps)

        def proj_tm(wv, c0, N, out_fn):
            per = 4 if N <= 128 else 1
            for tb0 in range(0, NTB, per):
                ps = PS.get()
                for i in range(per):
                    tb = tb0 + i
                    for k in range(8):
                        R.mm(ps[:, i * N:(i + 1) * N], hnT[:, k, tb * 128:(tb + 1) * 128], wv[:, k, c0:c0 + N],
                             start=(k == 0), stop=(k == 7))
                out_fn(tb0, per, ps)

        def outproj_add(wv, kparts, rhs_fn):
            nk = len(kparts)
            for fo in range(8):
                for g in range(NGR):
                    ps = PS.get()
                    for j, K in enumerate(kparts):
                        R.mm(ps[:, :], wv[0:K, j, fo * 128:(fo + 1) * 128], rhs_fn(j, g), start=(j == 0), stop=(j == nk - 1))
                    R.tt(hT[:, fo, g * GW:(g + 1) * GW], hT[:, fo, g * GW:(g + 1) * GW], ps[:, :], ALU.add)

        for seq in range(NSEQ):
            AR.reset()
            xts = [AR.alloc(1024, F32) for _ in range(2)]
            for tb in range(NTB):
                xt = xts[tb % 2]
                R.dma('sp', xt, x_d[seq, tb * 128:(tb + 1) * 128, :])
                for half in range(2):
                    ps = PS.get()
                    for i in range(4):
                        k = half * 4 + i
                        R.tr(ps[:, i * 128:(i + 1) * 128], xt[:, k * 128:(k + 1) * 128], identf)
                    evac(hT[:, half * 4:half * 4 + 4, tb * 128:(tb + 1) * 128],
                         ps[:, :].rearrange("p (a b) -> p a b", b=128))

            for l in range(NLAYER):
                V = vecs[l]
                norm_fm(0, l)
                dump(f"hn{l}", hnT[:, 0, 0:512], (128, 512), BF16)
                if stop == 'norm1':
                    break
                if not SKIPMIX:
                    AR.reset()
                    yaT = AR.alloc(3 * S, BF16).rearrange("p (j t) -> p j t", t=S)
                    qT2 = [AR.alloc(S, BF16) for _ in range(2)]
                    kT2 = [AR.alloc(S, BF16) for _ in range(2)]
                    vtm2 = [AR.alloc(NTB * 128, BF16).rearrange("p (b c) -> p b c", c=128) for _ in range(2)]
                    e_t = [[AR.alloc(512, BF16) for _ in range(2)] for _ in range(2)]
                    sp_t = [[AR.alloc(512, BF16) for _ in range(2)] for _ in range(2)]
                    p_t = [[AR.alloc(512, BF16) for _ in range(2)] for _ in range(2)]
                    a_t = [[AR.alloc(512, BF16) for _ in range(2)] for _ in range(2)]
                    wA = [None, None, None]

                    def loadA(j):
                        sl = next_slab()
                        load_in_cols(sl, 0, l, OFF['qa'] + j * 128, 128, 384)
                        load_in_cols(sl, 128, l, OFF['ka'] + j * 128, 128, 384)
                        wA[j] = load_in_cols(sl, 256, l, OFF['va'] + j * 128, 128, 384)
                    loadA(0)

                    def proj_chunks(jn):
                        wvn = wA[jn]
                        qn, kn, vn = qT2[jn % 2], kT2[jn % 2], vtm2[jn % 2]
                        chunks = []
                        for g in range(NGR):
                            def cq(ps, g=g):
                                for k in range(8):
                                    R.mm(ps[:, :], wvn[:, k, 0:128], hnT[:, k, g * GW:(g + 1) * GW], start=(k == 0), stop=(k == 7))
                                R.ts(qn[:, g * GW:(g + 1) * GW], ps[:, :], 0.125, None, ALU.mult)

                            def ck(ps, g=g):
                                for k in range(8):
                                    R.mm(ps[:, :], wvn[:, k, 128:256], hnT[:, k, g * GW:(g + 1) * GW], start=(k == 0), stop=(k == 7))
                                R.copy(kn[:, g * GW:(g + 1) * GW], ps[:, :])

                            def cv(ps, g=g):
                                for i in range(4):
                                    tb = 4 * g + i
                                    for k in range(8):
                                        R.mm(ps[:, i * 128:(i + 1) * 128], hnT[:, k, tb * 128:(tb + 1) * 128], wvn[:, k, 256:384],
                                             start=(k == 0), stop=(k == 7))
                                R.copy(vn[:, 4 * g:4 * g + 4, :], ps[:, :].rearrange("p (a b) -> p a b", b=128))
                            chunks += [cq, ck, cv]
                        return chunks
                    for c_ in proj_chunks(0):
                        c_(PS.get())
                    for j in range(3):
                        qT, kT, vtm = qT2[j % 2], kT2[j % 2], vtm2[j % 2]
                        pendA = []
                        if j + 1 < 3:
                            loadA(j + 1)
                            pendA = proj_chunks(j + 1)
                        groupsA = []
                        for G in range(NGR):
                            def mk(G=G, j=j):
                                nb = 4 * G + 4
                                its = list(range(nb - 1, -1, -1))
                                n = len(its)
                                stt_ = dict(accs=None, crs=None, psz={})

                                def c0f(i):
                                    return max(0, its[i] - 4 * G) * 128

                                def sZ(i):
                                    b = its[i]
                                    c0 = c0f(i)
                                    for hp in range(2):
                                        pl, ph = hp * 64, hp * 64 + 64
                                        psz = PS.pin()
                                        stt_['psz'][(i, hp)] = psz
                                        R.mm(psz[:, c0:512], kT[pl:ph, b * 128:(b + 1) * 128], qT[pl:ph, G * GW + c0:(G + 1) * GW])

                                def sEL(i):
                                    b = its[i]
                                    c0 = c0f(i)
                                    for hp in range(2):
                                        et = e_t[hp][i % 2][:, c0:512]
                                        pz_ = stt_['psz'].pop((i, hp))
                                        R.act(et, pz_[:, c0:512], AF.Exp)
                                        PS.unpin(pz_)
                                        if b >= 4 * G:
                                            jj = b - 4 * G
                                            R.tt(et, et, cb[:, C_MSB + jj * 512 + c0:C_MSB + (jj + 1) * 512], ALU.mult)
                                    for hp in range(2):
                                        et, spt = e_t[hp][i % 2][:, c0:512], sp_t[hp][i % 2][:, c0:512]
                                        R.act(spt, et, AF.Ln, bias=1.0)

                                def sT(i):
                                    if stt_['accs'] is None:
                                        stt_['accs'] = [PS.pin(), PS.pin()]
                                        stt_['crs'] = [PS.pin(), PS.pin()]
                                    c0 = c0f(i)
                                    for hp in range(2):
                                        R.mm(stt_['crs'][hp][:, c0:512], trisb, sp_t[hp][i % 2][:, c0:512], start=(i == 0), stop=False)

                                def sX(i):
                                    c0 = c0f(i)
                                    for hp in range(2):
                                        R.act(p_t[hp][i % 2][:, c0:512], stt_['crs'][hp][:, c0:512], AF.Exp, scale=-1.0)
                                        R.tt(a_t[hp][i % 2][:, c0:512], e_t[hp][i % 2][:, c0:512], p_t[hp][i % 2][:, c0:512], ALU.mult)

                                def sC(i):
                                    c0 = c0f(i)
                                    if i < n - 1:
                                        for hp in range(2):
                                            R.mm(stt_['crs'][hp][:, c0:512], tricb, sp_t[hp][i % 2][:, c0:512], start=False, stop=False)

                                def sA(i):
                                    b = its[i]
                                    c0 = c0f(i)
                                    for hp in range(2):
                                        pl, ph = hp * 64, hp * 64 + 64
                                        R.mm(stt_['accs'][hp][pl:ph, c0:512], vtm[:, b, pl:ph], a_t[hp][i % 2][:, c0:512],
                                             start=(i == 0), stop=(i == n - 1))

                                def rnd(r):
                                    if r % 2 == 0:
                                        wall_tick(1)
                                    if 0 <= r - 3 < n:
                                        sC(r - 3)
                                    if 0 <= r - 2 < n:
                                        sT(r - 2)
                                    if r < n:
                                        sZ(r)
                                    if 0 <= r - 3 < n:
                                        sA(r - 3)
                                    if 0 <= r - 1 < n:
                                        sEL(r - 1)
                                    if 0 <= r - 2 < n:
                                        sX(r - 2)
                                    if pendA and r % 2 == 1 and r < n:
                                        bk_ = PS.try_get()
                                        if bk_ is not None:
                                            pendA.pop(0)(bk_)
                                    if r == n + 2:
                                        for hp in range(2):
                                            pl, ph = hp * 64, hp * 64 + 64
                                            evac(yaT[pl:ph, j, G * GW:(G + 1) * GW], stt_['accs'][hp][pl:ph, :])
                                        for bk in stt_['accs'] + stt_['crs']:
                                            PS.unpin(bk)
                                return (n + 3, rnd, 2)
                            groupsA.append(mk())
                        run_pipeline(groupsA)
                        while pendA:
                            pendA.pop(0)(PS.get())
                    dump(f"yaT{l}", yaT[:, :, :], (128, 3, S), BF16)
                    if stop == 'attnA':
                        break
                    slo = next_slab()
                    wo = slo[:, 0:3 * 1024].rearrange("p (j n) -> p j n", n=1024)
                    R.dma('pool', wo, wout_d[l, 0:384, :].rearrange("(j p) n -> p j n", p=128))
                    for g in range(NGR):
                        ps = PS.get()
                        for j in range(3):
                            sq = tmpA[j % 2]
                            R.act(sq[:], yaT[:, j, g * GW:(g + 1) * GW], AF.Square)
                            R.mm(ps[:, :], onesf, sq[:], start=(j == 0), stop=(j == 2))
                        t = tmpA[2]
                        R.act(t[:], ps[:, :], AF.Ln, bias=epsc[:, 0:1], scale=1.0 / 384)
                        R.act(rstd_bc[:, g * GW:(g + 1) * GW], t[:], AF.Exp, scale=-0.5)
                        for j in range(3):
                            R.stt(yaT[:, j, g * GW:(g + 1) * GW], yaT[:, j, g * GW:(g + 1) * GW],
                                  V[:, 32 + j:33 + j], rstd_bc[:, g * GW:(g + 1) * GW], ALU.mult, ALU.mult)
                    dump(f"yanT{l}", yaT[:, :, :], (128, 3, S), BF16)
                    outproj_add(wo, [128, 128, 128], lambda j, g: yaT[:, j, g * GW:(g + 1) * GW])
                    if stop == 'outA':
                        break

                    AR.reset()
                    t1 = AR.alloc(S, F32)
                    AR.off -= S * 4
                    ybT = AR.alloc(4 * S, BF16).rearrange("p (h t) -> p h t", t=S)
                    Bf = rstd_bc
                    utm = AR.alloc(NTB * 4, F32).rearrange("p (b h) -> p b h", h=4)
                    nfb = AR.alloc(8, F32)[0:4, 0:1]
                    qb = AR.alloc(S, BF16)
                    kb = AR.alloc(S, BF16)
                    sgo = AR.alloc(S, BF16)
                    vbt = AR.alloc(NTB * 97, BF16).rearrange("p (b c) -> p b c", c=97)
                    R.memset(vbt[:, :, 96:97], 1.0)
                    numS = AR.alloc(GW, F32)
                    raw = [AR.alloc(GW + 4, BF16) for _ in range(3)]
                    dgw = AR.alloc(8 * 96, BF16).rearrange("p (m c) -> p m c", c=96)
                    d_t = [AR.alloc(512, BF16) for _ in range(3)]
                    w_t = [AR.alloc(512, BF16) for _ in range(2)]
                    slg = next_slab()
                    wg_ = load_in_cols(slg, 0, l, OFF['ib'], 8, 8)
                    R.ts(nfb, V[0:4, 82:83], -1.0, None, ALU.mult)
                    proj_fm(wg_, 4, 4, lambda g, ps: R.act(t1[0:4, g * GW:(g + 1) * GW], ps[0:4, :], AF.Exp, bias=nfb, scale=-1.0))
                    R.act(t1[0:4, :], t1[0:4, :], AF.Ln, bias=1.0)
                    R.add('dve', lambda e: e.tensor_tensor_scan(Bf[0:4, :], bcast(onesf[0:4, 0:1], [[0, S]]), t1[0:4, :], 0.0,
                                                                 ALU.mult, ALU.subtract),
                          reads=[onesf[0:4, 0:1], t1[0:4, :]], writes=[Bf[0:4, :]])
                    LNS = float(np.log(96.0 ** -0.5))

                    def u_out(g, ps):
                        uu = tmpA[0][0:4, :]
                        R.stt(uu, ps[0:4, :], V[0:4, 81:82], Bf[0:4, g * GW:(g + 1) * GW], ALU.add, ALU.subtract)
                        R.ts(uu, uu, LNS, None, ALU.add)
                        ps2 = PS.get()
                        for i in range(4):
                            R.tr(ps2[:, i * 4:(i + 1) * 4], uu[:, i * 128:(i + 1) * 128], identf[0:4, 0:4])
                        R.copy(utm[:, 4 * g:4 * g + 4, :], ps2[:, 0:16].rearrange("p (a b) -> p a b", b=4))
                    proj_fm(wg_, 0, 4, u_out)
                    dump(f"Bf{l}", Bf[0:4, :], (4, S))
                    dump(f"utm{l}", utm[:, :, :], (128, NTB, 4))
                    wB = [None] * 4

                    def loadB(h):
                        sl = next_slab()
                        load_in_cols(sl, 0, l, OFF['qb'] + h * 96, 96, 384)
                        load_in_cols(sl, 96, l, OFF['kb'] + h * 96, 96, 384)
                        load_in_cols(sl, 192, l, OFF['vb'] + h * 96, 96, 384)
                        wB[h] = load_in_cols(sl, 288, l, OFF['ob'] + h * 96, 96, 384)
                    loadB(0)
                    for h in range(4):
                        wv = wB[h]
                        if h + 1 < 4:
                            loadB(h + 1)
                        for qk in range(2):
                            cw = 41 + qk * 16 + h * 4
                            for tap in range(4):
                                R.ts(dgw[0:96, qk * 4 + tap, :], identf[0:96, 0:96], V[0:96, cw + tap:cw + tap + 1], None, ALU.mult)
                        for qk in range(2):
                            cbias = 73 + qk * 4 + h
                            dst = qb if qk == 0 else kb
                            R.memset(raw[0][0:96, 1:4], 0.0)

                            def conv_out(g, ps, qk=qk, cbias=cbias, dst=dst):
                                rw_ = raw[g % 3]
                                R.act(rw_[0:96, 4:4 + GW], ps[0:96, :], AF.Copy)
                                if g + 1 < NGR:
                                    R.copy(raw[(g + 1) % 3][0:96, 1:4], rw_[0:96, GW + 1:GW + 4])
                                pc = PS.get()
                                for tap in range(4):
                                    R.mm(pc[0:96, :], dgw[0:96, qk * 4 + tap, :], rw_[0:96, 1 + tap:1 + tap + GW],
                                         start=(tap == 0), stop=(tap == 3))
                                R.act(dst[0:96, g * GW:(g + 1) * GW], pc[0:96, :], AF.Silu, bias=V[0:96, cbias:cbias + 1])
                            proj_fm(wv, qk * 96, 96, conv_out)
                        if h == 0:
                            dump(f"qb0_{l}", qb[0:96, :], (96, S), BF16)
                        proj_tm(wv, 192, 96, lambda tb0, per, ps: evac(
                            vbt[:, tb0:tb0 + per, 0:96], ps[:, 0:per * 96].rearrange("p (a b) -> p a b", b=96)))
                        proj_fm(wv, 288, 96, lambda g, ps: R.act(sgo[0:96, g * GW:(g + 1) * GW], ps[0:96, :], AF.Sigmoid))
                        groupsB = []
                        pend_epi = []
                        for G in range(NGR):
                            def mkB(G=G, h=h):
                                nb = 4 * G + 4
                                n = nb
                                stb = dict(psB=None, num=None, den=None, pss={})

                                def sS(i):
                                    b = i
                                    if stb['psB'] is None:
                                        stb['psB'] = PS.pin()
                                        R.mm(stb['psB'][:, :], cx[0:4, CX_SEL4 + h * 128:CX_SEL4 + (h + 1) * 128], Bf[0:4, G * GW:(G + 1) * GW])
                                    pss = PS.get()
                                    stb['pss'][i] = pss
                                    c0 = max(0, b - 4 * G) * 128
                                    R.mm(pss[:, c0:512], kb[0:96, b * 128:(b + 1) * 128], qb[0:96, G * GW + c0:(G + 1) * GW])
                                    dt_ = d_t[i % 3][:, c0:512]
                                    R.act(dt_, stb['psB'][:, c0:512], AF.Exp, bias=utm[:, b, h:h + 1])
                                    if b >= 4 * G:
                                        jj = b - 4 * G
                                        R.tt(dt_, dt_, cb[:, C_MML + jj * 512 + c0:C_MML + (jj + 1) * 512], ALU.mult)
                                    if i == n - 1:
                                        PS.unpin(stb['psB'])

                                def sW(i):
                                    c0 = max(0, i - 4 * G) * 128
                                    R.tt(w_t[i % 2][:, c0:512], stb['pss'].pop(i)[:, c0:512], d_t[i % 3][:, c0:512], ALU.mult)

                                def sN(i):
                                    b = i
                                    c0 = max(0, i - 4 * G) * 128
                                    if stb['num'] is None:
                                        stb['num'] = PS.pin()
                                    R.mm(stb['num'][0:97, c0:512], vbt[:, b, :], w_t[i % 2][:, c0:512], start=(i == 0), stop=(i == n - 1))

                                def epi_steps():
                                    num = stb['num']
                                    dn, hh, sq = tmpA[0], tmpA[1], tmpA[2]
                                    box = {}

                                    def s0():
                                        R.act(numS[0:97, :], num[0:97, :], AF.Copy)
                                        PS.unpin(num)

                                    def s1():
                                        box['den'] = PS.get()
                                        R.mm(box['den'][0:96, :], cx[0:97, CX_SELR:CX_SELR + 96], numS[0:97, :])

                                    def s5():
                                        R.tt(hh[0:96, :], numS[0:96, :], dn[0:96, :], ALU.mult)
                                        if h == 0 and G == 0:
                                            dump(f"hb00_{l}", hh[0:96, :], (96, 512))

                                    def s7():
                                        box['ps'] = PS.get()
                                        R.mm(box['ps'][0:96, :], onesf[0:96, 0:96], sq[0:96, :])
                                    return [
                                        s0,
                                        s1,
                                        lambda: R.act(dn[0:96, :], box['den'][0:96, :], AF.Abs),
                                        lambda: R.ts(dn[0:96, :], dn[0:96, :], 1.0, None, ALU.max),
                                        lambda: R.act(dn[0:96, :], dn[0:96, :], AF.Ln),
                                        lambda: R.act(dn[0:96, :], dn[0:96, :], AF.Exp, scale=-1.0),
                                        s5,
                                        lambda: R.act(sq[0:96, :], hh[0:96, :], AF.Square),
                                        s7,
                                        lambda: R.act(sq[0:96, :], box['ps'][0:96, :], AF.Ln, bias=epsc[0:96, 0:1], scale=1.0 / 96),
                                        lambda: R.act(sq[0:96, :], sq[0:96, :], AF.Exp, scale=-0.5),
                                        lambda: R.stt(hh[0:96, :], hh[0:96, :], V[0:96, 35 + h:36 + h], sq[0:96, :], ALU.mult, ALU.mult),
                                        lambda: R.tt(ybT[0:96, h, G * GW:(G + 1) * GW], hh[0:96, :], sgo[0:96, G * GW:(G + 1) * GW], ALU.mult),
                                    ]

                                def rnd(r):
                                    if r % 4 == 0:
                                        wall_tick(1)
                                    if 0 <= r - 2 < n:
                                        sN(r - 2)
                                    if r < n:
                                        sS(r)
                                    if 0 <= r - 1 < n:
                                        sW(r - 1)
                                    if r >= 2 and pend_epi:
                                        pend_epi.pop(0)()
                                    if r == n + 1:
                                        while pend_epi:
                                            pend_epi.pop(0)()
                                        pend_epi.extend(epi_steps())
                                return (n + 2, rnd, 2)
                            groupsB.append(mkB())
                        run_pipeline(groupsB)
                        while pend_epi:
                            pend_epi.pop(0)()
                    dump(f"ybT{l}", ybT[0:96, :, :], (96, 4, S), BF16)
                    if stop == 'mlstm':
                        break
                    slo = next_slab()
                    wo = slo[:, 0:4 * 1024].rearrange("p (j n) -> p j n", n=1024)
                    R.dma('pool', wo[0:96, :, :], wout_d[l, 384:768, :].rearrange("(j p) n -> p j n", p=96))
                    outproj_add(wo, [96, 96, 96, 96], lambda j, g: ybT[0:96, j, g * GW:(g + 1) * GW])

                    AR.reset()
                    ycT = AR.alloc(2 * S, BF16).rearrange("p (j t) -> p j t", t=S)
                    uT = AR.alloc(S, BF16)
                    vtmC = AR.alloc(NTB * 128, F32).rearrange("p (b c) -> p b c", c=128)
                    vnb = AR.alloc(NTB * 128, BF16).rearrange("p (b c) -> p b c", c=128)
                    wsf = AR.alloc(4 * 128, F32).rearrange("p (g s) -> p g s", s=128)
                    wsT = AR.alloc(4 * 128, BF16).rearrange("p (g s) -> p g s", s=128)
                    bsT = AR.alloc(512, F32)
                    lng = AR.alloc(128, F32)
                    lnb = AR.alloc(128, F32)
                    st8 = AR.alloc(64, F32)
                    st8b = AR.alloc(64, F32)
                    gx = [AR.alloc(512, F32) for _ in range(3)]
                    R.dma('sp', wsf, sguw_d[l].rearrange("g t s -> t g s"))
                    R.memset(wsf[0:64, :, 64:128], 0.0)
                    ps = PS.get()
                    for gq in range(4):
                        R.tr(ps[:, gq * 128:(gq + 1) * 128], wsf[:, gq, :], identf)
                    R.copy(wsT, ps[:, :].rearrange("p (g s) -> p g s", s=128))

                    def gelu_chain(dst, src_ps, np_, n):
                        R.act(dst, src_ps, AF.Gelu_apprx_tanh)

                    for j in range(2):
                        slc = next_slab()
                        load_in_cols(slc, 0, l, OFF['uc'] + j * 128, 128, 256)
                        wv = load_in_cols(slc, 128, l, OFF['vc'] + j * 128, 128, 256)
                        R.dma('sp', lng, bass.AP(sguln_d, (l * 2 + 0) * 256 + j * 128, [[0, 128], [1, 128]]))
                        R.dma('sp', lnb, bass.AP(sguln_d, (l * 2 + 1) * 256 + j * 128, [[0, 128], [1, 128]]))
                        for gi in range(2):
                            for rep in range(4):
                                R.dma('sp', bsT[gi * 64:(gi + 1) * 64, rep * 128:(rep + 1) * 128],
                                      bass.AP(sgub_d, (l * 4 + 2 * j + gi) * 128, [[0, 64], [1, 128]]))
                        proj_fm(wv, 0, 128, lambda g, ps: gelu_chain(uT[:, g * GW:(g + 1) * GW], ps[:, :], 128, 512))

                        def v_out(tb0, per, ps):
                            gelu_chain(vtmC[:, tb0:tb0 + per, :].rearrange("p a b -> p (a b)"), ps[:, :], 128, 512)
                        proj_tm(wv, 128, 128, v_out)
                        for tb0 in range(0, NTB, 4):
                            vv = vtmC[:, tb0:tb0 + 4, :].rearrange("p a (g c) -> p (a g) c", c=64)
                            mu = st8[:, 0:8]
                            R.add('dve', lambda e, o=mu, i=vv: e.tensor_reduce(o, i, AX.X, ALU.add), reads=[vv], writes=[mu])
                            R.ts(mu, mu, 1.0 / 64, None, ALU.mult)
                            cen = gx[0].rearrange("p (a c) -> p a c", c=64)
                            R.tt(cen, vv, bcast(mu, [[1, 8], [0, 64]]), ALU.subtract)
                            sqv = gx[1].rearrange("p (a c) -> p a c", c=64)
                            R.act(sqv, cen, AF.Square)
                            var = st8b[:, 0:8]
                            R.add('dve', lambda e, o=var, i=sqv: e.tensor_reduce(o, i, AX.X, ALU.add), reads=[sqv], writes=[var])
                            R.act(var, var, AF.Ln, bias=epsc[:, 0:1], scale=1.0 / 64)
                            R.act(var, var, AF.Exp, scale=-0.5)
                            R.tt(cen, cen, bcast(var, [[1, 8], [0, 64]]), ALU.mult)
                            cen4 = gx[0].rearrange("p (a c) -> p a c", c=128)
                            R.tt(cen4, cen4, bcast(lng, [[0, 4], [1, 128]]), ALU.mult)
                            R.tt(vnb[:, tb0:tb0 + 4, :], cen4, bcast(lnb, [[0, 4], [1, 128]]), ALU.add)
                        for tb0 in range(0, NTB, 4):
                            ps = PS.get()
                            for i in range(4):
                                tb = tb0 + i
                                for gi in range(2):
                                    R.mm(ps[gi * 64:(gi + 1) * 64, i * 128:(i + 1) * 128], vnb[:, tb, gi * 64:(gi + 1) * 64],
                                         wsT[:, 2 * j + gi, :])
                            t = gx[2]
                            R.tt(t, ps[:, :], bsT, ALU.add)
                            R.tt(ycT[:, j, tb0 * 128:(tb0 + 4) * 128], t, uT[:, tb0 * 128:(tb0 + 4) * 128], ALU.mult)
                    dump(f"ycT{l}", ycT[:, :, :], (128, 2, S), BF16)
                    if stop == 'sgu':
                        break
                    ycn = ycT
                    slo = next_slab()
                    wo = slo[:, 0:2 * 1024].rearrange("p (j n) -> p j n", n=1024)
                    R.dma('pool', wo, wout_d[l, 768:1024, :].rearrange("(j p) n -> p j n", p=128))
                    for g in range(NGR):
                        ps = PS.get()
                        for j in range(2):
                            sq = tmpA[j % 2]
                            R.act(sq[:], ycT[:, j, g * GW:(g + 1) * GW], AF.Square)
                            R.mm(ps[:, :], onesf, sq[:], start=(j == 0), stop=(j == 1))
                        t = tmpA[2]
                        R.act(t[:], ps[:, :], AF.Ln, bias=epsc[:, 0:1], scale=1.0 / 256)
                        R.act(t[:], t[:], AF.Exp, scale=-0.5)
                        for j in range(2):
                            R.stt(ycn[:, j, g * GW:(g + 1) * GW], ycT[:, j, g * GW:(g + 1) * GW], V[:, 39 + j:40 + j], t[:],
                                  ALU.mult, ALU.mult)
                    outproj_add(wo, [128, 128], lambda j, g: ycn[:, j, g * GW:(g + 1) * GW])
                    dump(f"hmix{l}", hT[:, :, 0:256], (128, 8, 256))
                    if stop == 'mix':
                        break

                wall_tick(len(wall_jobs))
                slg0, slg1, slp = next_slab(), next_slab(), next_slab()
                wgA = slg0[:, 0:4096].rearrange("p (k n) -> p k n", n=1024)
                wgB = slg1[:, 0:4096].rearrange("p (k n) -> p k n", n=1024)
                wpp = slp[:, 0:2048].rearrange("p (k n) -> p k n", n=1024)
                R.dma('pool', wgA, pgw_d[l, 0:512, :].rearrange("(k p) n -> p k n", p=128))
                R.dma('pool', wgB, pgw_d[l, 512:1024, :].rearrange("(k p) n -> p k n", p=128))
                R.dma('pool', wpp, ppw_d[l].rearrange("(k p) n -> p k n", p=128))

                AR.reset()
                wts = AR.alloc(NTB * 2, F32).rearrange("p (b k) -> p b k", k=2)
                idx = AR.alloc(NTB * 2, I32).rearrange("p (b k) -> p b k", k=2)
                idxw = AR.alloc(NBLK, I32)
                mark = AR.off
                g2bc = AR.alloc(DM, F32)
                rws = AR.alloc(8 * 36, F32).rearrange("p (k n) -> p k n", n=36)
                rbb = AR.alloc(36, F32)
                sel = AR.alloc(NTB * 64, F32).rearrange("p (b k e) -> p b k e", k=2, e=32)
                Lall = AR.alloc(NTB * 36, F32)
                AR.off -= (NTB * 36 * 4 + 31) // 32 * 32
                posl = AR.alloc(512, F32).rearrange("p (b e) -> p b e", e=32)
                run = AR.alloc(512, F32).rearrange("p (b e) -> p b e", e=32)
                sm = AR.alloc(512, F32)
                s12b = AR.alloc(NTB * 32, BF16)
                xn2 = AR.alloc(NTB * DM, BF16).rearrange("p (b d) -> p b d", d=DM)
                R.dma('sp', g2bc, bass.AP(g2row_d, l * DM, [[0, 128], [1, DM]]))
                R.dma('sp', rbb, bass.AP(rb_d, l * 36, [[0, 128], [1, 36]]))
                R.dma('sp', rws, rw_d[l].rearrange("(k p) n -> p k n", p=128))
                for k in range(8):
                    R.ts(rws[:, k, :], rws[:, k, :], V[:, 8 + k:9 + k], None, ALU.mult)
                cnt_ps = PS.pin()
                pos_ps = PS.pin()
                L3 = Lall.rearrange("p (b n) -> p b n", n=36)
                for tb in range(NTB):
                    ssq = sm[:, 480 + 2 * (tb % 4):482 + 2 * (tb % 4)]
                    pst = [PS.get(), PS.get()]
                    for half in range(2):
                        for i in range(4):
                            k = half * 4 + i
                            R.tr(pst[half][:, i * 128:(i + 1) * 128], hT[:, k, tb * 128:(tb + 1) * 128], identf)
                        R.act(tmpA[half][:], pst[half][:, :], AF.Square, accum_out=ssq[:, half:half + 1])
                    rs = sm[:, 496 + (tb % 4):497 + (tb % 4)]
                    R.tt(rs, ssq[:, 0:1], ssq[:, 1:2], ALU.add)
                    R.act(rs, rs, AF.Ln, bias=epsc[:, 0:1], scale=1.0 / DM)
                    R.act(rs, rs, AF.Exp, scale=-0.5)
                    for half in range(2):
                        R.stt(xn2[:, tb, half * 512:(half + 1) * 512], pst[half][:, :], rs, g2bc[:, half * 512:(half + 1) * 512],
                              ALU.mult, ALU.mult)
                    psl = PS.get()
                    for k in range(8):
                        R.mm(psl[:, 0:36], hT[:, k, tb * 128:(tb + 1) * 128], rws[:, k, :], start=(k == 0), stop=(k == 7))
                    R.stt(L3[:, tb, :], psl[:, 0:36], rs, rbb, ALU.mult, ALU.add)
                dump(f"Lall{l}", Lall, (128, NTB * 36))
                if stop == 'route1':
                    break
                gl = L3[:, :, 0:4]
                el4 = bass.AP(Lall.tensor, Lall.offset + 4, [list(list(Lall.ap)[0]), [36, NTB], [8, 4], [1, 8]])
                gmax, gsum, v1, v2 = sm[:, 0:16], sm[:, 16:32], sm[:, 32:48], sm[:, 48:64]
                d21, ex, dn_ = sm[:, 64:80], sm[:, 80:96], sm[:, 96:112]
                goh = sm[:, 128:192].rearrange("p (b g) -> p b g", g=4)
                gex = sm[:, 192:256].rearrange("p (b g) -> p b g", g=4)
                pen = sm[:, 256:320].rearrange("p (b g) -> p b g", g=4)
                R.add('dve', lambda e: e.tensor_reduce(gmax, gl, AX.X, ALU.max), reads=[gl], writes=[gmax])
                R.tt(goh, gl, bcast(gmax, [[1, NTB], [0, 4]]), ALU.is_equal)
                R.tt(gex, gl, bcast(gmax, [[1, NTB], [0, 4]]), ALU.subtract)
                R.act(gex, gex, AF.Exp)
                R.add('dve', lambda e: e.tensor_reduce(gsum, gex, AX.X, ALU.add), reads=[gex], writes=[gsum])
                R.add('dve', lambda e: e.reciprocal(gsum, gsum), reads=[gsum], writes=[gsum])
                R.ts(pen, goh, 1e30, -1e30, ALU.mult, ALU.add)
                em = tmpA[0][:, :]
                em3 = em.rearrange("p (b e) -> p b e", e=32)
                R.tt(em.rearrange("p (b g e) -> p b g e", g=4, e=8), el4, bcast(sm[:, 256:320], [[4, NTB], [1, 4], [0, 8]]), ALU.add)
                R.add('dve', lambda e: e.tensor_reduce(v1, em3, AX.X, ALU.max), reads=[em], writes=[v1])
                R.tt(sel[:, :, 0, :], em3, bcast(v1, [[1, NTB], [0, 32]]), ALU.is_equal)
                em2 = tmpA[1][:, :]
                em23 = em2.rearrange("p (b e) -> p b e", e=32)
                R.stt(em23, sel[:, :, 0, :], -1e30, em3, ALU.mult, ALU.add)
                R.add('dve', lambda e: e.tensor_reduce(v2, em23, AX.X, ALU.max), reads=[em2], writes=[v2])
                R.tt(sel[:, :, 1, :], em23, bcast(v2, [[1, NTB], [0, 32]]), ALU.is_equal)
                R.tt(d21, v2, v1, ALU.subtract)
                R.act(ex, d21, AF.Exp)
                R.ts(dn_, ex, 1.0, None, ALU.add)
                R.add('dve', lambda e: e.reciprocal(dn_, dn_), reads=[dn_], writes=[dn_])
                R.tt(wts[:, :, 0], dn_, gsum, ALU.mult)
                R.tt(wts[:, :, 1], wts[:, :, 0], ex, ALU.mult)
                s12b3 = s12b.rearrange("p (b e) -> p b e", e=32)
                R.tt(s12b3, sel[:, :, 0, :], sel[:, :, 1, :], ALU.add)
                dump(f"sel{l}", sel[:, :, :, :], (128, NTB, 2, 32))
                dump(f"wts{l}", wts[:, :, :], (128, NTB, 2))
                if stop == 'route2':
                    break
                for tb in range(NTB):
                    R.mm(cnt_ps[:, tb * 32:(tb + 1) * 32], onesb, s12b3[:, tb, :])
                    R.mm(pos_ps[:, tb * 32:(tb + 1) * 32], tricb, s12b3[:, tb, :])
                cntb = tmpA[2][:, :].rearrange("p (b e) -> p b e", e=32)
                R.copy(cntb, cnt_ps[:, :].rearrange("p (b e) -> p b e", e=32))
                if stop == 'r3':
                    R.copy(tmpA[0][:, :], cnt_ps[:, :])
                    dump(f"c3_{l}", tmpA[0][:, :], (128, 512))
                    break
                R.memset(run[:, 0, :], 0.0)
                for b in range(NTB - 1):
                    R.tt(run[:, b + 1, :], run[:, b, :], cntb[:, b, :], ALU.add)
                ctot = sm[:, 0:32]
                R.tt(ctot, run[:, NTB - 1, :], cntb[:, NTB - 1, :], ALU.add)
                R.copy(posl, pos_ps[:, :].rearrange("p (b e) -> p b e", e=32))
                PS.unpin(cnt_ps)
                PS.unpin(pos_ps)
                cmpb = tmpA[1][:, 0:512].rearrange("p (e m) -> p e m", m=16)
                R.tt(cmpb, bcast(ctot, [[1, 32], [0, 16]]), bcast(cx[:, CX_THR:CX_THR + 16], [[0, 32], [1, 16]]), ALU.is_gt)
                blk = sm[:, 32:64]
                R.add('dve', lambda e, o=blk, i=cmpb: e.tensor_reduce(o, i, AX.X, ALU.add), reads=[tmpA[1][:, 0:512]], writes=[blk])
                pend = sm[:, 64:96]
                R.add('dve', lambda e: e.tensor_tensor_scan(pend, onesf[:, 0:32], blk, 0.0, ALU.mult, ALU.add),
                      reads=[onesf[:, 0:32], blk], writes=[pend])
                pst128 = sm[:, 96:128]
                R.tt(pst128, pend, blk, ALU.subtract)
                R.ts(pst128, pst128, 128.0, None, ALU.mult)
                R.tt(run, run, bcast(pst128, [[0, NTB], [1, 32]]), ALU.add)
                R.tt(posl, posl, run, ALU.add)
                ej = sm[:, 128:192]
                for jc in range(4):
                    cmpj = tmpA[0][:, 0:512].rearrange("p (j e) -> p j e", e=32)
                    R.tt(cmpj, bcast(cx[:, CX_IOTAJ + jc * 16:CX_IOTAJ + jc * 16 + 16], [[1, 16], [0, 32]]),
                         bcast(pend, [[0, 16], [1, 32]]), ALU.is_ge)
                    R.add('dve', lambda e, o=ej[:, jc * 16:(jc + 1) * 16], i=cmpj: e.tensor_reduce(o, i, AX.X, ALU.add),
                          reads=[tmpA[0][:, 0:512]], writes=[ej[:, jc * 16:(jc + 1) * 16]])
                oob = sm[:, 192:256]
                R.ts(oob, ej, 31.5, 1.0e6, ALU.is_gt, ALU.mult)
                R.ts(ej, ej, float(32 * l), 128.0, ALU.add, ALU.mult)
                R.tt(ej, ej, oob, ALU.add)
                R.ts(ej, ej, cx[:, CX_IOTAP:CX_IOTAP + 1], None, ALU.add)
                R.copy(idxw, ej)
                dump(f"ctot{l}", ctot, (128, 32))
                dump(f"ej{l}", ej, (128, 64))
                if stop == 'r4':
                    break
                for k in range(2):
                    tk = tmpA[k][:, :].rearrange("p (b e) -> p b e", e=32)
                    R.tt(tk, sel[:, :, k, :], posl, ALU.mult)
                    sf = sm[:, 320 + 16 * k:336 + 16 * k]
                    R.add('dve', lambda e, o=sf, i=tk: e.tensor_reduce(o, i, AX.X, ALU.add), reads=[tmpA[k][:, :]], writes=[sf])
                    R.copy(idx[:, :, k], sf)
                dump(f"idx{l}", idx[:, :, :], (128, NTB, 2), I32)
                dump(f"posl{l}", posl[:, :, :], (128, NTB, 32))
                if stop == 'route':
                    break
                for tb in range(NTB):
                    for k in range(2):
                        R.add('pool', lambda e, o=Xs.ap(), ix=idx[:, tb, k:k + 1], src=xn2[:, tb, :]: e.indirect_dma_start(
                            out=o, out_offset=bass.IndirectOffsetOnAxis(ap=ix, axis=0), in_=src, in_offset=None),
                            reads=[xn2[:, tb, :], idx[:, tb, k:k + 1]], writes=[("XsV", 0, 1, tb * 2 + k, tb * 2 + k + 1)], dma=True)
                AR.off = mark
                wbuf = [AR.alloc(WROW, BF16) for _ in range(3)]
                xg = [AR.alloc(DM, BF16) for _ in range(2)]
                xgT = [AR.alloc(8 * 128, BF16).rearrange("p (k r) -> p k r", r=128) for _ in range(2)]
                hact = [AR.alloc(256, BF16) for _ in range(2)]
                sgt = [AR.alloc(256, F32) for _ in range(2)]
                hTe = AR.alloc(256, BF16).rearrange("p (c r) -> p c r", r=128)
                ybuf = [AR.alloc(DM, BF16) for _ in range(2)]

                def issue_w(jb):
                    R.add('pool', lambda e, o=wbuf[jb % 3], ix=idxw[:, jb:jb + 1]: gather_w(e, o, ix),
                          reads=[R.whole(Wall), idxw[:, jb:jb + 1]], writes=[wbuf[jb % 3]], dma=True)

                def issue_x(jb):
                    R.dma('sp', xg[jb % 2], Xs[jb * 128:(jb + 1) * 128, :], extra_reads=[("XsV", 0, 1, 0, 1 << 30)])

                def stA(jb):
                    xg_, xgT_ = xg[jb % 2], xgT[jb % 2]
                    for half in range(2):
                        psb = PS.get().bitcast(BF16)
                        for i in range(4):
                            k = half * 4 + i
                            R.tr(psb[:, i * 128:(i + 1) * 128], xg_[:, k * 128:(k + 1) * 128], identb)
                        evac(xgT_[:, half * 4:half * 4 + 4, :], psb[:, 0:512].rearrange("p (a b) -> p a b", b=128))

                def stB(jb):
                    wb_, xgT_ = wbuf[jb % 3], xgT[jb % 2]
                    psg = PS.get()
                    for k in range(8):
                        R.mm(psg[:, :], xgT_[:, k, :], wb_[:, k * 512:(k + 1) * 512], start=(k == 0), stop=(k == 7))
                    R.act(sgt[jb % 2], psg[:, 0:256], AF.Silu)
                    R.tt(hact[jb % 2], sgt[jb % 2], psg[:, 256:512], ALU.mult)

                def stC(jb):
                    psb = PS.get().bitcast(BF16)
                    for c in range(2):
                        R.tr(psb[:, c * 128:(c + 1) * 128], hact[jb % 2][:, c * 128:(c + 1) * 128], identb)
                    evac(hTe, psb[:, 0:256].rearrange("p (a b) -> p a b", b=128))

                def stD(jb):
                    wb_, yb_ = wbuf[jb % 3], ybuf[jb % 2]
                    for fh in range(2):
                        psy = PS.get()
                        for c in range(2):
                            R.mm(psy[:, :], hTe[:, c, :], wb_[:, 4096 + c * 1024 + fh * 512:4096 + c * 1024 + (fh + 1) * 512],
                                 start=(c == 0), stop=(c == 1))
                        evac(yb_[:, fh * 512:(fh + 1) * 512], psy[:, :])
                    R.dma('act', Yd[jb * 128:(jb + 1) * 128, :], yb_)
                issue_w(0)
                issue_x(0)
                issue_w(1)
                issue_x(1)
                issue_w(2)
                for r in range(NBLK + 2):
                    if 0 <= r - 2 < NBLK:
                        stC(r - 2)
                    if 0 <= r - 1 < NBLK:
                        stB(r - 1)
                    if 0 <= r - 2 < NBLK:
                        stD(r - 2)
                        if r + 1 < NBLK and r + 1 >= 3:
                            issue_w(r + 1)
                    if r < NBLK:
                        stA(r)
                        if r + 2 < NBLK:
                            issue_x(r + 2)
                AR.off = mark
                yg = [[AR.alloc(DM, BF16) for _ in range(2)] for _ in range(3)]
                yacc = [AR.alloc(DM, F32) for _ in range(2)]
                for tb in range(NTB):
                    y1b, y2b = yg[tb % 3]
                    y1 = yacc[tb % 2]
                    for k, yy in enumerate((y1b, y2b)):
                        R.add('pool', lambda e, o=yy, ix=idx[:, tb, k:k + 1]: e.indirect_dma_start(
                            out=o, out_offset=None, in_=Yd.ap(), in_offset=bass.IndirectOffsetOnAxis(ap=ix, axis=0)),
                            reads=[R.whole(Yd), idx[:, tb, k:k + 1]], writes=[yy], dma=True)
                    R.act(y1, y1b, AF.Identity, scale=wts[:, tb, 0:1])
                    R.stt(y1, y2b, wts[:, tb, 1:2], y1, ALU.mult, ALU.add)
                    for half in range(2):
                        ps = PS.get()
                        for i in range(4):
                            k = half * 4 + i
                            R.tr(ps[:, i * 128:(i + 1) * 128], y1[:, k * 128:(k + 1) * 128], identf)
                        R.tt(hT[:, half * 4:half * 4 + 4, tb * 128:(tb + 1) * 128], hT[:, half * 4:half * 4 + 4, tb * 128:(tb + 1) * 128],
                             ps[:, :].rearrange("p (a b) -> p a b", b=128), ALU.add)
                dump(f"hmoe{l}", hT[:, :, 0:256], (128, 8, 256))
                if stop == 'moe':
                    break

                AR.reset()
                ptm = AR.alloc(NTB * 256, F32).rearrange("p (b c) -> p b c", c=256)
                pT = AR.alloc(2 * S, BF16).rearrange("p (c t) -> p c t", t=S)
                R.dma('sp', ptm, p_d[l, seq].rearrange("(b p) c -> p b c", p=128))
                for tb0 in range(0, NTB, 2):
                    ps = PS.get()
                    for c in range(2):
                        for tl in range(2):
                            R.tr(ps[:, (c * 2 + tl) * 128:(c * 2 + tl + 1) * 128], ptm[:, tb0 + tl, c * 128:(c + 1) * 128], identf)
                    evac(pT[:, :, tb0 * 128:(tb0 + 2) * 128], ps[:, :].rearrange("p (c t) -> p c t", t=256))
                norm_fm(16, l)
                for fo in range(8):
                    for g in range(NGR):
                        psg = PS.get()
                        for k in range(8):
                            wv = wgA if k < 4 else wgB
                            R.mm(psg[:, :], wv[:, k % 4, fo * 128:(fo + 1) * 128], hnT[:, k, g * GW:(g + 1) * GW],
                                 start=(k == 0), stop=(k == 7))
                        psp = PS.get()
                        for c in range(2):
                            R.mm(psp[:, :], wpp[:, c, fo * 128:(fo + 1) * 128], pT[:, c, g * GW:(g + 1) * GW],
                                 start=(c == 0), stop=(c == 1))
                        sg_ = tmpA[(fo * 4 + g) % 2]
                        R.act(sg_[:], psg[:, :], AF.Sigmoid)
                        R.tt(sg_[:], sg_[:], psp[:, :], ALU.mult)
                        R.tt(hT[:, fo, g * GW:(g + 1) * GW], hT[:, fo, g * GW:(g + 1) * GW], sg_[:], ALU.add, eng='pool')
                dump(f"hple{l}", hT[:, :, 0:256], (128, 8, 256))
            else:
                AR.reset()
                gfbc = AR.alloc(DM, F32)
                ot = [AR.alloc(DM, F32) for _ in range(2)]
                sm = AR.alloc(8, F32)
                R.dma('sp', gfbc, bass.AP(gfrow_d, 0, [[0, 128], [1, DM]]))
                for tb in range(NTB):
                    o_ = ot[tb % 2]
                    ssq = sm[:, 0:2]
                    pst = [PS.get(), PS.get()]
                    for half in range(2):
                        for i in range(4):
                            k = half * 4 + i
                            R.tr(pst[half][:, i * 128:(i + 1) * 128], hT[:, k, tb * 128:(tb + 1) * 128], identf)
                        R.act(tmpA[half][:], pst[half][:, :], AF.Square, accum_out=ssq[:, half:half + 1])
                    rs = sm[:, 2:3]
                    R.tt(rs, ssq[:, 0:1], ssq[:, 1:2], ALU.add)
                    R.act(rs, rs, AF.Ln, bias=epsc[:, 0:1], scale=1.0 / DM)
                    R.act(rs, rs, AF.Exp, scale=-0.5)
                    for half in range(2):
                        R.stt(o_[:, half * 512:(half + 1) * 512], pst[half][:, :], rs, gfbc[:, half * 512:(half + 1) * 512],
                              ALU.mult, ALU.mult)
                    R.dma('sp', out_d[seq, tb * 128:(tb + 1) * 128, :], o_)
                continue
            break
        toks = []
        for e in ('sp', 'pool', 'act'):
            toks += [(('d', e, s), 16 * c) for s, c in enumerate(R.slot_cnt[e]) if c > 0]
        R.wait_all('sp', toks)
        print("recorded ops:", {e: len(R.ops[e]) for e in R.ENG})
        R.emit()
    return nc, dbg_out


def prep_inputs(inputs, NLAYER=2):
    f = lambda a: np.ascontiguousarray(np.asarray(a, dtype=np.float32))
    wg, wu, wd = f(inputs["w_gate"]), f(inputs["w_up"]), f(inputs["w_down"])
    gu = np.concatenate([wg.reshape(2, 32, 8, 128, 256), wu.reshape(2, 32, 8, 128, 256)], axis=-1)
    gu = gu.transpose(0, 1, 3, 2, 4).reshape(2, 32, 128, 4096)
    dn = wd.reshape(2, 32, 2, 128, 1024).transpose(0, 1, 3, 2, 4).reshape(2, 32, 128, 2048)
    wall = np.ascontiguousarray(np.concatenate([gu, dn], axis=-1).reshape(2 * 32 * 128, WROW))
    rw = np.ascontiguousarray(np.concatenate([f(inputs["router_gw"]), f(inputs["router_ew"])], axis=-1))
    rb = np.ascontiguousarray(np.concatenate([f(inputs["router_gb"]), f(inputs["router_eb"])], axis=-1))
    vec = np.zeros((2, 128, NV), np.float32)
    for l in range(2):
        vec[l, :, 0:8] = f(inputs["norm1_g"])[l].reshape(8, 128).T
        vec[l, :, 8:16] = f(inputs["norm2_g"])[l].reshape(8, 128).T
        vec[l, :, 16:24] = f(inputs["ple_norm_g"])[l].reshape(8, 128).T
        vec[l, :, 24:32] = f(inputs["final_g"]).reshape(8, 128).T
        vec[l, :, 32:35] = f(inputs["sb_out_g"])[l].reshape(3, 128).T
        vec[l, 0:96, 35:39] = f(inputs["mnorm_g"])[l].reshape(4, 96).T
        vec[l, :, 39:41] = f(inputs["sgu_out_g"])[l].reshape(2, 128).T
        cw = f(inputs["conv_w"])[l].reshape(4, 2, 4, 96)
        vec[l, 0:96, 41:73] = cw.transpose(3, 1, 2, 0).reshape(96, 32)
        cbb = f(inputs["conv_b"])[l].reshape(2, 4, 96)
        vec[l, 0:96, 73:81] = cbb.transpose(2, 0, 1).reshape(96, 8)
        vec[l, 0:4, 81] = f(inputs["igate_b"])[l]
        vec[l, 0:4, 82] = f(inputs["fgate_b"])[l]
    sguln = np.ascontiguousarray(np.stack([f(inputs["sgu_ln_g"]), f(inputs["sgu_ln_b"])], axis=1))
    c, cx = make_consts()
    shared = dict(w_in=f(inputs["w_in"]), w_out=f(inputs["w_out"]), ple_gate_w=f(inputs["ple_gate_w"]),
                  ple_proj_w=f(inputs["ple_proj_w"]), wall_src=wall, rw=rw, rb=rb, vec=vec,
                  sgu_w=f(inputs["sgu_w"]), sgu_b=f(inputs["sgu_b"]), sgu_ln=sguln, g2row=f(inputs["norm2_g"]),
                  gfrow=f(inputs["final_g"]).reshape(1, DM), consts=c, constsx=cx)
    return shared


_CACHE = {}


def kernel(**inputs):
    NCORE = 8
    x = np.asarray(inputs["x"], dtype=np.float32)
    p = np.asarray(inputs["p"], dtype=np.float32)
    B = x.shape[0]
    nseq = B // NCORE
    shared = prep_inputs(inputs)
    if "nc" not in _CACHE:
        _CACHE["nc"] = build(NSEQ=nseq, NLAYER=2)[0]
    nc = _CACHE["nc"]
    in_maps = []
    for c in range(NCORE):
        m = dict(shared)
        m["x"] = np.ascontiguousarray(x[c * nseq:(c + 1) * nseq])
        m["p"] = np.ascontiguousarray(p[:, c * nseq:(c + 1) * nseq])
        in_maps.append(m)
    res = run_bass_kernel_spmd(nc, in_maps, core_ids=list(range(NCORE)))
    out = np.concatenate([np.asarray(r["out"]) for r in res.results], axis=0)
    return out.astype(np.float32)
```
